# Optimizing a Trainium2 kernel written in Bass

```python
import math
import jax, jax.numpy as jnp
from jax import lax
import numpy as np

D_MODEL = 1024
BATCH = 8
SEQ = 4096
DEPTH = 2

FOX_HEADS = 8
FOX_DIM = 64
FOX_W = FOX_HEADS * FOX_DIM
DSA_HEADS = 8
DSA_DIM = 64
DSA_W = DSA_HEADS * DSA_DIM
IDX_HEADS = 8
IDX_DIM = 64
TOPK_MAX = 256
D_FF = 2816
QB = 128
ALPHA = (2.0 * DEPTH) ** 0.25
BETA = (8.0 * DEPTH) ** -0.25
LN_EPS = 1e-5
NEG_INF = -1e30
IDX_SCALE = (IDX_HEADS ** -0.5) * (IDX_DIM ** -0.5)

IN_WIDTHS = (FOX_W, FOX_W, FOX_W, FOX_HEADS,
             DSA_W, DSA_W, DSA_W, IDX_HEADS * IDX_DIM, IDX_DIM, IDX_HEADS,
             D_MODEL, D_MODEL)
IN_SCALES = (1.0, 1.0, BETA, 1.0,
             1.0, 1.0, BETA, 1.0, 1.0, 1.0,
             1.0, 1.0)
N_IN = sum(IN_WIDTHS)

kernel_name = "fox_dsa_gated_hybrid_deepnorm"


def _layer_norm(x, g, b):
    xf = x.astype(jnp.float32)
    mu = jnp.mean(xf, axis=-1, keepdims=True)
    var = jnp.mean(jnp.square(xf - mu), axis=-1, keepdims=True)
    y = (xf - mu) * lax.rsqrt(var + LN_EPS)
    return (y * g.astype(jnp.float32) + b.astype(jnp.float32)).astype(x.dtype)


def _alibi_slopes(n_heads):
    h = jnp.arange(n_heads, dtype=jnp.float32)
    return jnp.exp2(-8.0 * (h + 1.0) / n_heads)


def _split_columns(proj):
    points = [int(p) for p in np.cumsum(IN_WIDTHS)[:-1]]
    return jnp.split(proj, points, axis=-1)


def _fox_attention(q, k, v, f_logit):
    B, L, H, dh = q.shape
    scale = dh ** -0.5
    c = jnp.cumsum(jax.nn.log_sigmoid(f_logit.astype(jnp.float32)), axis=1)
    c = jnp.transpose(c, (0, 2, 1))
    kpos = jnp.arange(L)

    def block(i):
        start = i * QB
        qb = lax.dynamic_slice_in_dim(q, start, QB, axis=1)
        cb = lax.dynamic_slice_in_dim(c, start, QB, axis=2)
        qpos = start + jnp.arange(QB)
        s = jnp.einsum('bqhd,bkhd->bhqk', qb, k).astype(jnp.float32) * scale
        s = s + (cb[..., None] - c[:, :, None, :])
        s = jnp.where(kpos[None, :] <= qpos[:, None], s, NEG_INF)
        p = jax.nn.softmax(s, axis=-1).astype(v.dtype)
        return jnp.einsum('bhqk,bkhd->bqhd', p, v)

    o = lax.map(block, jnp.arange(L // QB))
    return jnp.transpose(o, (1, 0, 2, 3, 4)).reshape(B, L, H * dh)


def _dsa_attention(q, k, v, iq, ik, iw):
    B, L, H, dh = q.shape
    scale = dh ** -0.5
    topk = min(TOPK_MAX, L // 4)
    slopes = _alibi_slopes(H)
    kpos = jnp.arange(L)
    gather = jax.vmap(lambda arr, idx: arr[idx])

    def block(i):
        start = i * QB
        qpos = start + jnp.arange(QB)
        iqb = lax.dynamic_slice_in_dim(iq, start, QB, axis=1)
        iwb = lax.dynamic_slice_in_dim(iw, start, QB, axis=1)
        rel = jax.nn.relu(jnp.einsum('bqhd,bkd->bqhk', iqb, ik).astype(jnp.float32))
        score = jnp.einsum('bqh,bqhk->bqk', iwb.astype(jnp.float32) * IDX_SCALE, rel)
        score = jnp.where(kpos[None, None, :] <= qpos[None, :, None], score, NEG_INF)
        _, idx = lax.top_k(score, topk)
        kg = gather(k, idx)
        vg = gather(v, idx)
        qb = lax.dynamic_slice_in_dim(q, start, QB, axis=1)
        dist = (qpos[None, :, None] - idx).astype(jnp.float32)
        s = jnp.einsum('bqhd,bqkhd->bhqk', qb, kg).astype(jnp.float32) * scale
        s = s - slopes[None, :, None, None] * dist[:, None]
        s = jnp.where((dist >= 0.0)[:, None], s, NEG_INF)
        p = jax.nn.softmax(s, axis=-1).astype(v.dtype)
        return jnp.einsum('bhqk,bqkhd->bqhd', p, vg)

    o = lax.map(block, jnp.arange(L // QB))
    return jnp.transpose(o, (1, 0, 2, 3, 4)).reshape(B, L, H * dh)


def setup_inputs(seed: int = 0) -> dict:
    key = jax.random.key(seed)
    keys = jax.random.split(key, 32)
    f32 = jnp.float32

    def nrm(k, shape, fan_in, scale=1.0):
        return jax.random.normal(k, shape, f32) * (scale * fan_in ** -0.5)

    x = jax.random.normal(keys[0], (BATCH, SEQ, D_MODEL), f32)
    in_keys = jax.random.split(keys[1], len(IN_WIDTHS))
    w_in = jnp.concatenate(
        [nrm(kk, (DEPTH, D_MODEL, w), D_MODEL, s)
         for kk, w, s in zip(in_keys, IN_WIDTHS, IN_SCALES)], axis=-1)
    b_forget = jax.random.uniform(keys[2], (DEPTH, FOX_HEADS), f32, 1.0, 6.0)
    w_branch_a = nrm(keys[3], (DEPTH, FOX_W, D_MODEL), FOX_W)
    w_branch_b = nrm(keys[4], (DEPTH, DSA_W, D_MODEL), DSA_W)
    w_out = nrm(keys[5], (DEPTH, D_MODEL, D_MODEL), D_MODEL, BETA)
    ln1_g = 1.0 + 0.02 * jax.random.normal(keys[6], (DEPTH, D_MODEL), f32)
    ln1_b = 0.02 * jax.random.normal(keys[7], (DEPTH, D_MODEL), f32)
    w_ffn_in = nrm(keys[8], (DEPTH, D_MODEL, 2 * D_FF), D_MODEL, BETA)
    w_ffn_out = nrm(keys[9], (DEPTH, D_FF, D_MODEL), D_FF, BETA)
    ln2_g = 1.0 + 0.02 * jax.random.normal(keys[10], (DEPTH, D_MODEL), f32)
    ln2_b = 0.02 * jax.random.normal(keys[11], (DEPTH, D_MODEL), f32)
    return {"x": x, "w_in": w_in, "b_forget": b_forget,
            "w_branch_a": w_branch_a, "w_branch_b": w_branch_b, "w_out": w_out,
            "ln1_g": ln1_g, "ln1_b": ln1_b,
            "w_ffn_in": w_ffn_in, "w_ffn_out": w_ffn_out,
            "ln2_g": ln2_g, "ln2_b": ln2_b}


def reference(x, w_in, b_forget, w_branch_a, w_branch_b, w_out,
              ln1_g, ln1_b, w_ffn_in, w_ffn_out, ln2_g, ln2_b):
    B, L, D = x.shape
    for l in range(DEPTH):
        proj = x @ w_in[l]
        (fq, fk, fv, f_logit, dq, dk, dv, iq, ik, iw, g_a, g_b) = _split_columns(proj)
        o_a = _fox_attention(fq.reshape(B, L, FOX_HEADS, FOX_DIM),
                             fk.reshape(B, L, FOX_HEADS, FOX_DIM),
                             fv.reshape(B, L, FOX_HEADS, FOX_DIM),
                             f_logit + b_forget[l])
        o_b = _dsa_attention(dq.reshape(B, L, DSA_HEADS, DSA_DIM),
                             dk.reshape(B, L, DSA_HEADS, DSA_DIM),
                             dv.reshape(B, L, DSA_HEADS, DSA_DIM),
                             iq.reshape(B, L, IDX_HEADS, IDX_DIM), ik, iw)
        merged = (jax.nn.sigmoid(g_a) * (o_a @ w_branch_a[l])
                  + jax.nn.sigmoid(g_b) * (o_b @ w_branch_b[l]))
        x = _layer_norm(ALPHA * x + merged @ w_out[l], ln1_g[l], ln1_b[l])
        gate, up = jnp.split(x @ w_ffn_in[l], 2, axis=-1)
        ffn = (jax.nn.silu(gate) * up) @ w_ffn_out[l]
        x = _layer_norm(ALPHA * x + ffn, ln2_g[l], ln2_b[l])
    return x
```

```python
import math
from contextlib import ExitStack
import numpy as np
import concourse.bass as bass
import concourse.mybir as mybir
from concourse.bass_utils import run_bass_kernel_spmd

F32 = mybir.dt.float32
BF16 = mybir.dt.bfloat16
U8 = mybir.dt.uint8
ALU = mybir.AluOpType
AF = mybir.ActivationFunctionType

D = 1024
DFF = 2816
ALPHA = 4.0 ** 0.25
LN_EPS = 1e-5
IDX_SCALE = (8 ** -0.5) * (64 ** -0.5)
TOPK = 256
BIS_B = 64.0
BIS_N = 14
ALIBI_C = 50.0


class Sched:
    def __init__(self, nc, stack):
        self.nc = nc
        self.names = ['sp', 'act', 'dve', 'pool', 'pe']
        self.lists = {k: [] for k in self.names}
        self.sem = {k: stack.enter_context(nc.semaphore("s_" + k)) for k in ['pe', 'act', 'dve', 'pool']}
        self.cnt = {k: 0 for k in self.sem}
        self.ndma = 12
        self.dsem = {q: [stack.enter_context(nc.semaphore("d_%s%d" % (q, i))) for i in range(self.ndma)]
                     for q in ['sp', 'pool', 'act']}
        self.dcnt = {q: 0 for q in self.dsem}
        self.waited = {k: {} for k in self.names}
        self.lastw = {}
        self.readers = {}
        self.ninstr = 0

    def _wait(self, eng, tok):
        semid, sem, val = tok[0], tok[1], tok[2]
        w = self.waited[eng]
        if w.get(semid, 0) >= val:
            return
        w[semid] = val
        self.lists[eng].append(('w', sem, val))

    def _deps(self, eng, reads, writes):
        for r in reads:
            t = self.lastw.get(r)
            if t is not None:
                if not (t[3] == eng and eng == 'pe'):
                    self._wait(eng, t)
        for wk in writes:
            t = self.lastw.get(wk)
            if t is not None and (t[3] != eng or eng != 'pe'):
                self._wait(eng, t)
            rd = self.readers.get(wk)
            if rd:
                for t in rd.values():
                    if t[3] != eng:
                        self._wait(eng, t)

    def _commit(self, tok, reads, writes):
        for wk in writes:
            self.lastw[wk] = tok
            self.readers[wk] = {}
        for r in reads:
            self.readers.setdefault(r, {})[tok[0]] = tok

    def op(self, eng, meth, kw, reads=(), writes=()):
        fn = lambda e: getattr(e, meth)(**kw)
        self._deps(eng, reads, writes)
        self.cnt[eng] += 1
        tok = (eng, self.sem[eng], self.cnt[eng], eng)
        self.lists[eng].append(('o', fn, self.sem[eng], 1))
        self._commit(tok, reads, writes)
        self.ninstr += 1
        return tok

    def dma(self, out_ap, in_ap, reads=(), writes=(), q='sp'):
        self._deps(q, reads, writes)
        i = self.dcnt[q]
        self.dcnt[q] += 1
        slot, rnd = i % self.ndma, i // self.ndma
        sem = self.dsem[q][slot]
        semid = ('d', q, slot)
        if rnd > 0:
            self._wait(q, (semid, sem, 16 * rnd))
        tok = (semid, sem, 16 * (rnd + 1), 'dma')
        self.lists[q].append(('o', lambda e: e.dma_start(out=out_ap, in_=in_ap), sem, 16))
        self._commit(tok, reads, writes)
        self.ninstr += 1
        return tok

    def end_phase(self, block):
        for q in self.dsem:
            n = self.dcnt[q]
            for slot in range(min(n, self.ndma)):
                last = ((n - 1 - slot) // self.ndma) + 1
                self._wait(q, (('d', q, slot), self.dsem[q][slot], 16 * last))
        decos = {'sp': block.sync, 'act': block.scalar, 'dve': block.vector,
                 'pool': block.gpsimd, 'pe': block.tensor}
        for name in self.names:
            lst = self.lists[name]
            self.lists[name] = []

            def body(e, lst=lst):
                for it in lst:
                    if it[0] == 'w':
                        e.wait_ge(it[1], it[2])
                    else:
                        it[1](e).then_inc(it[2], it[3])
            decos[name](body)


class Builder:
    def __init__(self, L, nlayers=2, dbg=()):
        self.L = L
        self.NT = L // 128
        self.NQ = L // 512
        self.nlayers = nlayers
        self.dbg = dbg
        self.nc = bass.Bass("TRN2", target_bir_lowering=False)
        nc = self.nc
        self.inputs = {}

        NT, NQ = self.NT, self.NQ
        self.shapes = {
            "x": ([L, D], F32), "w_in": ([2, 128, 8, 5712], F32), "w_iwrep": ([2, 128, 8, 512], F32),
            "bfg": ([2, 8, 1], F32), "w_a": ([2, 128, 4, D], F32), "w_b": ([2, 128, 4, D], F32),
            "w_o": ([2, 128, 8, D], F32), "lnp": ([2, 128, 4, 8], F32),
            "w_f1": ([2, 128, 8, 2 * DFF], F32), "w_f2": ([2, 128, 22, D], F32),
            "c_ident": ([128, 128], F32), "c_trimask": ([128, 128], F32),
            "c_causT": ([128, 4, 512], F32), "c_targ": ([128, NT], F32),
            "c_kbias": ([128, NT, 8], F32), "c_kbd": ([128, NT, 8], F32), "c_negslope": ([128, 8, 128], F32), "c_tpos": ([128, L], F32),
        }
        self.scr_shapes = {
            "out": ([L, D], F32),
            "xres": ([128, 8, L], F32), "xbf": ([128, 8, L], BF16), "oa": ([128, 4, L], BF16),
            "ob": ([128, 4, L], BF16), "cspl": ([8, 3, L], BF16), "mskd": ([NQ, 128, NT, 512], U8),
            "wb_a": ([128, 4, D], BF16), "wb_b": ([128, 4, D], BF16), "wb_o": ([128, 8, D], BF16),
            "wb_ga": ([128, 8, D], BF16), "wb_gb": ([128, 8, D], BF16),
            "wb_f1": ([128, 8, 2 * DFF], BF16), "wb_f2": ([128, DFF // 128, D], BF16),
        }
        self._aps = {}

    def __getattr__(self, name):
        d = self.__dict__
        if 'shapes' in d and name in d['shapes']:
            if name not in d['_aps']:
                shp, dt = d['shapes'][name]
                d['_aps'][name] = d['nc'].dram_tensor(name, list(shp), dt, kind="ExternalInput").ap()
                d['inputs'][name] = (tuple(shp), dt)
            return d['_aps'][name]
        if 'scr_shapes' in d and name in d['scr_shapes']:
            if name not in d['_aps']:
                shp, dt = d['scr_shapes'][name]
                kind = "ExternalOutput" if (name in d['dbg'] or name == "out") else "Internal"
                d['_aps'][name] = d['nc'].dram_tensor(name, list(shp), dt, kind=kind).ap()
            return d['_aps'][name]
        raise AttributeError(name)

    def T(self, es, name, shape, dt):
        self._uid = getattr(self, '_uid', 0) + 1
        return es.enter_context(self.nc.sbuf_tensor("%s_%d" % (name, self._uid), list(shape), dt))

    def PS(self, es, n=8):
        self._uid = getattr(self, '_uid', 0) + 1
        return [es.enter_context(self.nc.psum_tensor("ps%d_%d" % (i, self._uid), [128, 512], F32)) for i in range(n)]

    def wspecs(self, l):
        NJ = DFF // 128
        return [("a", self.wb_a, self.w_a[l], 4, D, 0), ("b", self.wb_b, self.w_b[l], 4, D, 0),
                ("o", self.wb_o, self.w_o[l], 8, D, 0), ("ga", self.wb_ga, self.w_in[l], 8, D, 3664),
                ("gb", self.wb_gb, self.w_in[l], 8, D, 4688), ("f1", self.wb_f1, self.w_f1[l], 8, 2 * DFF, 0),
                ("f2", self.wb_f2, self.w_f2[l], NJ, D, 0)]

    def load_wb(self, S, dst, src, C, N, name, tag):
        for c in range(C):
            S.dma(dst[:, c, :], src[:, c, :], reads=[('wb', name, c, n0) for n0 in range(0, N, 2048)], writes=[tag])

    def load_w(self, S, dst, src, C, N, stage, tag, col0=0):
        engs = ['dve', 'pool', 'act']
        step = stage[0].shape[1]
        for c in range(C):
            for n0 in range(0, N, step):
                n1 = min(N, n0 + step)
                i = self._ldi
                self._ldi += 1
                sl = i % 2
                st = stage[sl]
                S.dma(st[:, 0:n1 - n0], src[:, c, col0 + n0:col0 + n1], reads=[tag + 'src'], writes=[('stage', sl)])
                e = engs[i % 3]
                d_ap = dst[:, c, n0:n1]
                s_ap = st[:, 0:n1 - n0]
                S.op(e, 'copy' if e == 'act' else 'tensor_copy', dict(out=d_ap, in_=s_ap),
                     reads=[('stage', sl)], writes=[tag])

    def phase0(self, S):
        nc, L, NT, NQ = self.nc, self.L, self.NT, self.NQ
        with ExitStack() as es:
            ident = self.T(es, "ident", [128, 128], F32)
            xin = [self.T(es, "xin%d" % i, [128, D], F32) for i in range(2)]
            xTf = [self.T(es, "xTf%d" % i, [128, 8, 512], F32) for i in range(2)]
            xTb = [self.T(es, "xTb%d" % i, [128, 8, 512], BF16) for i in range(2)]
            import os
            NB = int(os.environ.get('P0_NB', '8'))
            ps = self.PS(es, NB)
            block = es.enter_context(nc.Block())
            S.dma(ident[:, :], self.c_ident[:, :], writes=['ident'])
            for Q in range(NQ):
                s = Q % 2
                for tt in range(4):
                    tile = Q * 4 + tt
                    sl = tile % 2
                    S.dma(xin[sl][:, :], self.x[tile * 128:(tile + 1) * 128, :], writes=[('xin', sl)])
                    for half in range(2):
                        b = (tile * 2 + half) % NB
                        for cc in range(4):
                            c = half * 4 + cc
                            o_ap = ps[b][:, cc * 128:(cc + 1) * 128]
                            i_ap = xin[sl][:, c * 128:(c + 1) * 128]
                            S.op('pe', 'transpose', dict(out=o_ap, in_=i_ap, identity=ident[:, :]),
                                 reads=[('xin', sl), 'ident'], writes=[('ps', b)])
                        src = ps[b][:, :].rearrange("p (a b) -> p a b", a=4)
                        d1 = xTf[s][:, half * 4:(half + 1) * 4, tt * 128:(tt + 1) * 128]
                        d2 = xTb[s][:, half * 4:(half + 1) * 4, tt * 128:(tt + 1) * 128]
                        MODE = os.environ.get('P0_MODE', 'ad')
                        if 'a' in MODE:
                            S.op('act', 'copy', dict(out=d1, in_=src),
                                 writes=[('ps', b), ('xTf', s, tt, half)])
                        if 'd' in MODE:
                            S.op('dve', 'tensor_copy', dict(out=d2, in_=src),
                                 writes=[('ps', b), ('xTb', s, tt, half)])
                rk = [('xTf', s, tt, h) for tt in range(4) for h in range(2)]
                S.dma(self.xres[:, :, Q * 512:(Q + 1) * 512], xTf[s][:, :, :], reads=rk, writes=[('xres', Q)])
                rk = [('xTb', s, tt, h) for tt in range(4) for h in range(2)]
                S.dma(self.xbf[:, :, Q * 512:(Q + 1) * 512], xTb[s][:, :, :], reads=rk, writes=[('xbf', Q)])
            S.end_phase(block)

    def emit_attention(self, S, ps, PT, PTm, items, Dp=4, sbanks=(0, 1, 2, 6, 7)):
        n = len(items)
        nS = len(sbanks)
        nP = len(PT)
        deferred = []
        for i in range(n + Dp):
            if i < n:
                it = items[i]
                sb = sbanks[i % nS]
                n0 = it['n0']
                nq = len(it['qk'])
                for j, (lh, rh, c0, c1, rk) in enumerate(it['qk']):
                    o_ap = ps[sb][:, c0:c1]
                    S.op('pe', 'matmul', dict(
                        out=o_ap, lhsT=lh, rhs=rh, start=(j == 0), stop=(j == nq - 1)),
                        reads=rk, writes=[('ps', sb)])
                pt = PT[i % nP]
                o_ap = pt[:, n0:512]
                i_ap = ps[sb][:, n0:512]
                b_ap = it['bias']
                S.op('act', 'activation', dict(
                    out=o_ap, in_=i_ap, func=AF.Exp, bias=b_ap, scale=1.0),
                    reads=it['bias_keys'], writes=[('ps', sb), ('pt', i % nP)])
                if it.get('mask') is not None:
                    m_ap, mkey = it['mask']
                    o2 = PTm[i % nP][:, n0:512]
                    S.op('dve', 'tensor_tensor', dict(
                        out=o2, in0=o_ap, in1=m_ap, op=ALU.mult),
                        reads=[('pt', i % nP), mkey], writes=[('ptm', i % nP)])
            k = i - Dp
            if k >= 0:
                it = items[k]
                n0 = it['n0']
                masked = it.get('mask') is not None
                rhs = (PTm if masked else PT)[k % nP][:, n0:512]
                ob = it['obank']
                o_ap = ps[ob][0:it['M'], n0:512]
                lh = it['pv_lhsT']
                S.op('pe', 'matmul', dict(
                    out=o_ap, lhsT=lh, rhs=rhs, start=it['first'], stop=it['last']),
                    reads=[('ptm' if masked else 'pt', k % nP)] + it['v_keys'], writes=[('ps', ob)])
                if it['last']:
                    it['normA']()
                    deferred.append((i + 2, it['normB']))
            while deferred and deferred[0][0] <= i:
                deferred.pop(0)[1]()
        for _, fn in deferred:
            fn()

    def make_norm(self, S, ps, ob, odd, rs, rs2, rinv, bcs, onesf, oT_ap_fn, okey):
        sr = 32 if odd else 64
        p0 = 64 if odd else 0

        def normA():
            S.op('dve', 'tensor_scalar', dict(out=rs[sr:sr + 1, :], in0=ps[ob][sr:sr + 1, :], scalar1=1e-30,
                                                  scalar2=None, op0=ALU.max), writes=[('ps', ob), 'rs'])
            S.op('dve', 'reciprocal', dict(out=rinv[sr:sr + 1, :], in_=rs[sr:sr + 1, :]), reads=['rs'], writes=['rinv'])

        def normB():
            S.op('pe', 'matmul', dict(out=ps[5][:, :], lhsT=onesf[sr:sr + 1, 0:128], rhs=rinv[sr:sr + 1, :],
                                          start=True, stop=True), reads=['rinv', 'onesf'], writes=[('ps', 5)])
            S.op('act', 'copy', dict(out=bcs[:, :], in_=ps[5][:, :]), writes=[('ps', 5), 'bcs'])
            S.op('dve', 'tensor_tensor', dict(out=oT_ap_fn(p0), in0=ps[ob][p0:p0 + 64, :], in1=bcs[p0:p0 + 64, :],
                                                  op=ALU.mult), reads=['bcs'], writes=[('ps', ob), okey])
        return normA, normB

    def phase_fox(self, S, l):
        nc, L, NT, NQ = self.nc, self.L, self.NT, self.NQ
        with ExitStack() as es:
            T = lambda name, shape, dt: self.T(es, name, shape, dt)
            xT = T("xT", [128, 8, L], BF16)
            wq = T("wq", [128, 8, 512], BF16)
            wk = T("wk", [128, 8, 512], BF16)
            wv = T("wv", [128, 8, 512], BF16)
            wf = T("wf", [128, 8, 8], BF16)
            stage = [T("stg%d" % i, [128, 1024], F32) for i in range(2)]
            Vb = T("Vb", [128, NT, 4, 160], BF16)
            qa = [T("qa%d" % i, [67, L], BF16) for i in range(2)]
            ka = [T("ka%d" % i, [67, L], BF16) for i in range(2)]
            negc = T("negc", [128, NT, 8], F32)
            identf = T("identf", [128, 128], F32)
            identb = T("identb", [128, 128], BF16)
            trim = T("trim", [128, 128], BF16)
            onesf = T("onesf", [128, 512], F32)
            bcol = T("bcol", [8, 2], F32)
            e8 = T("e8", [8, 512], F32)
            sp8 = T("sp8", [8, 512], F32)
            cb = [T("cb%d" % i, [8, 512], F32) for i in range(2)]
            r8 = e8
            r9 = sp8
            c3 = [T("c3%d" % i, [8, 3, 512], BF16) for i in range(2)]
            PT = [T("pt%d" % i, [128, 512], BF16) for i in range(6)]
            rs = T("rs", [128, 512], F32)
            rs2 = None
            rinv = T("rinv", [128, 512], F32)
            bcs = T("bcs", [128, 512], F32)
            oT = T("oT", [128, L], BF16)
            ps = self.PS(es)
            block = es.enter_context(nc.Block())

            for Q in range(NQ):
                S.dma(xT[:, :, Q * 512:(Q + 1) * 512], self.xbf[:, :, Q * 512:(Q + 1) * 512],
                      reads=[('xbf', Q)], writes=[('xT', Q)])
            S.dma(identf[:, :], self.c_ident[:, :], writes=['identf'])
            S.dma(stage[0][:, 0:128], self.c_trimask[:, :], writes=[('stage', 0)])
            S.op('dve', 'tensor_copy', dict(out=trim[:, :], in_=stage[0][:, 0:128]), reads=[('stage', 0)], writes=['trim'])
            S.op('dve', 'tensor_copy', dict(out=identb[:, :], in_=identf[:, :]), reads=['identf'], writes=['identb'])
            S.op('pool', 'memset', dict(ap=onesf[:, :], constant=1.0), writes=['onesf'])
            S.op('pool', 'memset', dict(ap=Vb[:, :, :, :], constant=0.0), writes=['Vb0'])
            S.op('pool', 'memset', dict(ap=Vb[:, :, :, 64:65], constant=1.0), reads=['Vb0'], writes=['Vb1'])
            for i in range(2):
                S.op('pool', 'memset', dict(ap=ka[i][64:67, :], constant=1.0), writes=[('kac', i)])
            S.dma(bcol[:, 0:1], self.bfg[l, :, :], writes=['bcol0'])
            S.op('dve', 'tensor_scalar', dict(out=bcol[:, 1:2], in0=bcol[:, 0:1], scalar1=-1.0, scalar2=None,
                                                  op0=ALU.mult), reads=['bcol0'], writes=['bcol1'])
            win = self.w_in[l]
            self.load_w(S, wq, win, 8, 512, stage, 'wq', col0=0)
            self.load_w(S, wk, win, 8, 512, stage, 'wk', col0=512)
            self.load_w(S, wv, win, 8, 512, stage, 'wv', col0=1024)
            self.load_w(S, wf, win, 8, 8, stage, 'wf', col0=1536)

            for Q in range(NQ):
                s = Q % 2
                qs = slice(Q * 512, (Q + 1) * 512)
                for c in range(8):
                    S.op('pe', 'matmul', dict(out=ps[6][0:8, :], lhsT=wf[:, c, :], rhs=xT[:, c, qs],
                                                             start=(c == 0), stop=(c == 7)),
                         reads=['wf', ('xT', Q)], writes=[('ps', 6)])
                S.op('act', 'activation', dict(out=e8[:, :], in_=ps[6][0:8, :], func=AF.Exp, bias=bcol[:, 1:2], scale=-1.0),
                     reads=['bcol1'], writes=[('ps', 6), 'e8'])
                S.op('act', 'activation', dict(out=sp8[:, :], in_=e8[:, :], func=AF.Ln, bias=onesf[0:8, 0:1], scale=1.0),
                     reads=['e8', 'onesf'], writes=['sp8'])
                if Q == 0:
                    init = 0.0
                else:
                    init = cb[1 - s][:, 511:512]
                S.op('dve', 'tensor_tensor_scan', dict(out=cb[s][:, :], data0=onesf[0:8, :], data1=sp8[:, :],
                                                                           initial=init, op0=ALU.mult, op1=ALU.subtract),
                     reads=['sp8', 'onesf', ('cb', 1 - s)], writes=[('cb', s)])
                S.op('dve', 'tensor_copy', dict(out=c3[s][:, 0, :], in_=cb[s][:, :]), reads=[('cb', s)], writes=[('c3a', s)])
                S.op('dve', 'tensor_tensor', dict(out=r8[:, :], in0=cb[s][:, :], in1=c3[s][:, 0, :], op=ALU.subtract),
                     reads=[('cb', s), ('c3a', s)], writes=['e8'])
                S.op('dve', 'tensor_copy', dict(out=c3[s][:, 1, :], in_=r8[:, :]), reads=['e8'], writes=[('c3b', s)])
                S.op('dve', 'tensor_tensor', dict(out=r9[:, :], in0=r8[:, :], in1=c3[s][:, 1, :], op=ALU.subtract),
                     reads=['e8', ('c3b', s)], writes=['sp8'])
                S.op('dve', 'tensor_copy', dict(out=c3[s][:, 2, :], in_=r9[:, :]), reads=['sp8'], writes=[('c3c', s)])
                S.dma(self.cspl[:, :, qs], c3[s][:, :, :], reads=[('c3a', s), ('c3b', s), ('c3c', s)], writes=[('cspl', Q)])
                for tt in range(4):
                    tile = Q * 4 + tt
                    S.op('pe', 'transpose', dict(out=ps[7][:, tile * 8:(tile + 1) * 8],
                                                                           in_=cb[s][:, tt * 128:(tt + 1) * 128],
                                                                           identity=identf[0:8, 0:8]),
                         reads=[('cb', s), 'identf'], writes=[('ps', 7)])
            S.op('dve', 'tensor_scalar', dict(out=negc[:, :, :].rearrange("p a b -> p (a b)"), in0=ps[7][:, 0:NT * 8],
                                                  scalar1=-1.0, scalar2=None, op0=ALU.mult),
                 writes=[('ps', 7), 'negc'])

            for tile in range(NT):
                b = 6 + tile % 2
                ts_ = slice(tile * 128, (tile + 1) * 128)
                for c in range(8):
                    S.op('pe', 'matmul', dict(out=ps[b][:, :], lhsT=xT[:, c, ts_], rhs=wv[:, c, :],
                                                                   start=(c == 0), stop=(c == 7)),
                         reads=['wv', ('xT', tile // 4)], writes=[('ps', b)])
                src = ps[b][:, :].rearrange("p (a b d) -> p a b d", a=4, b=2)
                S.op('act', 'copy', dict(out=Vb[:, tile, :, 0:64], in_=src[:, :, 0, :]),
                     reads=['Vb1'], writes=[('ps', b), ('V', tile, 0)])
                S.op('dve', 'tensor_copy', dict(out=Vb[:, tile, :, 96:160], in_=src[:, :, 1, :]),
                     reads=['Vb1'], writes=[('ps', b), ('V', tile, 1)])

            for h in range(8):
                hb = h % 2
                pr = h // 2
                odd = (h % 2 == 1)
                hs = slice(h * 64, (h + 1) * 64)
                S.dma(qa[hb][64:67, :], self.cspl[h, :, :], reads=[('cspl', Q) for Q in range(NQ)], writes=[('qac', hb)])
                for Q in range(NQ):
                    qs = slice(Q * 512, (Q + 1) * 512)
                    for c in range(8):
                        S.op('pe', 'matmul', dict(out=ps[6][0:64, :], lhsT=wq[:, c, hs], rhs=xT[:, c, qs],
                                                                 start=(c == 0), stop=(c == 7)),
                             reads=['wq', ('xT', Q)], writes=[('ps', 6)])
                    S.op('dve', 'tensor_scalar', dict(out=qa[hb][0:64, qs], in0=ps[6][0:64, :], scalar1=0.125,
                                                                scalar2=None, op0=ALU.mult),
                         writes=[('ps', 6), ('qa', hb, Q)])
                    for c in range(8):
                        S.op('pe', 'matmul', dict(out=ps[7][0:64, :], lhsT=wk[:, c, hs], rhs=xT[:, c, qs],
                                                                 start=(c == 0), stop=(c == 7)),
                             reads=['wk', ('xT', Q)], writes=[('ps', 7)])
                    S.op('dve', 'tensor_copy', dict(out=ka[hb][0:64, qs], in_=ps[7][0:64, :]),
                         writes=[('ps', 7), ('ka', hb, Q)])
                items = []
                for Q in range(NQ):
                    ob = 3 + Q % 2
                    qs = slice(Q * 512, (Q + 1) * 512)
                    nA, nB = self.make_norm(S, ps, ob, odd, rs, rs2, rinv, bcs, onesf,
                                            lambda p0, qs=qs: oT[p0:p0 + 64, qs], ('oT', Q))
                    for kb in range(4 * Q + 4):
                        j = kb - 4 * Q
                        n0 = 128 * j if j > 0 else 0
                        ks = slice(kb * 128, (kb + 1) * 128)
                        qk = [(ka[hb][0:67, ks], qa[hb][0:67, Q * 512 + n0:(Q + 1) * 512], n0, 512,
                               [('qa', hb, Q), ('qac', hb), ('ka', hb, kb // 4), ('kac', hb)])]
                        if j >= 0:
                            qk.append((identb[:, :], trim[:, :], n0, n0 + 128, ['identb', 'trim']))
                        if odd:
                            lh, M = Vb[:, kb, pr, 32:160], 128
                        else:
                            lh, M = Vb[:, kb, pr, 0:128], 128
                        items.append(dict(qk=qk, n0=n0, bias=negc[:, kb, h:h + 1], bias_keys=['negc'],
                                          pv_lhsT=lh, M=M, obank=ob, first=(kb == 0), last=(kb == 4 * Q + 3),
                                          v_keys=[('V', kb, 0), ('V', kb, 1), 'Vb1'], normA=nA, normB=nB))
                self.emit_attention(S, ps, PT, None, items)
                if odd:
                    S.dma(self.oa[:, pr, :], oT[:, :], reads=[('oT', Q) for Q in range(NQ)], writes=[('oa', pr)])
            S.end_phase(block)

    def phase_idx(self, S, l):
        nc, L, NT, NQ = self.nc, self.L, self.NT, self.NQ
        topk = min(TOPK, L // 4)
        with ExitStack() as es:
            T = lambda name, shape, dt: self.T(es, name, shape, dt)
            wik2 = T("wik2", [128, 8, 128], BF16)
            wiq = T("wiq", [128, 8, 512], BF16)
            wiw = T("wiw", [128, 8, 8], BF16)
            stage = [T("stg%d" % i, [128, 2048], F32) for i in range(2)]
            ikT = T("ikT", [128, L], BF16)
            xs = [T("xs%d" % i, [128, 8, 512], BF16) for i in range(2)]
            sc = T("sc", [128, 4, L], F32)
            mkq = T("mkq", [128, 4, L], BF16)
            junk = T("junk", [128, L], BF16)
            iqT = T("iqT", [128, 4, 512], BF16)
            rl = [T("rl%d" % i, [128, 512], F32) for i in range(4)]
            wtk = T("wtk", [128, 4, 8], F32)
            caus = T("caus", [128, 4, 512], F32)
            targ = T("targ", [128, NT], F32)
            lo = T("lo", [128, 4], F32)
            hi = T("hi", [128, 4], F32)
            step = T("step", [128, 4], F32)
            thr = T("thr", [128, 4], F32)
            cnt = T("cnt", [128, 4], F32)
            ge = T("ge", [128, 4], F32)
            identb = T("identb", [128, 128], BF16)
            mk = T("mk", [128, NT, 512], U8)
            ps = self.PS(es, 6)
            self._uid += 1
            pstb = [es.enter_context(nc.psum_tensor("pst%d_%d" % (i, self._uid), [128, 1024], BF16)) for i in range(2)]
            block = es.enter_context(nc.Block())

            win = self.w_in[l]
            S.dma(caus[:, :, :], self.c_causT[:, :, :], writes=['caus'])
            S.dma(targ[:, :], self.c_targ[:, :], writes=['targ'])
            S.dma(stage[0][:, 0:128], self.c_ident[:, :], writes=[('stage', 0)])
            S.op('dve', 'tensor_copy', dict(out=identb[:, :], in_=stage[0][:, 0:128]), reads=[('stage', 0)], writes=['identb'])
            self.load_w(S, wik2[:, :, 0:64], win, 8, 64, stage, 'wik', col0=3592)
            S.op('dve', 'tensor_copy', dict(out=wik2[:, :, 64:128], in_=wik2[:, :, 0:64]), reads=['wik'], writes=['wik2'])
            self.load_w(S, wiq, win, 8, 512, stage, 'wiq', col0=3080)
            self.load_w(S, wiw, win, 8, 8, stage, 'wiw', col0=3656)

            xcnt = [0]

            def load_xs(Q):
                i = xcnt[0] % 2
                xcnt[0] += 1
                S.dma(xs[i][:, :, :], self.xbf[:, :, Q * 512:(Q + 1) * 512], reads=[('xbf', Q)], writes=[('xs', i)])
                return i

            for Q in range(NQ):
                i = load_xs(Q)
                for c in range(8):
                    S.op('pe', 'matmul', dict(out=ps[4][:, :], lhsT=wik2[:, c, :], rhs=xs[i][:, c, :], start=(c == 0), stop=(c == 7)),
                         reads=['wik', 'wik2', ('xs', i)], writes=[('ps', 4)])
                S.op('act', 'copy', dict(out=ikT[:, Q * 512:(Q + 1) * 512], in_=ps[4][:, :]), writes=[('ps', 4), ('ikT', Q)])

            cvb = [T("cvb%d" % i, [128, 2048], BF16) for i in range(2)]
            wspecs = self.wspecs(l)
            chunks = []
            for (name, dst, src, C, N, col0) in wspecs:
                for c in range(C):
                    for n0 in range(0, N, 2048):
                        n1 = min(N, n0 + 2048)
                        chunks.append((dst[:, c, n0:n1], src[:, c, col0 + n0:col0 + n1], n1 - n0, ('wb', name, c, n0)))
            per_q = (len(chunks) + NQ - 1) // NQ
            per_q += per_q % 2

            def convert_some(Q):
                todo = chunks[Q * per_q:(Q + 1) * per_q]
                for p0 in range(0, len(todo), 2):
                    pair = todo[p0:p0 + 2]
                    for sl, (dst, src, n, key) in enumerate(pair):
                        S.dma(stage[sl][:, 0:n], src, writes=[('stage', sl)])
                    for sl, (dst, src, n, key) in enumerate(pair):
                        S.op('pool', 'tensor_copy', dict(out=cvb[sl][:, 0:n], in_=stage[sl][:, 0:n]), reads=[('stage', sl)], writes=[('cvb', sl)])
                    for sl, (dst, src, n, key) in enumerate(pair):
                        S.dma(dst, cvb[sl][:, 0:n], reads=[('cvb', sl)], writes=[key])

            rcnt = 0
            for Q in range(NQ):
                i = load_xs(Q)
                nk = (Q + 1) * 512
                nkb = 4 * Q + 4
                for pr in range(4):
                    ms = slice(pr * 128, (pr + 1) * 128)
                    for c in range(8):
                        S.op('pe', 'matmul', dict(out=ps[4][:, :], lhsT=wiq[:, c, ms], rhs=xs[i][:, c, :], start=(c == 0), stop=(c == 7)),
                             reads=['wiq', ('xs', i)], writes=[('ps', 4)])
                    S.op('act', 'copy', dict(out=iqT[:, pr, :], in_=ps[4][:, :]), writes=[('ps', 4), ('iqT', pr)])
                for qsub in range(4):
                    for c in range(8):
                        S.op('pe', 'matmul', dict(out=ps[5][:, qsub * 8:(qsub + 1) * 8], lhsT=xs[i][:, c, qsub * 128:(qsub + 1) * 128],
                                                  rhs=wiw[:, c, :], start=(c == 0), stop=(c == 7)),
                             reads=['wiw', ('xs', i)], writes=[('ps', 5)])
                S.op('dve', 'tensor_scalar', dict(out=wtk[:, :, :].rearrange("p a b -> p (a b)"), in0=ps[5][:, 0:32], scalar1=IDX_SCALE,
                                                  scalar2=None, op0=ALU.mult), writes=[('ps', 5), 'wtk'])
                for kc in range(Q + 1):
                    ksl = slice(kc * 512, (kc + 1) * 512)
                    for h in range(8):
                        pr, half = h // 2, h % 2
                        rows = slice(half * 64, half * 64 + 64)
                        for qsub in range(4):
                            rb = rcnt % 4
                            rcnt += 1
                            S.op('pe', 'matmul', dict(out=ps[rb][:, :], lhsT=iqT[rows, pr, qsub * 128:(qsub + 1) * 128], rhs=ikT[rows, ksl],
                                                      start=True, stop=True),
                                 reads=[('ikT', kc), ('iqT', pr)], writes=[('ps', rb)])
                            S.op('act', 'activation', dict(out=rl[rb][:, :], in_=ps[rb][:, :], func=AF.Relu),
                                 writes=[('ps', rb), ('rl', rb)])
                            if h == 0:
                                S.op('dve', 'tensor_scalar', dict(out=sc[:, qsub, ksl], in0=rl[rb][:, :], scalar1=wtk[:, qsub, h:h + 1],
                                                                  scalar2=None, op0=ALU.mult),
                                     reads=[('rl', rb), 'wtk'], writes=[('sc', qsub, kc)])
                            else:
                                S.op('dve', 'scalar_tensor_tensor', dict(out=sc[:, qsub, ksl], in0=rl[rb][:, :], scalar=wtk[:, qsub, h:h + 1],
                                                                         in1=sc[:, qsub, ksl], op0=ALU.mult, op1=ALU.add),
                                     reads=[('rl', rb), 'wtk', ('sc', qsub, kc)], writes=[('sc', qsub, kc)])
                allk = lambda qsub: [('sc', qsub, kc) for kc in range(Q + 1)]
                for qsub in range(4):
                    S.op('dve', 'tensor_reduce', dict(out=lo[:, qsub:qsub + 1], in_=sc[:, qsub, 0:nk], axis=mybir.AxisListType.X, op=ALU.min),
                         reads=allk(qsub), writes=[('lo', qsub)])
                for qsub in range(4):
                    S.op('pool', 'tensor_tensor', dict(out=sc[:, qsub, Q * 512:nk], in0=sc[:, qsub, Q * 512:nk], in1=caus[:, qsub, :], op=ALU.add),
                         reads=[('sc', qsub, Q), 'caus', ('lo', qsub)], writes=[('sc', qsub, Q)])
                for qsub in range(4):
                    S.op('dve', 'tensor_reduce', dict(out=hi[:, qsub:qsub + 1], in_=sc[:, qsub, 0:nk], axis=mybir.AxisListType.X, op=ALU.max),
                         reads=allk(qsub), writes=[('hi', qsub)])
                S.op('dve', 'tensor_tensor', dict(out=step[:, :], in0=hi[:, :], in1=lo[:, :], op=ALU.subtract),
                     reads=[('hi', q) for q in range(4)] + [('lo', q) for q in range(4)], writes=['step'])
                for itn in range(BIS_N):
                    S.op('dve', 'tensor_scalar', dict(out=step[:, :], in0=step[:, :], scalar1=0.5, scalar2=None, op0=ALU.mult),
                         reads=['step'], writes=['step'])
                    S.op('dve', 'tensor_tensor', dict(out=thr[:, :], in0=lo[:, :], in1=step[:, :], op=ALU.add),
                         reads=['step'] + [('lo', q) for q in range(4)], writes=['thr'])
                    for qsub in range(4):
                        S.op('dve', 'tensor_scalar', dict(out=junk[:, 0:nk], in0=sc[:, qsub, 0:nk], scalar1=thr[:, qsub:qsub + 1], scalar2=0.0,
                                                          op0=ALU.is_ge, op1=ALU.add, accum_out=cnt[:, qsub:qsub + 1]),
                             reads=allk(qsub) + ['thr'], writes=['junk', ('cnt', qsub)])
                    S.op('dve', 'tensor_tensor', dict(out=ge[:, :], in0=cnt[:, :], in1=targ[:, 4 * Q:4 * Q + 4], op=ALU.is_ge),
                         reads=[('cnt', q) for q in range(4)] + ['targ'], writes=['ge'])
                    S.op('dve', 'tensor_tensor', dict(out=ge[:, :], in0=ge[:, :], in1=step[:, :], op=ALU.mult),
                         reads=['ge', 'step'], writes=['ge'])
                    S.op('dve', 'tensor_tensor', dict(out=lo[:, :], in0=lo[:, :], in1=ge[:, :], op=ALU.add),
                         reads=['ge'] + [('lo', q) for q in range(4)], writes=[('lo', q) for q in range(4)])
                for qsub in range(4):
                    S.op('dve', 'tensor_scalar', dict(out=mkq[:, qsub, 0:nk], in0=sc[:, qsub, 0:nk], scalar1=lo[:, qsub:qsub + 1], scalar2=None,
                                                      op0=ALU.is_ge),
                         reads=allk(qsub) + [('lo', qsub)], writes=[('mkq', qsub)])
                for kb in range(nkb):
                    pb = kb % 2
                    for qsub in range(4):
                        S.op('pe', 'transpose', dict(out=pstb[pb][:, qsub * 128:(qsub + 1) * 128],
                                                     in_=mkq[:, qsub, kb * 128:(kb + 1) * 128], identity=identb[:, :]),
                             reads=[('mkq', qsub), 'identb'], writes=[('pst', pb)])
                    S.op('act', 'copy', dict(out=mk[:, kb, :], in_=pstb[pb][:, 0:512]), writes=[('pst', pb), ('mk', kb)])
                S.dma(self.mskd[Q, :, 0:nkb, :], mk[:, 0:nkb, :], reads=[('mk', kb) for kb in range(nkb)], writes=[('mskd', Q)])
                convert_some(Q)
            S.end_phase(block)

    def phase_dsa(self, S, l):
        nc, L, NT, NQ = self.nc, self.L, self.NT, self.NQ
        with ExitStack() as es:
            T = lambda name, shape, dt: self.T(es, name, shape, dt)
            wdq = T("wdq", [128, 8, 512], BF16)
            wdk = T("wdk", [128, 8, 512], BF16)
            wdv = T("wdv", [128, 8, 512], BF16)
            stage = [T("stg%d" % i, [128, 2048], F32) for i in range(2)]
            xs = [T("xs%d" % i, [128, 8, 512], BF16) for i in range(2)]
            kT = T("kT", [128, 4, L], BF16)
            Vb = T("Vb", [128, NT, 4, 160], BF16)
            qd = T("qd", [128, 4, 512], BF16)
            mk = T("mk", [128, NT, 512], U8)
            kbias = T("kbias", [128, NT, 8], F32)
            kbd = T("kbd", [128, NT, 8], F32)
            kbq = T("kbq", [128, NT, 8], F32)
            nsl = T("nsl", [128, 8, 128], BF16)
            tpq = T("tpq", [128, 512], BF16)
            PT = [T("pt%d" % i, [128, 512], BF16) for i in range(6)]
            PTm = [T("ptm%d" % i, [128, 512], BF16) for i in range(6)]
            rs = T("rs", [128, 512], F32)
            rs2 = None
            rinv = T("rinv", [128, 512], F32)
            bcs = T("bcs", [128, 512], F32)
            onesf = T("onesf", [128, 128], F32)
            oTq = [T("oTq%d" % i, [128, 4, 512], BF16) for i in range(2)]
            identb = T("identb", [128, 128], BF16)
            trim = T("trim", [128, 128], BF16)
            ps = self.PS(es)
            block = es.enter_context(nc.Block())
            S.dma(stage[1][:, 0:128], self.c_trimask[:, :], writes=[('stage', 1)])
            S.op('dve', 'tensor_copy', dict(out=trim[:, :], in_=stage[1][:, 0:128]), reads=[('stage', 1)], writes=['trim'])
            S.dma(stage[1][:, 128:256], self.c_ident[:, :], reads=['trim'], writes=[('stage', 1)])
            S.op('dve', 'tensor_copy', dict(out=identb[:, :], in_=stage[1][:, 128:256]), reads=[('stage', 1)], writes=['identb'])

            win = self.w_in[l]
            S.dma(kbias[:, :, :], self.c_kbias[:, :, :], writes=['kbias'])
            S.dma(kbd[:, :, :], self.c_kbd[:, :, :], writes=['kbd'])
            S.dma(stage[0][:, 0:1024], self.c_negslope[:, :, :].rearrange("p a b -> p (a b)"), writes=[('stage', 0)])
            S.op('dve', 'tensor_copy', dict(out=nsl[:, :, :].rearrange("p a b -> p (a b)"), in_=stage[0][:, 0:1024]),
                 reads=[('stage', 0)], writes=['nsl'])
            S.op('pool', 'memset', dict(ap=onesf[:, :], constant=1.0), writes=['onesf'])
            S.op('pool', 'memset', dict(ap=Vb[:, :, :, :], constant=0.0), writes=['Vb0'])
            S.op('pool', 'memset', dict(ap=Vb[:, :, :, 64:65], constant=1.0), reads=['Vb0'], writes=['Vb1'])
            self.load_w(S, wdq, win, 8, 512, stage, 'wdq', col0=1544)
            self.load_w(S, wdk, win, 8, 512, stage, 'wdk', col0=2056)
            self.load_w(S, wdv, win, 8, 512, stage, 'wdv', col0=2568)
            xcnt = [0]

            def load_xs(Q):
                i = xcnt[0] % 2
                xcnt[0] += 1
                S.dma(xs[i][:, :, :], self.xbf[:, :, Q * 512:(Q + 1) * 512], reads=[('xbf', Q)], writes=[('xs', i)])
                return i

            for Q in range(NQ):
                i = load_xs(Q)
                qs = slice(Q * 512, (Q + 1) * 512)
                for pr in range(4):
                    ms = slice(pr * 128, (pr + 1) * 128)
                    for c in range(8):
                        S.op('pe', 'matmul', dict(out=ps[6][:, :], lhsT=wdk[:, c, ms], rhs=xs[i][:, c, :], start=(c == 0), stop=(c == 7)),
                             reads=['wdk', ('xs', i)], writes=[('ps', 6)])
                    S.op('dve', 'tensor_copy', dict(out=kT[:, pr, qs], in_=ps[6][:, :]), writes=[('ps', 6), ('kT', Q)])
                for tt in range(4):
                    tile = Q * 4 + tt
                    for c in range(8):
                        S.op('pe', 'matmul', dict(out=ps[7][:, :], lhsT=xs[i][:, c, tt * 128:(tt + 1) * 128], rhs=wdv[:, c, :],
                                                  start=(c == 0), stop=(c == 7)),
                             reads=['wdv', ('xs', i)], writes=[('ps', 7)])
                    src = ps[7][:, :].rearrange("p (a b d) -> p a b d", a=4, b=2)
                    S.op('act', 'copy', dict(out=Vb[:, tile, :, 0:64], in_=src[:, :, 0, :]), reads=['Vb1'], writes=[('ps', 7), ('V', tile, 0)])
                    S.op('dve', 'tensor_copy', dict(out=Vb[:, tile, :, 96:160], in_=src[:, :, 1, :]), reads=['Vb1'],
                         writes=[('ps', 7), ('V', tile, 1)])

            for Q in range(NQ):
                i = load_xs(Q)
                nkb = 4 * Q + 4
                qs = slice(Q * 512, (Q + 1) * 512)
                S.dma(mk[:, 0:nkb, :], self.mskd[Q, :, 0:nkb, :], reads=[('mskd', Q)], writes=['mk'])
                S.dma(stage[1][:, 0:512], self.c_tpos[:, qs], writes=[('stage', 1)])
                S.op('dve', 'tensor_copy', dict(out=tpq[:, :], in_=stage[1][:, 0:512]), reads=[('stage', 1)], writes=['tpq'])
                for pr in range(4):
                    ms = slice(pr * 128, (pr + 1) * 128)
                    for c in range(8):
                        S.op('pe', 'matmul', dict(out=ps[6][:, :], lhsT=wdq[:, c, ms], rhs=xs[i][:, c, :], start=(c == 0), stop=(c == 7)),
                             reads=['wdq', ('xs', i)], writes=[('ps', 6)])
                    S.op('dve', 'tensor_scalar', dict(out=qd[:, pr, :], in0=ps[6][:, :], scalar1=0.125, scalar2=None, op0=ALU.mult),
                         writes=[('ps', 6), ('qd', pr)])
                S.op('dve', 'scalar_tensor_tensor', dict(out=kbq[:, :, :].rearrange("p a b -> p (a b)"),
                                                         in0=kbd[:, :, :].rearrange("p a b -> p (a b)"), scalar=float(Q * 512 + 511),
                                                         in1=kbias[:, :, :].rearrange("p a b -> p (a b)"), op0=ALU.mult, op1=ALU.add),
                     reads=['kbias', 'kbd'], writes=['kbq'])
                items = []
                oq = oTq[Q % 2]
                for h in range(8):
                    half, pr, odd = h % 2, h // 2, (h % 2 == 1)
                    rows = slice(half * 64, half * 64 + 64)
                    r2 = slice(half * 64, half * 64 + 2)
                    ob = 3 + h % 2
                    nA, nB = self.make_norm(S, ps, ob, odd, rs, rs2, rinv, bcs, onesf,
                                            lambda p0, oq=oq, pr=pr: oq[p0:p0 + 64, pr, :], ('oTq', Q % 2, h))
                    for kb in range(nkb):
                        j = kb - 4 * Q
                        n0 = 128 * j if j > 0 else 0
                        ks = slice(kb * 128, (kb + 1) * 128)
                        qk = [(kT[rows, pr, ks], qd[rows, pr, n0:512], n0, 512, [('kT', kb // 4), ('qd', pr)])]
                        if h < 2:
                            qk.append((nsl[rows, h, :], tpq[rows, n0:512], n0, 512, ['nsl', 'tpq']))
                        if j >= 0:
                            qk.append((identb[:, :], trim[:, :], n0, n0 + 128, ['identb', 'trim']))
                        lh = Vb[:, kb, pr, 32:160] if odd else Vb[:, kb, pr, 0:128]
                        items.append(dict(qk=qk, n0=n0, bias=kbq[:, kb, h:h + 1], bias_keys=['kbq'],
                                          mask=(mk[:, kb, n0:512], 'mk'),
                                          pv_lhsT=lh, M=128, obank=ob, first=(kb == 0), last=(kb == nkb - 1),
                                          v_keys=[('V', kb, 0), ('V', kb, 1), 'Vb1'], normA=nA, normB=nB))
                self.emit_attention(S, ps, PT, PTm, items)
                S.dma(self.ob[:, :, qs], oq[:, :, :], reads=[('oTq', Q % 2, h) for h in range(8)], writes=[('ob', Q)])
            S.end_phase(block)

    def ln_block(self, S, ps, xr, sq, mean, lnv, rstd, lnp, gi, onesf, epst, outb, xkey):
        for m in range(8):
            S.op('pe', 'matmul', dict(out=ps[5][:, :], lhsT=onesf[:, :], rhs=xr[:, m, :], start=(m == 0), stop=(m == 7)),
                 reads=['onesf', (xkey, m)], writes=[('ps', 5)])
            S.op('act', 'activation', dict(out=sq[m % 2][:, :], in_=xr[:, m, :], func=AF.Square), reads=[(xkey, m)], writes=[('sq', m % 2)])
            S.op('pe', 'matmul', dict(out=ps[6][:, :], lhsT=onesf[:, :], rhs=sq[m % 2][:, :], start=(m == 0), stop=(m == 7)),
                 reads=['onesf', ('sq', m % 2)], writes=[('ps', 6)])
        S.op('dve', 'tensor_scalar', dict(out=mean[:, :], in0=ps[5][:, :], scalar1=1.0 / D, scalar2=None, op0=ALU.mult),
             writes=[('ps', 5), 'mean'])
        S.op('dve', 'tensor_tensor', dict(out=lnv[:, :], in0=mean[:, :], in1=mean[:, :], op=ALU.mult), reads=['mean'], writes=['lnv'])
        S.op('dve', 'scalar_tensor_tensor', dict(out=lnv[:, :], in0=ps[6][:, :], scalar=1.0 / D, in1=lnv[:, :], op0=ALU.mult, op1=ALU.subtract),
             reads=['lnv'], writes=[('ps', 6), 'lnv'])
        S.op('act', 'activation', dict(out=lnv[:, :], in_=lnv[:, :], func=AF.Ln, bias=epst[:, 0:1], scale=1.0), reads=['lnv', 'epst'], writes=['lnv'])
        S.op('act', 'activation', dict(out=rstd[:, :], in_=lnv[:, :], func=AF.Exp, scale=-0.5), reads=['lnv'], writes=['rstd'])
        for m in range(8):
            S.op('dve', 'tensor_tensor', dict(out=xr[:, m, :], in0=xr[:, m, :], in1=mean[:, :], op=ALU.subtract),
                 reads=[(xkey, m), 'mean'], writes=[(xkey, m)])
            S.op('dve', 'tensor_tensor', dict(out=xr[:, m, :], in0=xr[:, m, :], in1=rstd[:, :], op=ALU.mult),
                 reads=[(xkey, m), 'rstd'], writes=[(xkey, m)])
            S.op('dve', 'tensor_scalar', dict(out=xr[:, m, :], in0=xr[:, m, :], scalar1=lnp[:, gi, m:m + 1], scalar2=lnp[:, gi + 1, m:m + 1],
                                              op0=ALU.mult, op1=ALU.add),
                 reads=[(xkey, m), 'lnp'], writes=[(xkey, m)])
            if outb is not None:
                S.op('pool', 'tensor_copy', dict(out=outb[:, m, :], in_=xr[:, m, :]), reads=[(xkey, m)], writes=[('xob', m)])

    def phase_merge(self, S, l):
        nc, L, NT, NQ = self.nc, self.L, self.NT, self.NQ
        with ExitStack() as es:
            T = lambda name, shape, dt: self.T(es, name, shape, dt)
            wA = T("wA", [128, 4, D], BF16)
            wB = T("wB", [128, 4, D], BF16)
            wO = T("wO", [128, 8, D], BF16)
            wga = T("wga", [128, 8, D], BF16)
            wgb = T("wgb", [128, 8, D], BF16)
            stage = [T("stg%d" % i, [128, 2048], F32) for i in range(2)]
            lnp = T("lnp", [128, 4, 8], F32)
            oab = T("oab", [128, 4, 512], BF16)
            obb = T("obb", [128, 4, 512], BF16)
            xs = T("xs", [128, 8, 512], BF16)
            xr = T("xr", [128, 8, 512], F32)
            mg = T("mg", [128, 8, 512], BF16)
            sa = T("sa", [128, 512], F32)
            sb = T("sb", [128, 512], F32)
            sq = [T("sq%d" % i, [128, 512], F32) for i in range(2)]
            mean = T("mean", [128, 512], F32)
            lnv = T("lnv", [128, 512], F32)
            rstd = T("rstd", [128, 512], F32)
            x1b = T("x1b", [128, 8, 512], BF16)
            onesf = T("onesf", [128, 128], F32)
            epst = T("epst", [128, 1], F32)
            ps = self.PS(es)
            block = es.enter_context(nc.Block())
            S.op('pool', 'memset', dict(ap=onesf[:, :], constant=1.0), writes=['onesf'])
            S.op('pool', 'memset', dict(ap=epst[:, :], constant=LN_EPS), writes=['epst'])
            S.dma(lnp[:, :, :], self.lnp[l], writes=['lnp'])
            self.load_wb(S, wga, self.wb_ga, 8, D, 'ga', 'wga')
            self.load_wb(S, wgb, self.wb_gb, 8, D, 'gb', 'wgb')
            self.load_wb(S, wA, self.wb_a, 4, D, 'a', 'wA')
            self.load_wb(S, wB, self.wb_b, 4, D, 'b', 'wB')
            self.load_wb(S, wO, self.wb_o, 8, D, 'o', 'wO')
            for Q in range(NQ):
                qs = slice(Q * 512, (Q + 1) * 512)
                S.dma(oab[:, :, :], self.oa[:, :, qs], reads=[('oa', p) for p in range(4)], writes=['oab'])
                S.dma(obb[:, :, :], self.ob[:, :, qs], reads=[('ob', Q)], writes=['obb'])
                S.dma(xs[:, :, :], self.xbf[:, :, qs], reads=[('xbf', Q)], writes=['xs'])
                S.dma(xr[:, :, :], self.xres[:, :, qs], reads=[('xres', Q)], writes=[('xr', m) for m in range(8)])
                for m in range(8):
                    ms = slice(m * 128, (m + 1) * 128)
                    for c in range(4):
                        S.op('pe', 'matmul', dict(out=ps[0][:, :], lhsT=wA[:, c, ms], rhs=oab[:, c, :], start=(c == 0), stop=(c == 3)),
                             reads=['wA', 'oab'], writes=[('ps', 0)])
                    for c in range(4):
                        S.op('pe', 'matmul', dict(out=ps[1][:, :], lhsT=wB[:, c, ms], rhs=obb[:, c, :], start=(c == 0), stop=(c == 3)),
                             reads=['wB', 'obb'], writes=[('ps', 1)])
                    for c in range(8):
                        S.op('pe', 'matmul', dict(out=ps[2][:, :], lhsT=wga[:, c, ms], rhs=xs[:, c, :], start=(c == 0), stop=(c == 7)),
                             reads=['wga', 'xs'], writes=[('ps', 2)])
                    for c in range(8):
                        S.op('pe', 'matmul', dict(out=ps[3][:, :], lhsT=wgb[:, c, ms], rhs=xs[:, c, :], start=(c == 0), stop=(c == 7)),
                             reads=['wgb', 'xs'], writes=[('ps', 3)])
                    S.op('act', 'activation', dict(out=sa[:, :], in_=ps[2][:, :], func=AF.Sigmoid), writes=[('ps', 2), 'sa'])
                    S.op('act', 'activation', dict(out=sb[:, :], in_=ps[3][:, :], func=AF.Sigmoid), writes=[('ps', 3), 'sb'])
                    S.op('dve', 'tensor_tensor', dict(out=sa[:, :], in0=sa[:, :], in1=ps[0][:, :], op=ALU.mult), reads=['sa'], writes=[('ps', 0), 'sa'])
                    S.op('dve', 'tensor_tensor', dict(out=sb[:, :], in0=sb[:, :], in1=ps[1][:, :], op=ALU.mult), reads=['sb'], writes=[('ps', 1), 'sb'])
                    S.op('pool', 'tensor_tensor', dict(out=mg[:, m, :], in0=sa[:, :], in1=sb[:, :], op=ALU.add), reads=['sa', 'sb'], writes=[('mg', m)])
                for m in range(8):
                    ms = slice(m * 128, (m + 1) * 128)
                    for c in range(8):
                        S.op('pe', 'matmul', dict(out=ps[4][:, :], lhsT=wO[:, c, ms], rhs=mg[:, c, :], start=(c == 0), stop=(c == 7)),
                             reads=['wO', ('mg', c)], writes=[('ps', 4)])
                    S.op('dve', 'scalar_tensor_tensor', dict(out=xr[:, m, :], in0=xr[:, m, :], scalar=ALPHA, in1=ps[4][:, :], op0=ALU.mult, op1=ALU.add),
                         reads=[('xr', m)], writes=[('ps', 4), ('xr', m)])
                self.ln_block(S, ps, xr, sq, mean, lnv, rstd, lnp, 0, onesf, epst, x1b, 'xr')
                S.dma(self.xres[:, :, qs], xr[:, :, :], reads=[('xr', m) for m in range(8)], writes=[('xres', Q)])
                S.dma(self.xbf[:, :, qs], x1b[:, :, :], reads=[('xob', m) for m in range(8)], writes=[('xbf', Q)])
            S.end_phase(block)

    def phase_ffn(self, S, l, last):
        nc, L, NT, NQ = self.nc, self.L, self.NT, self.NQ
        NJ = DFF // 128
        with ExitStack() as es:
            T = lambda name, shape, dt: self.T(es, name, shape, dt)
            w1 = T("w1", [128, 8, 2 * DFF], BF16)
            w2 = T("w2", [128, NJ, D], BF16)
            stage = [T("stg%d" % i, [128, 512], F32) for i in range(2)]
            lnp = T("lnp", [128, 4, 8], F32)
            xs = T("xs", [128, 8, 512], BF16)
            xr = T("xr", [128, 8, 512], F32)
            hT = T("hT", [128, NJ, 512], BF16)
            sq = [T("sq%d" % i, [128, 512], F32) for i in range(2)]
            mean = T("mean", [128, 512], F32)
            lnv = T("lnv", [128, 512], F32)
            rstd = T("rstd", [128, 512], F32)
            if last:
                outst = [T("outst%d" % i, [128, 512], F32) for i in range(4)]
                identf = T("identf", [128, 128], F32)
                x2b = None
            else:
                x2b = T("x2b", [128, 8, 512], BF16)
            onesf = T("onesf", [128, 128], F32)
            epst = T("epst", [128, 1], F32)
            ps = self.PS(es)
            block = es.enter_context(nc.Block())
            S.op('pool', 'memset', dict(ap=onesf[:, :], constant=1.0), writes=['onesf'])
            S.op('pool', 'memset', dict(ap=epst[:, :], constant=LN_EPS), writes=['epst'])
            S.dma(lnp[:, :, :], self.lnp[l], writes=['lnp'])
            if last:
                S.dma(identf[:, :], self.c_ident[:, :], writes=['identf'])
            self.load_wb(S, w1, self.wb_f1, 8, 2 * DFF, 'f1', 'w1')
            self.load_wb(S, w2, self.wb_f2, NJ, D, 'f2', 'w2')
            ocnt = 0
            for Q in range(NQ):
                qs = slice(Q * 512, (Q + 1) * 512)
                S.dma(xs[:, :, :], self.xbf[:, :, qs], reads=[('xbf', Q)], writes=['xs'])
                S.dma(xr[:, :, :], self.xres[:, :, qs], reads=[('xres', Q)], writes=[('xr', m) for m in range(8)])
                for j in range(NJ):
                    bg, bu = 2 * (j % 2), 2 * (j % 2) + 1
                    for c in range(8):
                        S.op('pe', 'matmul', dict(out=ps[bg][:, :], lhsT=w1[:, c, j * 128:(j + 1) * 128], rhs=xs[:, c, :], start=(c == 0), stop=(c == 7)),
                             reads=['w1', 'xs'], writes=[('ps', bg)])
                    for c in range(8):
                        S.op('pe', 'matmul', dict(out=ps[bu][:, :], lhsT=w1[:, c, DFF + j * 128:DFF + (j + 1) * 128], rhs=xs[:, c, :],
                                                  start=(c == 0), stop=(c == 7)),
                             reads=['w1', 'xs'], writes=[('ps', bu)])
                    S.op('act', 'activation', dict(out=sq[j % 2][:, :], in_=ps[bg][:, :], func=AF.Silu), writes=[('ps', bg), ('sq', j % 2)])
                    S.op('dve', 'tensor_tensor', dict(out=hT[:, j, :], in0=sq[j % 2][:, :], in1=ps[bu][:, :], op=ALU.mult),
                         reads=[('sq', j % 2)], writes=[('ps', bu), ('hT', j)])
                for m in range(8):
                    ms = slice(m * 128, (m + 1) * 128)
                    b = 4 if m % 2 == 0 else 7
                    for j in range(NJ):
                        S.op('pe', 'matmul', dict(out=ps[b][:, :], lhsT=w2[:, j, ms], rhs=hT[:, j, :], start=(j == 0), stop=(j == NJ - 1)),
                             reads=['w2', ('hT', j)], writes=[('ps', b)])
                    S.op('dve', 'scalar_tensor_tensor', dict(out=xr[:, m, :], in0=xr[:, m, :], scalar=ALPHA, in1=ps[b][:, :], op0=ALU.mult, op1=ALU.add),
                         reads=[('xr', m)], writes=[('ps', b), ('xr', m)])
                self.ln_block(S, ps, xr, sq, mean, lnv, rstd, lnp, 2, onesf, epst, x2b, 'xr')
                if not last:
                    S.dma(self.xres[:, :, qs], xr[:, :, :], reads=[('xr', m) for m in range(8)], writes=[('xres', Q)])
                    S.dma(self.xbf[:, :, qs], x2b[:, :, :], reads=[('xob', m) for m in range(8)], writes=[('xbf', Q)])
                else:
                    for tt in range(4):
                        tile = Q * 4 + tt
                        for half in range(2):
                            b = 0 + (ocnt % 4)
                            oi = ocnt % 4
                            ocnt += 1
                            for cc in range(4):
                                c = half * 4 + cc
                                S.op('pe', 'transpose', dict(out=ps[b][:, cc * 128:(cc + 1) * 128], in_=xr[:, c, tt * 128:(tt + 1) * 128],
                                                             identity=identf[:, :]),
                                     reads=[('xr', c), 'identf'], writes=[('ps', b)])
                            S.op('act', 'copy', dict(out=outst[oi][:, :], in_=ps[b][:, :]), writes=[('ps', b), ('outst', oi)])
                            S.dma(self.out[tile * 128:(tile + 1) * 128, half * 512:(half + 1) * 512], outst[oi][:, :],
                                  reads=[('outst', oi)], writes=[('out', tile, half)])
            S.end_phase(block)

    def build(self):
        nc = self.nc
        self._ldi = 0
        with ExitStack() as gs:
            S = Sched(nc, gs)
            self.S = S
            self.phase0(S)
            stop = self.dbg
            for l in range(self.nlayers):
                last = (l == self.nlayers - 1)
                if ('stop_p0' in stop):
                    break
                self.phase_fox(S, l)
                if ('stop_fox' in stop):
                    break
                self.phase_idx(S, l)
                if ('stop_idx' in stop):
                    break
                self.phase_dsa(S, l)
                if ('stop_dsa' in stop):
                    break
                self.phase_merge(S, l)
                if ('stop_merge' in stop):
                    break
                self.phase_ffn(S, l, last)
        return nc


def host_consts(L):
    NT = L // 128
    p = np.arange(128)[:, None]
    f = np.arange(128)[None, :]
    c = {}
    c["c_ident"] = np.eye(128, dtype=np.float32)
    c["c_trimask"] = np.where(p > f, -30000.0, 0.0).astype(np.float32)
    cm = np.zeros((128, 4, 512), np.float32)
    f5 = np.arange(512)[None, :]
    for j in range(4):
        cm[:, j, :] = np.where(f5 > 128 * j + p, -1e30, 0.0)
    c["c_causT"] = cm
    tq = np.arange(NT)[None, :] * 128 + p
    c["c_targ"] = np.minimum(min(TOPK, L // 4), tq + 1).astype(np.float32)
    slopes = np.exp2(-8.0 * (np.arange(8, dtype=np.float32) + 1.0) / 8).astype(np.float32)
    kb = np.zeros((128, NT, 8), np.float32)
    for t in range(NT):
        kb[:, t, :] = (t * 128 + np.arange(128))[:, None] * slopes[None, :] + ALIBI_C
    c["c_kbias"] = kb
    kd = np.zeros((128, NT, 8), np.float32)
    kd[:, :, 2:] = -slopes[None, None, 2:]
    c["c_kbd"] = kd
    ns = np.zeros((128, 8, 128), np.float32)
    for base in (0, 1, 64, 65):
        ns[base, :, :] = -slopes[:, None]
    c["c_negslope"] = ns
    tp = np.zeros((128, L), np.float32)
    t = np.arange(L)
    for base in (0, 64):
        tp[base, :] = (t // 16) * 16
        tp[base + 1, :] = t % 16
    c["c_tpos"] = tp
    return c


def host_weights(w_in, b_forget, w_branch_a, w_branch_b, w_out, ln1_g, ln1_b, w_ffn_in, w_ffn_out, ln2_g, ln2_b):
    def fm(w):
        k = w.shape[1]
        return np.ascontiguousarray(w.reshape(2, k // 128, 128, w.shape[2]).transpose(0, 2, 1, 3))
    m = {}
    m["w_in"] = fm(w_in)
    iw = w_in[:, :, 3656:3664]
    m["w_iwrep"] = fm(np.repeat(iw, 64, axis=2))
    m["bfg"] = np.ascontiguousarray(b_forget.reshape(2, 8, 1))
    m["w_a"] = fm(w_branch_a)
    m["w_b"] = fm(w_branch_b)
    m["w_o"] = fm(w_out)
    lp = np.stack([ln1_g, ln1_b, ln2_g, ln2_b], axis=1)
    m["lnp"] = np.ascontiguousarray(lp.reshape(2, 4, 8, 128).transpose(0, 3, 1, 2))
    m["w_f1"] = fm(w_ffn_in)
    m["w_f2"] = fm(w_ffn_out)
    return m


_CACHE = {}


def kernel(x, w_in, b_forget, w_branch_a, w_branch_b, w_out, ln1_g, ln1_b,
           w_ffn_in, w_ffn_out, ln2_g, ln2_b):
    x = np.asarray(x, np.float32)
    B, L, _ = x.shape
    if L not in _CACHE:
        _CACHE[L] = Builder(L).build()
    nc = _CACHE[L]
    shared = host_consts(L)
    shared.update(host_weights(*[np.asarray(a, np.float32) for a in
                                 (w_in, b_forget, w_branch_a, w_branch_b, w_out, ln1_g, ln1_b,
                                  w_ffn_in, w_ffn_out, ln2_g, ln2_b)]))
    in_maps = []
    for b in range(B):
        m = dict(shared)
        m["x"] = np.ascontiguousarray(x[b])
        in_maps.append(m)
    res = run_bass_kernel_spmd(nc, in_maps, core_ids=list(range(B)))
    return np.stack([np.asarray(r["out"], np.float32) for r in res.results], axis=0)
```

```python
import math
from contextlib import ExitStack
import numpy as np
import concourse.bass as bass
import concourse.mybir as mybir
from concourse.bass_utils import run_bass_kernel_spmd

F32 = mybir.dt.float32
BF16 = mybir.dt.bfloat16
U8 = mybir.dt.uint8
ALU = mybir.AluOpType
AF = mybir.ActivationFunctionType

D = 1024
DFF = 2816
ALPHA = 4.0 ** 0.25
LN_EPS = 1e-5
IDX_SCALE = (8 ** -0.5) * (64 ** -0.5)
TOPK = 256
BIS_B = 64.0
BIS_N = 14
ALIBI_C = 50.0


class Sched:
    def __init__(self, nc, stack):
        self.nc = nc
        self.names = ['sp', 'act', 'dve', 'pool', 'pe']
        self.lists = {k: [] for k in self.names}
        self.sem = {k: stack.enter_context(nc.semaphore("s_" + k)) for k in ['pe', 'act', 'dve', 'pool']}
        self.cnt = {k: 0 for k in self.sem}
        self.ndma = 12
        self.dsem = {q: [stack.enter_context(nc.semaphore("d_%s%d" % (q, i))) for i in range(self.ndma)]
                     for q in ['sp', 'pool', 'act']}
        self.dcnt = {q: 0 for q in self.dsem}
        self.waited = {k: {} for k in self.names}
        self.lastw = {}
        self.readers = {}
        self.ninstr = 0

    def _wait(self, eng, tok):
        semid, sem, val = tok[0], tok[1], tok[2]
        w = self.waited[eng]
        if w.get(semid, 0) >= val:
            return
        w[semid] = val
        self.lists[eng].append(('w', sem, val))

    def _deps(self, eng, reads, writes):
        for r in reads:
            t = self.lastw.get(r)
            if t is not None:
                if not (t[3] == eng and eng == 'pe'):
                    self._wait(eng, t)
        for wk in writes:
            t = self.lastw.get(wk)
            if t is not None and (t[3] != eng or eng != 'pe'):
                self._wait(eng, t)
            rd = self.readers.get(wk)
            if rd:
                for t in rd.values():
                    if t[3] != eng:
                        self._wait(eng, t)

    def _commit(self, tok, reads, writes):
        for wk in writes:
            self.lastw[wk] = tok
            self.readers[wk] = {}
        for r in reads:
            self.readers.setdefault(r, {})[tok[0]] = tok

    def op(self, eng, meth, kw, reads=(), writes=()):
        fn = lambda e: getattr(e, meth)(**kw)
        self._deps(eng, reads, writes)
        self.cnt[eng] += 1
        tok = (eng, self.sem[eng], self.cnt[eng], eng)
        self.lists[eng].append(('o', fn, self.sem[eng], 1))
        self._commit(tok, reads, writes)
        self.ninstr += 1
        return tok

    def dma(self, out_ap, in_ap, reads=(), writes=(), q='sp'):
        self._deps(q, reads, writes)
        i = self.dcnt[q]
        self.dcnt[q] += 1
        slot, rnd = i % self.ndma, i // self.ndma
        sem = self.dsem[q][slot]
        semid = ('d', q, slot)
        if rnd > 0:
            self._wait(q, (semid, sem, 16 * rnd))
        tok = (semid, sem, 16 * (rnd + 1), 'dma')
        self.lists[q].append(('o', lambda e: e.dma_start(out=out_ap, in_=in_ap), sem, 16))
        self._commit(tok, reads, writes)
        self.ninstr += 1
        return tok

    def end_phase(self, block):
        for q in self.dsem:
            n = self.dcnt[q]
            for slot in range(min(n, self.ndma)):
                last = ((n - 1 - slot) // self.ndma) + 1
                self._wait(q, (('d', q, slot), self.dsem[q][slot], 16 * last))
        decos = {'sp': block.sync, 'act': block.scalar, 'dve': block.vector,
                 'pool': block.gpsimd, 'pe': block.tensor}
        for name in self.names:
            lst = self.lists[name]
            self.lists[name] = []

            def body(e, lst=lst):
                for it in lst:
                    if it[0] == 'w':
                        e.wait_ge(it[1], it[2])
                    else:
                        it[1](e).then_inc(it[2], it[3])
            decos[name](body)


class Builder:
    def __init__(self, L, nlayers=2, dbg=()):
        self.L = L
        self.NT = L // 128
        self.NQ = L // 512
        self.nlayers = nlayers
        self.dbg = dbg
        self.nc = bass.Bass("TRN2", target_bir_lowering=False)
        nc = self.nc
        self.inputs = {}

        NT, NQ = self.NT, self.NQ
        self.shapes = {
            "x": ([L, D], F32), "w_in": ([2, 128, 8, 5712], F32), "w_iwrep": ([2, 128, 8, 512], F32),
            "bfg": ([2, 8, 1], F32), "w_a": ([2, 128, 4, D], F32), "w_b": ([2, 128, 4, D], F32),
            "w_o": ([2, 128, 8, D], F32), "lnp": ([2, 128, 4, 8], F32),
            "w_f1": ([2, 128, 8, 2 * DFF], F32), "w_f2": ([2, 128, 22, D], F32),
            "c_ident": ([128, 128], F32), "c_trimask": ([128, 128], F32),
            "c_causT": ([128, 4, 512], F32), "c_targ": ([128, NT], F32),
            "c_kbias": ([128, NT, 8], F32), "c_kbd": ([128, NT, 8], F32), "c_negslope": ([128, 8, 128], F32), "c_tpos": ([128, L], F32),
        }
        self.scr_shapes = {
            "out": ([L, D], F32),
            "xres": ([128, 8, L], F32), "xbf": ([128, 8, L], BF16), "oa": ([128, 4, L], BF16),
            "ob": ([128, 4, L], BF16), "cspl": ([8, 3, L], BF16), "mskd": ([NQ, 128, NT, 512], U8),
            "wb_a": ([128, 4, D], BF16), "wb_b": ([128, 4, D], BF16), "wb_o": ([128, 8, D], BF16),
            "wb_ga": ([128, 8, D], BF16), "wb_gb": ([128, 8, D], BF16),
            "wb_f1": ([128, 8, 2 * DFF], BF16), "wb_f2": ([128, DFF // 128, D], BF16),
        }
        self._aps = {}

    def __getattr__(self, name):
        d = self.__dict__
        if 'shapes' in d and name in d['shapes']:
            if name not in d['_aps']:
                shp, dt = d['shapes'][name]
                d['_aps'][name] = d['nc'].dram_tensor(name, list(shp), dt, kind="ExternalInput").ap()
                d['inputs'][name] = (tuple(shp), dt)
            return d['_aps'][name]
        if 'scr_shapes' in d and name in d['scr_shapes']:
            if name not in d['_aps']:
                shp, dt = d['scr_shapes'][name]
                kind = "ExternalOutput" if (name in d['dbg'] or name == "out") else "Internal"
                d['_aps'][name] = d['nc'].dram_tensor(name, list(shp), dt, kind=kind).ap()
            return d['_aps'][name]
        raise AttributeError(name)

    def T(self, es, name, shape, dt):
        self._uid = getattr(self, '_uid', 0) + 1
        return es.enter_context(self.nc.sbuf_tensor("%s_%d" % (name, self._uid), list(shape), dt))

    def PS(self, es, n=8):
        self._uid = getattr(self, '_uid', 0) + 1
        return [es.enter_context(self.nc.psum_tensor("ps%d_%d" % (i, self._uid), [128, 512], F32)) for i in range(n)]

    def wspecs(self, l):
        NJ = DFF // 128
        return [("a", self.wb_a, self.w_a[l], 4, D, 0), ("b", self.wb_b, self.w_b[l], 4, D, 0),
                ("o", self.wb_o, self.w_o[l], 8, D, 0), ("ga", self.wb_ga, self.w_in[l], 8, D, 3664),
                ("gb", self.wb_gb, self.w_in[l], 8, D, 4688), ("f1", self.wb_f1, self.w_f1[l], 8, 2 * DFF, 0),
                ("f2", self.wb_f2, self.w_f2[l], NJ, D, 0)]

    def load_wb(self, S, dst, src, C, N, name, tag):
        for c in range(C):
            S.dma(dst[:, c, :], src[:, c, :], reads=[('wb', name, c, n0) for n0 in range(0, N, 2048)], writes=[tag])

    def load_w(self, S, dst, src, C, N, stage, tag, col0=0):
        engs = ['dve', 'pool', 'act']
        step = stage[0].shape[1]
        for c in range(C):
            for n0 in range(0, N, step):
                n1 = min(N, n0 + step)
                i = self._ldi
                self._ldi += 1
                sl = i % 2
                st = stage[sl]
                S.dma(st[:, 0:n1 - n0], src[:, c, col0 + n0:col0 + n1], reads=[tag + 'src'], writes=[('stage', sl)])
                e = engs[i % 3]
                d_ap = dst[:, c, n0:n1]
                s_ap = st[:, 0:n1 - n0]
                S.op(e, 'copy' if e == 'act' else 'tensor_copy', dict(out=d_ap, in_=s_ap),
                     reads=[('stage', sl)], writes=[tag])

    def phase0(self, S):
        nc, L, NT, NQ = self.nc, self.L, self.NT, self.NQ
        with ExitStack() as es:
            ident = self.T(es, "ident", [128, 128], F32)
            xin = [self.T(es, "xin%d" % i, [128, D], F32) for i in range(2)]
            xTf = [self.T(es, "xTf%d" % i, [128, 8, 512], F32) for i in range(2)]
            xTb = [self.T(es, "xTb%d" % i, [128, 8, 512], BF16) for i in range(2)]
            import os
            NB = int(os.environ.get('P0_NB', '8'))
            ps = self.PS(es, NB)
            block = es.enter_context(nc.Block())
            S.dma(ident[:, :], self.c_ident[:, :], writes=['ident'])
            for Q in range(NQ):
                s = Q % 2
                for tt in range(4):
                    tile = Q * 4 + tt
                    sl = tile % 2
                    S.dma(xin[sl][:, :], self.x[tile * 128:(tile + 1) * 128, :], writes=[('xin', sl)])
                    for half in range(2):
                        b = (tile * 2 + half) % NB
                        for cc in range(4):
                            c = half * 4 + cc
                            o_ap = ps[b][:, cc * 128:(cc + 1) * 128]
                            i_ap = xin[sl][:, c * 128:(c + 1) * 128]
                            S.op('pe', 'transpose', dict(out=o_ap, in_=i_ap, identity=ident[:, :]),
                                 reads=[('xin', sl), 'ident'], writes=[('ps', b)])
                        src = ps[b][:, :].rearrange("p (a b) -> p a b", a=4)
                        d1 = xTf[s][:, half * 4:(half + 1) * 4, tt * 128:(tt + 1) * 128]
                        d2 = xTb[s][:, half * 4:(half + 1) * 4, tt * 128:(tt + 1) * 128]
                        MODE = os.environ.get('P0_MODE', 'ad')
                        if 'a' in MODE:
                            S.op('act', 'copy', dict(out=d1, in_=src),
                                 writes=[('ps', b), ('xTf', s, tt, half)])
                        if 'd' in MODE:
                            S.op('dve', 'tensor_copy', dict(out=d2, in_=src),
                                 writes=[('ps', b), ('xTb', s, tt, half)])
                rk = [('xTf', s, tt, h) for tt in range(4) for h in range(2)]
                S.dma(self.xres[:, :, Q * 512:(Q + 1) * 512], xTf[s][:, :, :], reads=rk, writes=[('xres', Q)])
                rk = [('xTb', s, tt, h) for tt in range(4) for h in range(2)]
                S.dma(self.xbf[:, :, Q * 512:(Q + 1) * 512], xTb[s][:, :, :], reads=rk, writes=[('xbf', Q)])
            S.end_phase(block)

    def emit_attention(self, S, ps, PT, PTm, items, Dp=4, sbanks=(0, 1, 2, 6, 7)):
        n = len(items)
        nS = len(sbanks)
        nP = len(PT)
        deferred = []
        for i in range(n + Dp):
            if i < n:
                it = items[i]
                sb = sbanks[i % nS]
                n0 = it['n0']
                nq = len(it['qk'])
                for j, (lh, rh, c0, c1, rk) in enumerate(it['qk']):
                    o_ap = ps[sb][:, c0:c1]
                    S.op('pe', 'matmul', dict(
                        out=o_ap, lhsT=lh, rhs=rh, start=(j == 0), stop=(j == nq - 1)),
                        reads=rk, writes=[('ps', sb)])
                pt = PT[i % nP]
                o_ap = pt[:, n0:512]
                i_ap = ps[sb][:, n0:512]
                b_ap = it['bias']
                S.op('act', 'activation', dict(
                    out=o_ap, in_=i_ap, func=AF.Exp, bias=b_ap, scale=1.0),
                    reads=it['bias_keys'], writes=[('ps', sb), ('pt', i % nP)])
                if it.get('mask') is not None:
                    m_ap, mkey = it['mask']
                    o2 = PTm[i % nP][:, n0:512]
                    S.op('dve', 'tensor_tensor', dict(
                        out=o2, in0=o_ap, in1=m_ap, op=ALU.mult),
                        reads=[('pt', i % nP), mkey], writes=[('ptm', i % nP)])
            k = i - Dp
            if k >= 0:
                it = items[k]
                n0 = it['n0']
                masked = it.get('mask') is not None
                rhs = (PTm if masked else PT)[k % nP][:, n0:512]
                ob = it['obank']
                o_ap = ps[ob][0:it['M'], n0:512]
                lh = it['pv_lhsT']
                S.op('pe', 'matmul', dict(
                    out=o_ap, lhsT=lh, rhs=rhs, start=it['first'], stop=it['last']),
                    reads=[('ptm' if masked else 'pt', k % nP)] + it['v_keys'], writes=[('ps', ob)])
                if it['last']:
                    it['normA']()
                    deferred.append((i + 2, it['normB']))
            while deferred and deferred[0][0] <= i:
                deferred.pop(0)[1]()
        for _, fn in deferred:
            fn()

    def make_norm(self, S, ps, ob, odd, rs, rs2, rinv, bcs, onesf, oT_ap_fn, okey):
        sr = 32 if odd else 64
        p0 = 64 if odd else 0

        def normA():
            S.op('dve', 'tensor_scalar', dict(out=rs[sr:sr + 1, :], in0=ps[ob][sr:sr + 1, :], scalar1=1e-30,
                                                  scalar2=None, op0=ALU.max), writes=[('ps', ob), 'rs'])
            S.op('dve', 'reciprocal', dict(out=rinv[sr:sr + 1, :], in_=rs[sr:sr + 1, :]), reads=['rs'], writes=['rinv'])

        def normB():
            S.op('pe', 'matmul', dict(out=ps[5][:, :], lhsT=onesf[sr:sr + 1, 0:128], rhs=rinv[sr:sr + 1, :],
                                          start=True, stop=True), reads=['rinv', 'onesf'], writes=[('ps', 5)])
            S.op('act', 'copy', dict(out=bcs[:, :], in_=ps[5][:, :]), writes=[('ps', 5), 'bcs'])
            S.op('dve', 'tensor_tensor', dict(out=oT_ap_fn(p0), in0=ps[ob][p0:p0 + 64, :], in1=bcs[p0:p0 + 64, :],
                                                  op=ALU.mult), reads=['bcs'], writes=[('ps', ob), okey])
        return normA, normB

    def phase_fox(self, S, l):
        nc, L, NT, NQ = self.nc, self.L, self.NT, self.NQ
        with ExitStack() as es:
            T = lambda name, shape, dt: self.T(es, name, shape, dt)
            xT = T("xT", [128, 8, L], BF16)
            wq = T("wq", [128, 8, 512], BF16)
            wk = T("wk", [128, 8, 512], BF16)
            wv = T("wv", [128, 8, 512], BF16)
            wf = T("wf", [128, 8, 8], BF16)
            stage = [T("stg%d" % i, [128, 1024], F32) for i in range(2)]
            Vb = T("Vb", [128, NT, 4, 160], BF16)
            qa = [T("qa%d" % i, [67, L], BF16) for i in range(2)]
            ka = [T("ka%d" % i, [67, L], BF16) for i in range(2)]
            negc = T("negc", [128, NT, 8], F32)
            identf = T("identf", [128, 128], F32)
            identb = T("identb", [128, 128], BF16)
            trim = T("trim", [128, 128], BF16)
            onesf = T("onesf", [128, 512], F32)
            bcol = T("bcol", [8, 2], F32)
            e8 = T("e8", [8, 512], F32)
            sp8 = T("sp8", [8, 512], F32)
            cb = [T("cb%d" % i, [8, 512], F32) for i in range(2)]
            r8 = e8
            r9 = sp8
            c3 = [T("c3%d" % i, [8, 3, 512], BF16) for i in range(2)]
            PT = [T("pt%d" % i, [128, 512], BF16) for i in range(6)]
            rs = T("rs", [128, 512], F32)
            rs2 = None
            rinv = T("rinv", [128, 512], F32)
            bcs = T("bcs", [128, 512], F32)
            oT = T("oT", [128, L], BF16)
            ps = self.PS(es)
            block = es.enter_context(nc.Block())

            for Q in range(NQ):
                S.dma(xT[:, :, Q * 512:(Q + 1) * 512], self.xbf[:, :, Q * 512:(Q + 1) * 512],
                      reads=[('xbf', Q)], writes=[('xT', Q)])
            S.dma(identf[:, :], self.c_ident[:, :], writes=['identf'])
            S.dma(stage[0][:, 0:128], self.c_trimask[:, :], writes=[('stage', 0)])
            S.op('dve', 'tensor_copy', dict(out=trim[:, :], in_=stage[0][:, 0:128]), reads=[('stage', 0)], writes=['trim'])
            S.op('dve', 'tensor_copy', dict(out=identb[:, :], in_=identf[:, :]), reads=['identf'], writes=['identb'])
            S.op('pool', 'memset', dict(ap=onesf[:, :], constant=1.0), writes=['onesf'])
            S.op('pool', 'memset', dict(ap=Vb[:, :, :, :], constant=0.0), writes=['Vb0'])
            S.op('pool', 'memset', dict(ap=Vb[:, :, :, 64:65], constant=1.0), reads=['Vb0'], writes=['Vb1'])
            for i in range(2):
                S.op('pool', 'memset', dict(ap=ka[i][64:67, :], constant=1.0), writes=[('kac', i)])
            S.dma(bcol[:, 0:1], self.bfg[l, :, :], writes=['bcol0'])
            S.op('dve', 'tensor_scalar', dict(out=bcol[:, 1:2], in0=bcol[:, 0:1], scalar1=-1.0, scalar2=None,
                                                  op0=ALU.mult), reads=['bcol0'], writes=['bcol1'])
            win = self.w_in[l]
            self.load_w(S, wq, win, 8, 512, stage, 'wq', col0=0)
            self.load_w(S, wk, win, 8, 512, stage, 'wk', col0=512)
            self.load_w(S, wv, win, 8, 512, stage, 'wv', col0=1024)
            self.load_w(S, wf, win, 8, 8, stage, 'wf', col0=1536)

            for Q in range(NQ):
                s = Q % 2
                qs = slice(Q * 512, (Q + 1) * 512)
                for c in range(8):
                    S.op('pe', 'matmul', dict(out=ps[6][0:8, :], lhsT=wf[:, c, :], rhs=xT[:, c, qs],
                                                             start=(c == 0), stop=(c == 7)),
                         reads=['wf', ('xT', Q)], writes=[('ps', 6)])
                S.op('act', 'activation', dict(out=e8[:, :], in_=ps[6][0:8, :], func=AF.Exp, bias=bcol[:, 1:2], scale=-1.0),
                     reads=['bcol1'], writes=[('ps', 6), 'e8'])
                S.op('act', 'activation', dict(out=sp8[:, :], in_=e8[:, :], func=AF.Ln, bias=onesf[0:8, 0:1], scale=1.0),
                     reads=['e8', 'onesf'], writes=['sp8'])
                if Q == 0:
                    init = 0.0
                else:
                    init = cb[1 - s][:, 511:512]
                S.op('dve', 'tensor_tensor_scan', dict(out=cb[s][:, :], data0=onesf[0:8, :], data1=sp8[:, :],
                                                                           initial=init, op0=ALU.mult, op1=ALU.subtract),
                     reads=['sp8', 'onesf', ('cb', 1 - s)], writes=[('cb', s)])
                S.op('dve', 'tensor_copy', dict(out=c3[s][:, 0, :], in_=cb[s][:, :]), reads=[('cb', s)], writes=[('c3a', s)])
                S.op('dve', 'tensor_tensor', dict(out=r8[:, :], in0=cb[s][:, :], in1=c3[s][:, 0, :], op=ALU.subtract),
                     reads=[('cb', s), ('c3a', s)], writes=['e8'])
                S.op('dve', 'tensor_copy', dict(out=c3[s][:, 1, :], in_=r8[:, :]), reads=['e8'], writes=[('c3b', s)])
                S.op('dve', 'tensor_tensor', dict(out=r9[:, :], in0=r8[:, :], in1=c3[s][:, 1, :], op=ALU.subtract),
                     reads=['e8', ('c3b', s)], writes=['sp8'])
                S.op('dve', 'tensor_copy', dict(out=c3[s][:, 2, :], in_=r9[:, :]), reads=['sp8'], writes=[('c3c', s)])
                S.dma(self.cspl[:, :, qs], c3[s][:, :, :], reads=[('c3a', s), ('c3b', s), ('c3c', s)], writes=[('cspl', Q)])
                for tt in range(4):
                    tile = Q * 4 + tt
                    S.op('pe', 'transpose', dict(out=ps[7][:, tile * 8:(tile + 1) * 8],
                                                                           in_=cb[s][:, tt * 128:(tt + 1) * 128],
                                                                           identity=identf[0:8, 0:8]),
                         reads=[('cb', s), 'identf'], writes=[('ps', 7)])
            S.op('dve', 'tensor_scalar', dict(out=negc[:, :, :].rearrange("p a b -> p (a b)"), in0=ps[7][:, 0:NT * 8],
                                                  scalar1=-1.0, scalar2=None, op0=ALU.mult),
                 writes=[('ps', 7), 'negc'])

            for tile in range(NT):
                b = 6 + tile % 2
                ts_ = slice(tile * 128, (tile + 1) * 128)
                for c in range(8):
                    S.op('pe', 'matmul', dict(out=ps[b][:, :], lhsT=xT[:, c, ts_], rhs=wv[:, c, :],
                                                                   start=(c == 0), stop=(c == 7)),
                         reads=['wv', ('xT', tile // 4)], writes=[('ps', b)])
                src = ps[b][:, :].rearrange("p (a b d) -> p a b d", a=4, b=2)
                S.op('act', 'copy', dict(out=Vb[:, tile, :, 0:64], in_=src[:, :, 0, :]),
                     reads=['Vb1'], writes=[('ps', b), ('V', tile, 0)])
                S.op('dve', 'tensor_copy', dict(out=Vb[:, tile, :, 96:160], in_=src[:, :, 1, :]),
                     reads=['Vb1'], writes=[('ps', b), ('V', tile, 1)])

            for h in range(8):
                hb = h % 2
                pr = h // 2
                odd = (h % 2 == 1)
                hs = slice(h * 64, (h + 1) * 64)
                S.dma(qa[hb][64:67, :], self.cspl[h, :, :], reads=[('cspl', Q) for Q in range(NQ)], writes=[('qac', hb)])
                for Q in range(NQ):
                    qs = slice(Q * 512, (Q + 1) * 512)
                    for c in range(8):
                        S.op('pe', 'matmul', dict(out=ps[6][0:64, :], lhsT=wq[:, c, hs], rhs=xT[:, c, qs],
                                                                 start=(c == 0), stop=(c == 7)),
                             reads=['wq', ('xT', Q)], writes=[('ps', 6)])
                    S.op('dve', 'tensor_scalar', dict(out=qa[hb][0:64, qs], in0=ps[6][0:64, :], scalar1=0.125,
                                                                scalar2=None, op0=ALU.mult),
                         writes=[('ps', 6), ('qa', hb, Q)])
                    for c in range(8):
                        S.op('pe', 'matmul', dict(out=ps[7][0:64, :], lhsT=wk[:, c, hs], rhs=xT[:, c, qs],
                                                                 start=(c == 0), stop=(c == 7)),
                             reads=['wk', ('xT', Q)], writes=[('ps', 7)])
                    S.op('dve', 'tensor_copy', dict(out=ka[hb][0:64, qs], in_=ps[7][0:64, :]),
                         writes=[('ps', 7), ('ka', hb, Q)])
                items = []
                for Q in range(NQ):
                    ob = 3 + Q % 2
                    qs = slice(Q * 512, (Q + 1) * 512)
                    nA, nB = self.make_norm(S, ps, ob, odd, rs, rs2, rinv, bcs, onesf,
                                            lambda p0, qs=qs: oT[p0:p0 + 64, qs], ('oT', Q))
                    for kb in range(4 * Q + 4):
                        j = kb - 4 * Q
                        n0 = 128 * j if j > 0 else 0
                        ks = slice(kb * 128, (kb + 1) * 128)
                        qk = [(ka[hb][0:67, ks], qa[hb][0:67, Q * 512 + n0:(Q + 1) * 512], n0, 512,
                               [('qa', hb, Q), ('qac', hb), ('ka', hb, kb // 4), ('kac', hb)])]
                        if j >= 0:
                            qk.append((identb[:, :], trim[:, :], n0, n0 + 128, ['identb', 'trim']))
                        if odd:
                            lh, M = Vb[:, kb, pr, 32:160], 128
                        else:
                            lh, M = Vb[:, kb, pr, 0:128], 128
                        items.append(dict(qk=qk, n0=n0, bias=negc[:, kb, h:h + 1], bias_keys=['negc'],
                                          pv_lhsT=lh, M=M, obank=ob, first=(kb == 0), last=(kb == 4 * Q + 3),
                                          v_keys=[('V', kb, 0), ('V', kb, 1), 'Vb1'], normA=nA, normB=nB))
                self.emit_attention(S, ps, PT, None, items)
                if odd:
                    S.dma(self.oa[:, pr, :], oT[:, :], reads=[('oT', Q) for Q in range(NQ)], writes=[('oa', pr)])
            S.end_phase(block)

    def phase_idx(self, S, l):
        nc, L, NT, NQ = self.nc, self.L, self.NT, self.NQ
        topk = min(TOPK, L // 4)
        with ExitStack() as es:
            T = lambda name, shape, dt: self.T(es, name, shape, dt)
            wik2 = T("wik2", [128, 8, 128], BF16)
            wiq = T("wiq", [128, 8, 512], BF16)
            wiw = T("wiw", [128, 8, 8], BF16)
            stage = [T("stg%d" % i, [128, 2048], F32) for i in range(2)]
            ikT = T("ikT", [128, L], BF16)
            xs = [T("xs%d" % i, [128, 8, 512], BF16) for i in range(2)]
            sc = T("sc", [128, 4, L], F32)
            mkq = T("mkq", [128, 4, L], BF16)
            junk = T("junk", [128, L], BF16)
            iqT = T("iqT", [128, 4, 512], BF16)
            rl = [T("rl%d" % i, [128, 512], F32) for i in range(4)]
            wtk = T("wtk", [128, 4, 8], F32)
            caus = T("caus", [128, 4, 512], F32)
            targ = T("targ", [128, NT], F32)
            lo = T("lo", [128, 4], F32)
            hi = T("hi", [128, 4], F32)
            step = T("step", [128, 4], F32)
            thr = T("thr", [128, 4], F32)
            cnt = T("cnt", [128, 4], F32)
            ge = T("ge", [128, 4], F32)
            identb = T("identb", [128, 128], BF16)
            mk = T("mk", [128, NT, 512], U8)
            ps = self.PS(es, 6)
            self._uid += 1
            pstb = [es.enter_context(nc.psum_tensor("pst%d_%d" % (i, self._uid), [128, 1024], BF16)) for i in range(2)]
            block = es.enter_context(nc.Block())

            win = self.w_in[l]
            S.dma(caus[:, :, :], self.c_causT[:, :, :], writes=['caus'])
            S.dma(targ[:, :], self.c_targ[:, :], writes=['targ'])
            S.dma(stage[0][:, 0:128], self.c_ident[:, :], writes=[('stage', 0)])
            S.op('dve', 'tensor_copy', dict(out=identb[:, :], in_=stage[0][:, 0:128]), reads=[('stage', 0)], writes=['identb'])
            self.load_w(S, wik2[:, :, 0:64], win, 8, 64, stage, 'wik', col0=3592)
            S.op('dve', 'tensor_copy', dict(out=wik2[:, :, 64:128], in_=wik2[:, :, 0:64]), reads=['wik'], writes=['wik2'])
            self.load_w(S, wiq, win, 8, 512, stage, 'wiq', col0=3080)
            self.load_w(S, wiw, win, 8, 8, stage, 'wiw', col0=3656)

            xcnt = [0]

            def load_xs(Q):
                i = xcnt[0] % 2
                xcnt[0] += 1
                S.dma(xs[i][:, :, :], self.xbf[:, :, Q * 512:(Q + 1) * 512], reads=[('xbf', Q)], writes=[('xs', i)])
                return i

            for Q in range(NQ):
                i = load_xs(Q)
                for c in range(8):
                    S.op('pe', 'matmul', dict(out=ps[4][:, :], lhsT=wik2[:, c, :], rhs=xs[i][:, c, :], start=(c == 0), stop=(c == 7)),
                         reads=['wik', 'wik2', ('xs', i)], writes=[('ps', 4)])
                S.op('act', 'copy', dict(out=ikT[:, Q * 512:(Q + 1) * 512], in_=ps[4][:, :]), writes=[('ps', 4), ('ikT', Q)])

            cvb = [T("cvb%d" % i, [128, 2048], BF16) for i in range(2)]
            wspecs = self.wspecs(l)
            chunks = []
            for (name, dst, src, C, N, col0) in wspecs:
                for c in range(C):
                    for n0 in range(0, N, 2048):
                        n1 = min(N, n0 + 2048)
                        chunks.append((dst[:, c, n0:n1], src[:, c, col0 + n0:col0 + n1], n1 - n0, ('wb', name, c, n0)))
            per_q = (len(chunks) + NQ - 1) // NQ
            per_q += per_q % 2

            def convert_some(Q):
                todo = chunks[Q * per_q:(Q + 1) * per_q]
                for p0 in range(0, len(todo), 2):
                    pair = todo[p0:p0 + 2]
                    for sl, (dst, src, n, key) in enumerate(pair):
                        S.dma(stage[sl][:, 0:n], src, writes=[('stage', sl)])
                    for sl, (dst, src, n, key) in enumerate(pair):
                        S.op('act', 'copy', dict(out=cvb[sl][:, 0:n], in_=stage[sl][:, 0:n]), reads=[('stage', sl)], writes=[('cvb', sl)])
                    for sl, (dst, src, n, key) in enumerate(pair):
                        S.dma(dst, cvb[sl][:, 0:n], reads=[('cvb', sl)], writes=[key])

            rcnt = 0
            for Q in range(NQ):
                i = load_xs(Q)
                nk = (Q + 1) * 512
                nkb = 4 * Q + 4
                for pr in range(4):
                    ms = slice(pr * 128, (pr + 1) * 128)
                    for c in range(8):
                        S.op('pe', 'matmul', dict(out=ps[4][:, :], lhsT=wiq[:, c, ms], rhs=xs[i][:, c, :], start=(c == 0), stop=(c == 7)),
                             reads=['wiq', ('xs', i)], writes=[('ps', 4)])
                    S.op('act', 'copy', dict(out=iqT[:, pr, :], in_=ps[4][:, :]), writes=[('ps', 4), ('iqT', pr)])
                for qsub in range(4):
                    for c in range(8):
                        S.op('pe', 'matmul', dict(out=ps[5][:, qsub * 8:(qsub + 1) * 8], lhsT=xs[i][:, c, qsub * 128:(qsub + 1) * 128],
                                                  rhs=wiw[:, c, :], start=(c == 0), stop=(c == 7)),
                             reads=['wiw', ('xs', i)], writes=[('ps', 5)])
                S.op('dve', 'tensor_scalar', dict(out=wtk[:, :, :].rearrange("p a b -> p (a b)"), in0=ps[5][:, 0:32], scalar1=IDX_SCALE,
                                                  scalar2=None, op0=ALU.mult), writes=[('ps', 5), 'wtk'])
                for kc in range(Q + 1):
                    ksl = slice(kc * 512, (kc + 1) * 512)
                    for h in range(8):
                        pr, half = h // 2, h % 2
                        rows = slice(half * 64, half * 64 + 64)
                        for qsub in range(4):
                            rb = rcnt % 4
                            rcnt += 1
                            S.op('pe', 'matmul', dict(out=ps[rb][:, :], lhsT=iqT[rows, pr, qsub * 128:(qsub + 1) * 128], rhs=ikT[rows, ksl],
                                                      start=True, stop=True),
                                 reads=[('ikT', kc), ('iqT', pr)], writes=[('ps', rb)])
                            S.op('act', 'activation', dict(out=rl[rb][:, :], in_=ps[rb][:, :], func=AF.Relu),
                                 writes=[('ps', rb), ('rl', rb)])
                            if h == 0:
                                S.op('dve', 'tensor_scalar', dict(out=sc[:, qsub, ksl], in0=rl[rb][:, :], scalar1=wtk[:, qsub, h:h + 1],
                                                                  scalar2=None, op0=ALU.mult),
                                     reads=[('rl', rb), 'wtk'], writes=[('sc', qsub, kc)])
                            else:
                                S.op('dve', 'scalar_tensor_tensor', dict(out=sc[:, qsub, ksl], in0=rl[rb][:, :], scalar=wtk[:, qsub, h:h + 1],
                                                                         in1=sc[:, qsub, ksl], op0=ALU.mult, op1=ALU.add),
                                     reads=[('rl', rb), 'wtk', ('sc', qsub, kc)], writes=[('sc', qsub, kc)])
                allk = lambda qsub: [('sc', qsub, kc) for kc in range(Q + 1)]
                for qsub in range(4):
                    S.op('dve', 'tensor_reduce', dict(out=lo[:, qsub:qsub + 1], in_=sc[:, qsub, 0:nk], axis=mybir.AxisListType.X, op=ALU.min),
                         reads=allk(qsub), writes=[('lo', qsub)])
                for qsub in range(4):
                    S.op('pool', 'tensor_tensor', dict(out=sc[:, qsub, Q * 512:nk], in0=sc[:, qsub, Q * 512:nk], in1=caus[:, qsub, :], op=ALU.add),
                         reads=[('sc', qsub, Q), 'caus', ('lo', qsub)], writes=[('sc', qsub, Q)])
                for qsub in range(4):
                    S.op('dve', 'tensor_reduce', dict(out=hi[:, qsub:qsub + 1], in_=sc[:, qsub, 0:nk], axis=mybir.AxisListType.X, op=ALU.max),
                         reads=allk(qsub), writes=[('hi', qsub)])
                S.op('dve', 'tensor_tensor', dict(out=step[:, :], in0=hi[:, :], in1=lo[:, :], op=ALU.subtract),
                     reads=[('hi', q) for q in range(4)] + [('lo', q) for q in range(4)], writes=['step'])
                for itn in range(BIS_N):
                    S.op('dve', 'tensor_scalar', dict(out=step[:, :], in0=step[:, :], scalar1=0.5, scalar2=None, op0=ALU.mult),
                         reads=['step'], writes=['step'])
                    S.op('dve', 'tensor_tensor', dict(out=thr[:, :], in0=lo[:, :], in1=step[:, :], op=ALU.add),
                         reads=['step'] + [('lo', q) for q in range(4)], writes=['thr'])
                    for qsub in range(4):
                        S.op('dve', 'tensor_scalar', dict(out=junk[:, 0:nk], in0=sc[:, qsub, 0:nk], scalar1=thr[:, qsub:qsub + 1], scalar2=0.0,
                                                          op0=ALU.is_ge, op1=ALU.add, accum_out=cnt[:, qsub:qsub + 1]),
                             reads=allk(qsub) + ['thr'], writes=['junk', ('cnt', qsub)])
                    S.op('dve', 'tensor_tensor', dict(out=ge[:, :], in0=cnt[:, :], in1=targ[:, 4 * Q:4 * Q + 4], op=ALU.is_ge),
                         reads=[('cnt', q) for q in range(4)] + ['targ'], writes=['ge'])
                    S.op('dve', 'tensor_tensor', dict(out=ge[:, :], in0=ge[:, :], in1=step[:, :], op=ALU.mult),
                         reads=['ge', 'step'], writes=['ge'])
                    S.op('dve', 'tensor_tensor', dict(out=lo[:, :], in0=lo[:, :], in1=ge[:, :], op=ALU.add),
                         reads=['ge'] + [('lo', q) for q in range(4)], writes=[('lo', q) for q in range(4)])
                for qsub in range(4):
                    S.op('dve', 'tensor_scalar', dict(out=mkq[:, qsub, 0:nk], in0=sc[:, qsub, 0:nk], scalar1=lo[:, qsub:qsub + 1], scalar2=None,
                                                      op0=ALU.is_ge),
                         reads=allk(qsub) + [('lo', qsub)], writes=[('mkq', qsub)])
                for kb in range(nkb):
                    pb = kb % 2
                    for qsub in range(4):
                        S.op('pe', 'transpose', dict(out=pstb[pb][:, qsub * 128:(qsub + 1) * 128],
                                                     in_=mkq[:, qsub, kb * 128:(kb + 1) * 128], identity=identb[:, :]),
                             reads=[('mkq', qsub), 'identb'], writes=[('pst', pb)])
                    S.op('act', 'copy', dict(out=mk[:, kb, :], in_=pstb[pb][:, 0:512]), writes=[('pst', pb), ('mk', kb)])
                S.dma(self.mskd[Q, :, 0:nkb, :], mk[:, 0:nkb, :], reads=[('mk', kb) for kb in range(nkb)], writes=[('mskd', Q)])
                convert_some(Q)
            S.end_phase(block)

    def phase_dsa(self, S, l):
        nc, L, NT, NQ = self.nc, self.L, self.NT, self.NQ
        with ExitStack() as es:
            T = lambda name, shape, dt: self.T(es, name, shape, dt)
            wdq = T("wdq", [128, 8, 512], BF16)
            wdk = T("wdk", [128, 8, 512], BF16)
            wdv = T("wdv", [128, 8, 512], BF16)
            stage = [T("stg%d" % i, [128, 2048], F32) for i in range(2)]
            xs = [T("xs%d" % i, [128, 8, 512], BF16) for i in range(2)]
            kT = T("kT", [128, 4, L], BF16)
            Vb = T("Vb", [128, NT, 4, 160], BF16)
            qd = T("qd", [128, 4, 512], BF16)
            mk = T("mk", [128, NT, 512], U8)
            kbias = T("kbias", [128, NT, 8], F32)
            kbd = T("kbd", [128, NT, 8], F32)
            kbq = T("kbq", [128, NT, 8], F32)
            nsl = T("nsl", [128, 8, 128], BF16)
            tpq = T("tpq", [128, 512], BF16)
            PT = [T("pt%d" % i, [128, 512], BF16) for i in range(6)]
            PTm = [T("ptm%d" % i, [128, 512], BF16) for i in range(6)]
            rs = T("rs", [128, 512], F32)
            rs2 = None
            rinv = T("rinv", [128, 512], F32)
            bcs = T("bcs", [128, 512], F32)
            onesf = T("onesf", [128, 128], F32)
            oTq = [T("oTq%d" % i, [128, 4, 512], BF16) for i in range(2)]
            identb = T("identb", [128, 128], BF16)
            trim = T("trim", [128, 128], BF16)
            ps = self.PS(es)
            block = es.enter_context(nc.Block())
            S.dma(stage[1][:, 0:128], self.c_trimask[:, :], writes=[('stage', 1)])
            S.op('dve', 'tensor_copy', dict(out=trim[:, :], in_=stage[1][:, 0:128]), reads=[('stage', 1)], writes=['trim'])
            S.dma(stage[1][:, 128:256], self.c_ident[:, :], reads=['trim'], writes=[('stage', 1)])
            S.op('dve', 'tensor_copy', dict(out=identb[:, :], in_=stage[1][:, 128:256]), reads=[('stage', 1)], writes=['identb'])

            win = self.w_in[l]
            S.dma(kbias[:, :, :], self.c_kbias[:, :, :], writes=['kbias'])
            S.dma(kbd[:, :, :], self.c_kbd[:, :, :], writes=['kbd'])
            S.dma(stage[0][:, 0:1024], self.c_negslope[:, :, :].rearrange("p a b -> p (a b)"), writes=[('stage', 0)])
            S.op('dve', 'tensor_copy', dict(out=nsl[:, :, :].rearrange("p a b -> p (a b)"), in_=stage[0][:, 0:1024]),
                 reads=[('stage', 0)], writes=['nsl'])
            S.op('pool', 'memset', dict(ap=onesf[:, :], constant=1.0), writes=['onesf'])
            S.op('pool', 'memset', dict(ap=Vb[:, :, :, :], constant=0.0), writes=['Vb0'])
            S.op('pool', 'memset', dict(ap=Vb[:, :, :, 64:65], constant=1.0), reads=['Vb0'], writes=['Vb1'])
            self.load_w(S, wdq, win, 8, 512, stage, 'wdq', col0=1544)
            self.load_w(S, wdk, win, 8, 512, stage, 'wdk', col0=2056)
            self.load_w(S, wdv, win, 8, 512, stage, 'wdv', col0=2568)
            xcnt = [0]

            def load_xs(Q):
                i = xcnt[0] % 2
                xcnt[0] += 1
                S.dma(xs[i][:, :, :], self.xbf[:, :, Q * 512:(Q + 1) * 512], reads=[('xbf', Q)], writes=[('xs', i)])
                return i

            for Q in range(NQ):
                i = load_xs(Q)
                qs = slice(Q * 512, (Q + 1) * 512)
                for pr in range(4):
                    ms = slice(pr * 128, (pr + 1) * 128)
                    for c in range(8):
                        S.op('pe', 'matmul', dict(out=ps[6][:, :], lhsT=wdk[:, c, ms], rhs=xs[i][:, c, :], start=(c == 0), stop=(c == 7)),
                             reads=['wdk', ('xs', i)], writes=[('ps', 6)])
                    S.op('dve', 'tensor_copy', dict(out=kT[:, pr, qs], in_=ps[6][:, :]), writes=[('ps', 6), ('kT', Q)])
                for tt in range(4):
                    tile = Q * 4 + tt
                    for c in range(8):
                        S.op('pe', 'matmul', dict(out=ps[7][:, :], lhsT=xs[i][:, c, tt * 128:(tt + 1) * 128], rhs=wdv[:, c, :],
                                                  start=(c == 0), stop=(c == 7)),
                             reads=['wdv', ('xs', i)], writes=[('ps', 7)])
                    src = ps[7][:, :].rearrange("p (a b d) -> p a b d", a=4, b=2)
                    S.op('act', 'copy', dict(out=Vb[:, tile, :, 0:64], in_=src[:, :, 0, :]), reads=['Vb1'], writes=[('ps', 7), ('V', tile, 0)])
                    S.op('dve', 'tensor_copy', dict(out=Vb[:, tile, :, 96:160], in_=src[:, :, 1, :]), reads=['Vb1'],
                         writes=[('ps', 7), ('V', tile, 1)])

            for Q in range(NQ):
                i = load_xs(Q)
                nkb = 4 * Q + 4
                qs = slice(Q * 512, (Q + 1) * 512)
                S.dma(mk[:, 0:nkb, :], self.mskd[Q, :, 0:nkb, :], reads=[('mskd', Q)], writes=['mk'])
                S.dma(stage[1][:, 0:512], self.c_tpos[:, qs], writes=[('stage', 1)])
                S.op('dve', 'tensor_copy', dict(out=tpq[:, :], in_=stage[1][:, 0:512]), reads=[('stage', 1)], writes=['tpq'])
                for pr in range(4):
                    ms = slice(pr * 128, (pr + 1) * 128)
                    for c in range(8):
                        S.op('pe', 'matmul', dict(out=ps[6][:, :], lhsT=wdq[:, c, ms], rhs=xs[i][:, c, :], start=(c == 0), stop=(c == 7)),
                             reads=['wdq', ('xs', i)], writes=[('ps', 6)])
                    S.op('dve', 'tensor_scalar', dict(out=qd[:, pr, :], in0=ps[6][:, :], scalar1=0.125, scalar2=None, op0=ALU.mult),
                         writes=[('ps', 6), ('qd', pr)])
                S.op('dve', 'scalar_tensor_tensor', dict(out=kbq[:, :, :].rearrange("p a b -> p (a b)"),
                                                         in0=kbd[:, :, :].rearrange("p a b -> p (a b)"), scalar=float(Q * 512 + 511),
                                                         in1=kbias[:, :, :].rearrange("p a b -> p (a b)"), op0=ALU.mult, op1=ALU.add),
                     reads=['kbias', 'kbd'], writes=['kbq'])
                items = []
                oq = oTq[Q % 2]
                for h in range(8):
                    half, pr, odd = h % 2, h // 2, (h % 2 == 1)
                    rows = slice(half * 64, half * 64 + 64)
                    r2 = slice(half * 64, half * 64 + 2)
                    ob = 3 + h % 2
                    nA, nB = self.make_norm(S, ps, ob, odd, rs, rs2, rinv, bcs, onesf,
                                            lambda p0, oq=oq, pr=pr: oq[p0:p0 + 64, pr, :], ('oTq', Q % 2, h))
                    for kb in range(nkb):
                        j = kb - 4 * Q
                        n0 = 128 * j if j > 0 else 0
                        ks = slice(kb * 128, (kb + 1) * 128)
                        qk = [(kT[rows, pr, ks], qd[rows, pr, n0:512], n0, 512, [('kT', kb // 4), ('qd', pr)])]
                        if h < 2:
                            qk.append((nsl[rows, h, :], tpq[rows, n0:512], n0, 512, ['nsl', 'tpq']))
                        if j >= 0:
                            qk.append((identb[:, :], trim[:, :], n0, n0 + 128, ['identb', 'trim']))
                        lh = Vb[:, kb, pr, 32:160] if odd else Vb[:, kb, pr, 0:128]
                        items.append(dict(qk=qk, n0=n0, bias=kbq[:, kb, h:h + 1], bias_keys=['kbq'],
                                          mask=(mk[:, kb, n0:512], 'mk'),
                                          pv_lhsT=lh, M=128, obank=ob, first=(kb == 0), last=(kb == nkb - 1),
                                          v_keys=[('V', kb, 0), ('V', kb, 1), 'Vb1'], normA=nA, normB=nB))
                self.emit_attention(S, ps, PT, PTm, items)
                S.dma(self.ob[:, :, qs], oq[:, :, :], reads=[('oTq', Q % 2, h) for h in range(8)], writes=[('ob', Q)])
            S.end_phase(block)

    def ln_block(self, S, ps, xr, sq, mean, lnv, rstd, lnp, gi, onesf, epst, outb, xkey):
        for m in range(8):
            S.op('pe', 'matmul', dict(out=ps[5][:, :], lhsT=onesf[:, :], rhs=xr[:, m, :], start=(m == 0), stop=(m == 7)),
                 reads=['onesf', (xkey, m)], writes=[('ps', 5)])
            S.op('act', 'activation', dict(out=sq[m % 2][:, :], in_=xr[:, m, :], func=AF.Square), reads=[(xkey, m)], writes=[('sq', m % 2)])
            S.op('pe', 'matmul', dict(out=ps[6][:, :], lhsT=onesf[:, :], rhs=sq[m % 2][:, :], start=(m == 0), stop=(m == 7)),
                 reads=['onesf', ('sq', m % 2)], writes=[('ps', 6)])
        S.op('dve', 'tensor_scalar', dict(out=mean[:, :], in0=ps[5][:, :], scalar1=1.0 / D, scalar2=None, op0=ALU.mult),
             writes=[('ps', 5), 'mean'])
        S.op('dve', 'tensor_tensor', dict(out=lnv[:, :], in0=mean[:, :], in1=mean[:, :], op=ALU.mult), reads=['mean'], writes=['lnv'])
        S.op('dve', 'scalar_tensor_tensor', dict(out=lnv[:, :], in0=ps[6][:, :], scalar=1.0 / D, in1=lnv[:, :], op0=ALU.mult, op1=ALU.subtract),
             reads=['lnv'], writes=[('ps', 6), 'lnv'])
        S.op('act', 'activation', dict(out=lnv[:, :], in_=lnv[:, :], func=AF.Ln, bias=epst[:, 0:1], scale=1.0), reads=['lnv', 'epst'], writes=['lnv'])
        S.op('act', 'activation', dict(out=rstd[:, :], in_=lnv[:, :], func=AF.Exp, scale=-0.5), reads=['lnv'], writes=['rstd'])
        for m in range(8):
            S.op('dve', 'tensor_tensor', dict(out=xr[:, m, :], in0=xr[:, m, :], in1=mean[:, :], op=ALU.subtract),
                 reads=[(xkey, m), 'mean'], writes=[(xkey, m)])
            S.op('dve', 'tensor_tensor', dict(out=xr[:, m, :], in0=xr[:, m, :], in1=rstd[:, :], op=ALU.mult),
                 reads=[(xkey, m), 'rstd'], writes=[(xkey, m)])
            S.op('dve', 'tensor_scalar', dict(out=xr[:, m, :], in0=xr[:, m, :], scalar1=lnp[:, gi, m:m + 1], scalar2=lnp[:, gi + 1, m:m + 1],
                                              op0=ALU.mult, op1=ALU.add),
                 reads=[(xkey, m), 'lnp'], writes=[(xkey, m)])
            if outb is not None:
                S.op('pool', 'tensor_copy', dict(out=outb[:, m, :], in_=xr[:, m, :]), reads=[(xkey, m)], writes=[('xob', m)])

    def phase_merge(self, S, l):
        nc, L, NT, NQ = self.nc, self.L, self.NT, self.NQ
        with ExitStack() as es:
            T = lambda name, shape, dt: self.T(es, name, shape, dt)
            wA = T("wA", [128, 4, D], BF16)
            wB = T("wB", [128, 4, D], BF16)
            wO = T("wO", [128, 8, D], BF16)
            wga = T("wga", [128, 8, D], BF16)
            wgb = T("wgb", [128, 8, D], BF16)
            stage = [T("stg%d" % i, [128, 2048], F32) for i in range(2)]
            lnp = T("lnp", [128, 4, 8], F32)
            oab = T("oab", [128, 4, 512], BF16)
            obb = T("obb", [128, 4, 512], BF16)
            xs = T("xs", [128, 8, 512], BF16)
            xr = T("xr", [128, 8, 512], F32)
            mg = T("mg", [128, 8, 512], BF16)
            sa = T("sa", [128, 512], F32)
            sb = T("sb", [128, 512], F32)
            sq = [T("sq%d" % i, [128, 512], F32) for i in range(2)]
            mean = T("mean", [128, 512], F32)
            lnv = T("lnv", [128, 512], F32)
            rstd = T("rstd", [128, 512], F32)
            x1b = T("x1b", [128, 8, 512], BF16)
            onesf = T("onesf", [128, 128], F32)
            epst = T("epst", [128, 1], F32)
            ps = self.PS(es)
            block = es.enter_context(nc.Block())
            S.op('pool', 'memset', dict(ap=onesf[:, :], constant=1.0), writes=['onesf'])
            S.op('pool', 'memset', dict(ap=epst[:, :], constant=LN_EPS), writes=['epst'])
            S.dma(lnp[:, :, :], self.lnp[l], writes=['lnp'])
            self.load_wb(S, wga, self.wb_ga, 8, D, 'ga', 'wga')
            self.load_wb(S, wgb, self.wb_gb, 8, D, 'gb', 'wgb')
            self.load_wb(S, wA, self.wb_a, 4, D, 'a', 'wA')
            self.load_wb(S, wB, self.wb_b, 4, D, 'b', 'wB')
            self.load_wb(S, wO, self.wb_o, 8, D, 'o', 'wO')
            for Q in range(NQ):
                qs = slice(Q * 512, (Q + 1) * 512)
                S.dma(oab[:, :, :], self.oa[:, :, qs], reads=[('oa', p) for p in range(4)], writes=['oab'])
                S.dma(obb[:, :, :], self.ob[:, :, qs], reads=[('ob', Q)], writes=['obb'])
                S.dma(xs[:, :, :], self.xbf[:, :, qs], reads=[('xbf', Q)], writes=['xs'])
                S.dma(xr[:, :, :], self.xres[:, :, qs], reads=[('xres', Q)], writes=[('xr', m) for m in range(8)])
                for m in range(8):
                    ms = slice(m * 128, (m + 1) * 128)
                    for c in range(4):
                        S.op('pe', 'matmul', dict(out=ps[0][:, :], lhsT=wA[:, c, ms], rhs=oab[:, c, :], start=(c == 0), stop=(c == 3)),
                             reads=['wA', 'oab'], writes=[('ps', 0)])
                    for c in range(4):
                        S.op('pe', 'matmul', dict(out=ps[1][:, :], lhsT=wB[:, c, ms], rhs=obb[:, c, :], start=(c == 0), stop=(c == 3)),
                             reads=['wB', 'obb'], writes=[('ps', 1)])
                    for c in range(8):
                        S.op('pe', 'matmul', dict(out=ps[2][:, :], lhsT=wga[:, c, ms], rhs=xs[:, c, :], start=(c == 0), stop=(c == 7)),
                             reads=['wga', 'xs'], writes=[('ps', 2)])
                    for c in range(8):
                        S.op('pe', 'matmul', dict(out=ps[3][:, :], lhsT=wgb[:, c, ms], rhs=xs[:, c, :], start=(c == 0), stop=(c == 7)),
                             reads=['wgb', 'xs'], writes=[('ps', 3)])
                    S.op('act', 'activation', dict(out=sa[:, :], in_=ps[2][:, :], func=AF.Sigmoid), writes=[('ps', 2), 'sa'])
                    S.op('act', 'activation', dict(out=sb[:, :], in_=ps[3][:, :], func=AF.Sigmoid), writes=[('ps', 3), 'sb'])
                    S.op('dve', 'tensor_tensor', dict(out=sa[:, :], in0=sa[:, :], in1=ps[0][:, :], op=ALU.mult), reads=['sa'], writes=[('ps', 0), 'sa'])
                    S.op('dve', 'tensor_tensor', dict(out=sb[:, :], in0=sb[:, :], in1=ps[1][:, :], op=ALU.mult), reads=['sb'], writes=[('ps', 1), 'sb'])
                    S.op('pool', 'tensor_tensor', dict(out=mg[:, m, :], in0=sa[:, :], in1=sb[:, :], op=ALU.add), reads=['sa', 'sb'], writes=[('mg', m)])
                for m in range(8):
                    ms = slice(m * 128, (m + 1) * 128)
                    for c in range(8):
                        S.op('pe', 'matmul', dict(out=ps[4][:, :], lhsT=wO[:, c, ms], rhs=mg[:, c, :], start=(c == 0), stop=(c == 7)),
                             reads=['wO', ('mg', c)], writes=[('ps', 4)])
                    S.op('dve', 'scalar_tensor_tensor', dict(out=xr[:, m, :], in0=xr[:, m, :], scalar=ALPHA, in1=ps[4][:, :], op0=ALU.mult, op1=ALU.add),
                         reads=[('xr', m)], writes=[('ps', 4), ('xr', m)])
                self.ln_block(S, ps, xr, sq, mean, lnv, rstd, lnp, 0, onesf, epst, x1b, 'xr')
                S.dma(self.xres[:, :, qs], xr[:, :, :], reads=[('xr', m) for m in range(8)], writes=[('xres', Q)])
                S.dma(self.xbf[:, :, qs], x1b[:, :, :], reads=[('xob', m) for m in range(8)], writes=[('xbf', Q)])
            S.end_phase(block)

    def phase_ffn(self, S, l, last):
        nc, L, NT, NQ = self.nc, self.L, self.NT, self.NQ
        NJ = DFF // 128
        with ExitStack() as es:
            T = lambda name, shape, dt: self.T(es, name, shape, dt)
            w1 = T("w1", [128, 8, 2 * DFF], BF16)
            w2 = T("w2", [128, NJ, D], BF16)
            stage = [T("stg%d" % i, [128, 512], F32) for i in range(2)]
            lnp = T("lnp", [128, 4, 8], F32)
            xs = T("xs", [128, 8, 512], BF16)
            xr = T("xr", [128, 8, 512], F32)
            hT = T("hT", [128, NJ, 512], BF16)
            sq = [T("sq%d" % i, [128, 512], F32) for i in range(2)]
            mean = T("mean", [128, 512], F32)
            lnv = T("lnv", [128, 512], F32)
            rstd = T("rstd", [128, 512], F32)
            if last:
                outst = [T("outst%d" % i, [128, 512], F32) for i in range(4)]
                identf = T("identf", [128, 128], F32)
                x2b = None
            else:
                x2b = T("x2b", [128, 8, 512], BF16)
            onesf = T("onesf", [128, 128], F32)
            epst = T("epst", [128, 1], F32)
            ps = self.PS(es)
            block = es.enter_context(nc.Block())
            S.op('pool', 'memset', dict(ap=onesf[:, :], constant=1.0), writes=['onesf'])
            S.op('pool', 'memset', dict(ap=epst[:, :], constant=LN_EPS), writes=['epst'])
            S.dma(lnp[:, :, :], self.lnp[l], writes=['lnp'])
            if last:
                S.dma(identf[:, :], self.c_ident[:, :], writes=['identf'])
            self.load_wb(S, w1, self.wb_f1, 8, 2 * DFF, 'f1', 'w1')
            self.load_wb(S, w2, self.wb_f2, NJ, D, 'f2', 'w2')
            ocnt = 0
            for Q in range(NQ):
                qs = slice(Q * 512, (Q + 1) * 512)
                S.dma(xs[:, :, :], self.xbf[:, :, qs], reads=[('xbf', Q)], writes=['xs'])
                S.dma(xr[:, :, :], self.xres[:, :, qs], reads=[('xres', Q)], writes=[('xr', m) for m in range(8)])
                for j in range(NJ):
                    bg, bu = 2 * (j % 2), 2 * (j % 2) + 1
                    for c in range(8):
                        S.op('pe', 'matmul', dict(out=ps[bg][:, :], lhsT=w1[:, c, j * 128:(j + 1) * 128], rhs=xs[:, c, :], start=(c == 0), stop=(c == 7)),
                             reads=['w1', 'xs'], writes=[('ps', bg)])
                    for c in range(8):
                        S.op('pe', 'matmul', dict(out=ps[bu][:, :], lhsT=w1[:, c, DFF + j * 128:DFF + (j + 1) * 128], rhs=xs[:, c, :],
                                                  start=(c == 0), stop=(c == 7)),
                             reads=['w1', 'xs'], writes=[('ps', bu)])
                    S.op('act', 'activation', dict(out=sq[j % 2][:, :], in_=ps[bg][:, :], func=AF.Silu), writes=[('ps', bg), ('sq', j % 2)])
                    S.op('dve', 'tensor_tensor', dict(out=hT[:, j, :], in0=sq[j % 2][:, :], in1=ps[bu][:, :], op=ALU.mult),
                         reads=[('sq', j % 2)], writes=[('ps', bu), ('hT', j)])
                for m in range(8):
                    ms = slice(m * 128, (m + 1) * 128)
                    b = 4 if m % 2 == 0 else 7
                    for j in range(NJ):
                        S.op('pe', 'matmul', dict(out=ps[b][:, :], lhsT=w2[:, j, ms], rhs=hT[:, j, :], start=(j == 0), stop=(j == NJ - 1)),
                             reads=['w2', ('hT', j)], writes=[('ps', b)])
                    S.op('dve', 'scalar_tensor_tensor', dict(out=xr[:, m, :], in0=xr[:, m, :], scalar=ALPHA, in1=ps[b][:, :], op0=ALU.mult, op1=ALU.add),
                         reads=[('xr', m)], writes=[('ps', b), ('xr', m)])
                self.ln_block(S, ps, xr, sq, mean, lnv, rstd, lnp, 2, onesf, epst, x2b, 'xr')
                if not last:
                    S.dma(self.xres[:, :, qs], xr[:, :, :], reads=[('xr', m) for m in range(8)], writes=[('xres', Q)])
                    S.dma(self.xbf[:, :, qs], x2b[:, :, :], reads=[('xob', m) for m in range(8)], writes=[('xbf', Q)])
                else:
                    for tt in range(4):
                        tile = Q * 4 + tt
                        for half in range(2):
                            b = 0 + (ocnt % 4)
                            oi = ocnt % 4
                            ocnt += 1
                            for cc in range(4):
                                c = half * 4 + cc
                                S.op('pe', 'transpose', dict(out=ps[b][:, cc * 128:(cc + 1) * 128], in_=xr[:, c, tt * 128:(tt + 1) * 128],
                                                             identity=identf[:, :]),
                                     reads=[('xr', c), 'identf'], writes=[('ps', b)])
                            S.op('act', 'copy', dict(out=outst[oi][:, :], in_=ps[b][:, :]), writes=[('ps', b), ('outst', oi)])
                            S.dma(self.out[tile * 128:(tile + 1) * 128, half * 512:(half + 1) * 512], outst[oi][:, :],
                                  reads=[('outst', oi)], writes=[('out', tile, half)])
            S.end_phase(block)

    def build(self):
        nc = self.nc
        self._ldi = 0
        with ExitStack() as gs:
            S = Sched(nc, gs)
            self.S = S
            self.phase0(S)
            stop = self.dbg
            for l in range(self.nlayers):
                last = (l == self.nlayers - 1)
                if ('stop_p0' in stop):
                    break
                self.phase_fox(S, l)
                if ('stop_fox' in stop):
                    break
                self.phase_idx(S, l)
                if ('stop_idx' in stop):
                    break
                self.phase_dsa(S, l)
                if ('stop_dsa' in stop):
                    break
                self.phase_merge(S, l)
                if ('stop_merge' in stop):
                    break
                self.phase_ffn(S, l, last)
        return nc


def host_consts(L):
    NT = L // 128
    p = np.arange(128)[:, None]
    f = np.arange(128)[None, :]
    c = {}
    c["c_ident"] = np.eye(128, dtype=np.float32)
    c["c_trimask"] = np.where(p > f, -30000.0, 0.0).astype(np.float32)
    cm = np.zeros((128, 4, 512), np.float32)
    f5 = np.arange(512)[None, :]
    for j in range(4):
        cm[:, j, :] = np.where(f5 > 128 * j + p, -1e30, 0.0)
    c["c_causT"] = cm
    tq = np.arange(NT)[None, :] * 128 + p
    c["c_targ"] = np.minimum(min(TOPK, L // 4), tq + 1).astype(np.float32)
    slopes = np.exp2(-8.0 * (np.arange(8, dtype=np.float32) + 1.0) / 8).astype(np.float32)
    kb = np.zeros((128, NT, 8), np.float32)
    for t in range(NT):
        kb[:, t, :] = (t * 128 + np.arange(128))[:, None] * slopes[None, :] + ALIBI_C
    c["c_kbias"] = kb
    kd = np.zeros((128, NT, 8), np.float32)
    kd[:, :, 2:] = -slopes[None, None, 2:]
    c["c_kbd"] = kd
    ns = np.zeros((128, 8, 128), np.float32)
    for base in (0, 1, 64, 65):
        ns[base, :, :] = -slopes[:, None]
    c["c_negslope"] = ns
    tp = np.zeros((128, L), np.float32)
    t = np.arange(L)
    for base in (0, 64):
        tp[base, :] = (t // 16) * 16
        tp[base + 1, :] = t % 16
    c["c_tpos"] = tp
    return c


def host_weights(w_in, b_forget, w_branch_a, w_branch_b, w_out, ln1_g, ln1_b, w_ffn_in, w_ffn_out, ln2_g, ln2_b):
    def fm(w):
        k = w.shape[1]
        return np.ascontiguousarray(w.reshape(2, k // 128, 128, w.shape[2]).transpose(0, 2, 1, 3))
    m = {}
    m["w_in"] = fm(w_in)
    iw = w_in[:, :, 3656:3664]
    m["w_iwrep"] = fm(np.repeat(iw, 64, axis=2))
    m["bfg"] = np.ascontiguousarray(b_forget.reshape(2, 8, 1))
    m["w_a"] = fm(w_branch_a)
    m["w_b"] = fm(w_branch_b)
    m["w_o"] = fm(w_out)
    lp = np.stack([ln1_g, ln1_b, ln2_g, ln2_b], axis=1)
    m["lnp"] = np.ascontiguousarray(lp.reshape(2, 4, 8, 128).transpose(0, 3, 1, 2))
    m["w_f1"] = fm(w_ffn_in)
    m["w_f2"] = fm(w_ffn_out)
    return m


_CACHE = {}


def kernel(x, w_in, b_forget, w_branch_a, w_branch_b, w_out, ln1_g, ln1_b,
           w_ffn_in, w_ffn_out, ln2_g, ln2_b):
    x = np.asarray(x, np.float32)
    B, L, _ = x.shape
    if L not in _CACHE:
        _CACHE[L] = Builder(L).build()
    nc = _CACHE[L]
    shared = host_consts(L)
    shared.update(host_weights(*[np.asarray(a, np.float32) for a in
                                 (w_in, b_forget, w_branch_a, w_branch_b, w_out, ln1_g, ln1_b,
                                  w_ffn_in, w_ffn_out, ln2_g, ln2_b)]))
    in_maps = []
    for b in range(B):
        m = dict(shared)
        m["x"] = np.ascontiguousarray(x[b])
        in_maps.append(m)
    res = run_bass_kernel_spmd(nc, in_maps, core_ids=list(range(B)))
    return np.stack([np.asarray(r["out"], np.float32) for r in res.results], axis=0)
```

```python
import math
from contextlib import ExitStack
import numpy as np
import concourse.bass as bass
import concourse.mybir as mybir
from concourse.bass_utils import run_bass_kernel_spmd

F32 = mybir.dt.float32
BF16 = mybir.dt.bfloat16
U8 = mybir.dt.uint8
ALU = mybir.AluOpType
AF = mybir.ActivationFunctionType

D = 1024
DFF = 2816
ALPHA = 4.0 ** 0.25
LN_EPS = 1e-5
IDX_SCALE = (8 ** -0.5) * (64 ** -0.5)
TOPK = 256
BIS_B = 64.0
BIS_N = 14
ALIBI_C = 50.0


class Sched:
    def __init__(self, nc, stack):
        self.nc = nc
        self.names = ['sp', 'act', 'dve', 'pool', 'pe']
        self.lists = {k: [] for k in self.names}
        self.sem = {k: stack.enter_context(nc.semaphore("s_" + k)) for k in ['pe', 'act', 'dve', 'pool']}
        self.cnt = {k: 0 for k in self.sem}
        self.ndma = 12
        self.dsem = {q: [stack.enter_context(nc.semaphore("d_%s%d" % (q, i))) for i in range(self.ndma)]
                     for q in ['sp', 'pool', 'act']}
        self.dcnt = {q: 0 for q in self.dsem}
        self.waited = {k: {} for k in self.names}
        self.lastw = {}
        self.readers = {}
        self.ninstr = 0

    def _wait(self, eng, tok):
        semid, sem, val = tok[0], tok[1], tok[2]
        w = self.waited[eng]
        if w.get(semid, 0) >= val:
            return
        w[semid] = val
        self.lists[eng].append(('w', sem, val))

    def _deps(self, eng, reads, writes):
        for r in reads:
            t = self.lastw.get(r)
            if t is not None:
                if not (t[3] == eng and eng == 'pe'):
                    self._wait(eng, t)
        for wk in writes:
            t = self.lastw.get(wk)
            if t is not None and (t[3] != eng or eng != 'pe'):
                self._wait(eng, t)
            rd = self.readers.get(wk)
            if rd:
                for t in rd.values():
                    if t[3] != eng:
                        self._wait(eng, t)

    def _commit(self, tok, reads, writes):
        for wk in writes:
            self.lastw[wk] = tok
            self.readers[wk] = {}
        for r in reads:
            self.readers.setdefault(r, {})[tok[0]] = tok

    def op(self, eng, meth, kw, reads=(), writes=()):
        fn = lambda e: getattr(e, meth)(**kw)
        self._deps(eng, reads, writes)
        self.cnt[eng] += 1
        tok = (eng, self.sem[eng], self.cnt[eng], eng)
        self.lists[eng].append(('o', fn, self.sem[eng], 1))
        self._commit(tok, reads, writes)
        self.ninstr += 1
        return tok

    def dma(self, out_ap, in_ap, reads=(), writes=(), q='sp'):
        self._deps(q, reads, writes)
        i = self.dcnt[q]
        self.dcnt[q] += 1
        slot, rnd = i % self.ndma, i // self.ndma
        sem = self.dsem[q][slot]
        semid = ('d', q, slot)
        if rnd > 0:
            self._wait(q, (semid, sem, 16 * rnd))
        tok = (semid, sem, 16 * (rnd + 1), 'dma')
        self.lists[q].append(('o', lambda e: e.dma_start(out=out_ap, in_=in_ap), sem, 16))
        self._commit(tok, reads, writes)
        self.ninstr += 1
        return tok

    def end_phase(self, block):
        for q in self.dsem:
            n = self.dcnt[q]
            for slot in range(min(n, self.ndma)):
                last = ((n - 1 - slot) // self.ndma) + 1
                self._wait(q, (('d', q, slot), self.dsem[q][slot], 16 * last))
        decos = {'sp': block.sync, 'act': block.scalar, 'dve': block.vector,
                 'pool': block.gpsimd, 'pe': block.tensor}
        for name in self.names:
            lst = self.lists[name]
            self.lists[name] = []

            def body(e, lst=lst):
                for it in lst:
                    if it[0] == 'w':
                        e.wait_ge(it[1], it[2])
                    else:
                        it[1](e).then_inc(it[2], it[3])
            decos[name](body)


class Builder:
    def __init__(self, L, nlayers=2, dbg=()):
        self.L = L
        self.NT = L // 128
        self.NQ = L // 512
        self.nlayers = nlayers
        self.dbg = dbg
        self.nc = bass.Bass("TRN2", target_bir_lowering=False)
        nc = self.nc
        self.inputs = {}

        NT, NQ = self.NT, self.NQ
        self.shapes = {
            "x": ([L, D], F32), "w_in": ([2, 128, 8, 5712], F32), "w_iwrep": ([2, 128, 8, 512], F32),
            "bfg": ([2, 8, 1], F32), "w_a": ([2, 128, 4, D], F32), "w_b": ([2, 128, 4, D], F32),
            "w_o": ([2, 128, 8, D], F32), "lnp": ([2, 128, 4, 8], F32),
            "w_f1": ([2, 128, 8, 2 * DFF], F32), "w_f2": ([2, 128, 22, D], F32),
            "c_ident": ([128, 128], F32), "c_trimask": ([128, 128], F32),
            "c_causT": ([128, 4, 512], F32), "c_targ": ([128, NT], F32),
            "c_kbias": ([128, NT, 8], F32), "c_kbd": ([128, NT, 8], F32), "c_negslope": ([128, 8, 128], F32), "c_tpos": ([128, L], F32),
        }
        self.scr_shapes = {
            "out": ([L, D], F32),
            "xres": ([128, 8, L], F32), "xbf": ([128, 8, L], BF16), "oa": ([128, 4, L], BF16),
            "ob": ([128, 4, L], BF16), "cspl": ([8, 3, L], BF16), "mskd": ([NQ, 128, NT, 512], U8),
        }
        self._aps = {}

    def __getattr__(self, name):
        d = self.__dict__
        if 'shapes' in d and name in d['shapes']:
            if name not in d['_aps']:
                shp, dt = d['shapes'][name]
                d['_aps'][name] = d['nc'].dram_tensor(name, list(shp), dt, kind="ExternalInput").ap()
                d['inputs'][name] = (tuple(shp), dt)
            return d['_aps'][name]
        if 'scr_shapes' in d and name in d['scr_shapes']:
            if name not in d['_aps']:
                shp, dt = d['scr_shapes'][name]
                kind = "ExternalOutput" if (name in d['dbg'] or name == "out") else "Internal"
                d['_aps'][name] = d['nc'].dram_tensor(name, list(shp), dt, kind=kind).ap()
            return d['_aps'][name]
        raise AttributeError(name)

    def T(self, es, name, shape, dt):
        self._uid = getattr(self, '_uid', 0) + 1
        return es.enter_context(self.nc.sbuf_tensor("%s_%d" % (name, self._uid), list(shape), dt))

    def PS(self, es, n=8):
        self._uid = getattr(self, '_uid', 0) + 1
        return [es.enter_context(self.nc.psum_tensor("ps%d_%d" % (i, self._uid), [128, 512], F32)) for i in range(n)]

    def load_w(self, S, dst, src, C, N, stage, tag, col0=0):
        engs = ['dve', 'act']
        step = stage[0].shape[1]
        for c in range(C):
            for n0 in range(0, N, step):
                n1 = min(N, n0 + step)
                i = self._ldi
                self._ldi += 1
                sl = i % 2
                st = stage[sl]
                S.dma(st[:, 0:n1 - n0], src[:, c, col0 + n0:col0 + n1], reads=[tag + 'src'], writes=[('stage', sl)])
                e = engs[i % 2]
                d_ap = dst[:, c, n0:n1]
                s_ap = st[:, 0:n1 - n0]
                S.op(e, 'copy' if e == 'act' else 'tensor_copy', dict(out=d_ap, in_=s_ap),
                     reads=[('stage', sl)], writes=[tag])

    def phase0(self, S):
        nc, L, NT, NQ = self.nc, self.L, self.NT, self.NQ
        with ExitStack() as es:
            ident = self.T(es, "ident", [128, 128], F32)
            xin = [self.T(es, "xin%d" % i, [128, D], F32) for i in range(2)]
            xTf = [self.T(es, "xTf%d" % i, [128, 8, 512], F32) for i in range(2)]
            xTb = [self.T(es, "xTb%d" % i, [128, 8, 512], BF16) for i in range(2)]
            import os
            NB = int(os.environ.get('P0_NB', '8'))
            ps = self.PS(es, NB)
            block = es.enter_context(nc.Block())
            S.dma(ident[:, :], self.c_ident[:, :], writes=['ident'])
            for Q in range(NQ):
                s = Q % 2
                for tt in range(4):
                    tile = Q * 4 + tt
                    sl = tile % 2
                    S.dma(xin[sl][:, :], self.x[tile * 128:(tile + 1) * 128, :], writes=[('xin', sl)])
                    for half in range(2):
                        b = (tile * 2 + half) % NB
                        for cc in range(4):
                            c = half * 4 + cc
                            o_ap = ps[b][:, cc * 128:(cc + 1) * 128]
                            i_ap = xin[sl][:, c * 128:(c + 1) * 128]
                            S.op('pe', 'transpose', dict(out=o_ap, in_=i_ap, identity=ident[:, :]),
                                 reads=[('xin', sl), 'ident'], writes=[('ps', b)])
                        src = ps[b][:, :].rearrange("p (a b) -> p a b", a=4)
                        d1 = xTf[s][:, half * 4:(half + 1) * 4, tt * 128:(tt + 1) * 128]
                        d2 = xTb[s][:, half * 4:(half + 1) * 4, tt * 128:(tt + 1) * 128]
                        MODE = os.environ.get('P0_MODE', 'ad')
                        if 'a' in MODE:
                            S.op('act', 'copy', dict(out=d1, in_=src),
                                 writes=[('ps', b), ('xTf', s, tt, half)])
                        if 'd' in MODE:
                            S.op('dve', 'tensor_copy', dict(out=d2, in_=src),
                                 writes=[('ps', b), ('xTb', s, tt, half)])
                rk = [('xTf', s, tt, h) for tt in range(4) for h in range(2)]
                S.dma(self.xres[:, :, Q * 512:(Q + 1) * 512], xTf[s][:, :, :], reads=rk, writes=[('xres', Q)])
                rk = [('xTb', s, tt, h) for tt in range(4) for h in range(2)]
                S.dma(self.xbf[:, :, Q * 512:(Q + 1) * 512], xTb[s][:, :, :], reads=rk, writes=[('xbf', Q)])
            S.end_phase(block)

    def emit_attention(self, S, ps, PT, PTm, items, Dp=4, sbanks=(0, 1, 2, 6, 7)):
        n = len(items)
        nS = len(sbanks)
        nP = len(PT)
        deferred = []
        for i in range(n + Dp):
            if i < n:
                it = items[i]
                sb = sbanks[i % nS]
                n0 = it['n0']
                nq = len(it['qk'])
                for j, (lh, rh, c0, c1, rk) in enumerate(it['qk']):
                    o_ap = ps[sb][:, c0:c1]
                    S.op('pe', 'matmul', dict(
                        out=o_ap, lhsT=lh, rhs=rh, start=(j == 0), stop=(j == nq - 1)),
                        reads=rk, writes=[('ps', sb)])
                pt = PT[i % nP]
                o_ap = pt[:, n0:512]
                i_ap = ps[sb][:, n0:512]
                b_ap = it['bias']
                S.op('act', 'activation', dict(
                    out=o_ap, in_=i_ap, func=AF.Exp, bias=b_ap, scale=1.0),
                    reads=it['bias_keys'], writes=[('ps', sb), ('pt', i % nP)])
                if it.get('mask') is not None:
                    m_ap, mkey = it['mask']
                    o2 = PTm[i % nP][:, n0:512]
                    S.op('dve', 'tensor_tensor', dict(
                        out=o2, in0=o_ap, in1=m_ap, op=ALU.mult),
                        reads=[('pt', i % nP), mkey], writes=[('ptm', i % nP)])
            k = i - Dp
            if k >= 0:
                it = items[k]
                n0 = it['n0']
                masked = it.get('mask') is not None
                rhs = (PTm if masked else PT)[k % nP][:, n0:512]
                ob = it['obank']
                o_ap = ps[ob][0:it['M'], n0:512]
                lh = it['pv_lhsT']
                S.op('pe', 'matmul', dict(
                    out=o_ap, lhsT=lh, rhs=rhs, start=it['first'], stop=it['last']),
                    reads=[('ptm' if masked else 'pt', k % nP)] + it['v_keys'], writes=[('ps', ob)])
                if it['last']:
                    it['normA']()
                    deferred.append((i + 2, it['normB']))
            while deferred and deferred[0][0] <= i:
                deferred.pop(0)[1]()
        for _, fn in deferred:
            fn()

    def make_norm(self, S, ps, ob, odd, rs, rs2, rinv, bcs, onesf, oT_ap_fn, okey):
        sr = 32 if odd else 64
        p0 = 64 if odd else 0

        def normA():
            S.op('dve', 'tensor_scalar', dict(out=rs[sr:sr + 1, :], in0=ps[ob][sr:sr + 1, :], scalar1=1e-30,
                                                  scalar2=None, op0=ALU.max), writes=[('ps', ob), 'rs'])
            S.op('dve', 'reciprocal', dict(out=rinv[sr:sr + 1, :], in_=rs[sr:sr + 1, :]), reads=['rs'], writes=['rinv'])

        def normB():
            S.op('pe', 'matmul', dict(out=ps[5][:, :], lhsT=onesf[sr:sr + 1, 0:128], rhs=rinv[sr:sr + 1, :],
                                          start=True, stop=True), reads=['rinv', 'onesf'], writes=[('ps', 5)])
            S.op('act', 'copy', dict(out=bcs[:, :], in_=ps[5][:, :]), writes=[('ps', 5), 'bcs'])
            S.op('dve', 'tensor_tensor', dict(out=oT_ap_fn(p0), in0=ps[ob][p0:p0 + 64, :], in1=bcs[p0:p0 + 64, :],
                                                  op=ALU.mult), reads=['bcs'], writes=[('ps', ob), okey])
        return normA, normB

    def phase_fox(self, S, l):
        nc, L, NT, NQ = self.nc, self.L, self.NT, self.NQ
        with ExitStack() as es:
            T = lambda name, shape, dt: self.T(es, name, shape, dt)
            xT = T("xT", [128, 8, L], BF16)
            wq = T("wq", [128, 8, 512], BF16)
            wk = T("wk", [128, 8, 512], BF16)
            wv = T("wv", [128, 8, 512], BF16)
            wf = T("wf", [128, 8, 8], BF16)
            stage = [T("stg%d" % i, [128, 1024], F32) for i in range(2)]
            Vb = T("Vb", [128, NT, 4, 160], BF16)
            qa = [T("qa%d" % i, [67, L], BF16) for i in range(2)]
            ka = [T("ka%d" % i, [67, L], BF16) for i in range(2)]
            negc = T("negc", [128, NT, 8], F32)
            identf = T("identf", [128, 128], F32)
            identb = T("identb", [128, 128], BF16)
            trim = T("trim", [128, 128], BF16)
            onesf = T("onesf", [128, 512], F32)
            bcol = T("bcol", [8, 2], F32)
            e8 = T("e8", [8, 512], F32)
            sp8 = T("sp8", [8, 512], F32)
            cb = [T("cb%d" % i, [8, 512], F32) for i in range(2)]
            r8 = e8
            r9 = sp8
            c3 = [T("c3%d" % i, [8, 3, 512], BF16) for i in range(2)]
            PT = [T("pt%d" % i, [128, 512], BF16) for i in range(6)]
            rs = T("rs", [128, 512], F32)
            rs2 = None
            rinv = T("rinv", [128, 512], F32)
            bcs = T("bcs", [128, 512], F32)
            oT = T("oT", [128, L], BF16)
            ps = self.PS(es)
            block = es.enter_context(nc.Block())

            for Q in range(NQ):
                S.dma(xT[:, :, Q * 512:(Q + 1) * 512], self.xbf[:, :, Q * 512:(Q + 1) * 512],
                      reads=[('xbf', Q)], writes=[('xT', Q)])
            S.dma(identf[:, :], self.c_ident[:, :], writes=['identf'])
            S.dma(stage[0][:, 0:128], self.c_trimask[:, :], writes=[('stage', 0)])
            S.op('dve', 'tensor_copy', dict(out=trim[:, :], in_=stage[0][:, 0:128]), reads=[('stage', 0)], writes=['trim'])
            S.op('dve', 'tensor_copy', dict(out=identb[:, :], in_=identf[:, :]), reads=['identf'], writes=['identb'])
            S.op('pool', 'memset', dict(ap=onesf[:, :], constant=1.0), writes=['onesf'])
            S.op('pool', 'memset', dict(ap=Vb[:, :, :, :], constant=0.0), writes=['Vb0'])
            S.op('pool', 'memset', dict(ap=Vb[:, :, :, 64:65], constant=1.0), reads=['Vb0'], writes=['Vb1'])
            for i in range(2):
                S.op('pool', 'memset', dict(ap=ka[i][64:67, :], constant=1.0), writes=[('kac', i)])
            S.dma(bcol[:, 0:1], self.bfg[l, :, :], writes=['bcol0'])
            S.op('dve', 'tensor_scalar', dict(out=bcol[:, 1:2], in0=bcol[:, 0:1], scalar1=-1.0, scalar2=None,
                                                  op0=ALU.mult), reads=['bcol0'], writes=['bcol1'])
            win = self.w_in[l]
            self.load_w(S, wq, win, 8, 512, stage, 'wq', col0=0)
            self.load_w(S, wk, win, 8, 512, stage, 'wk', col0=512)
            self.load_w(S, wv, win, 8, 512, stage, 'wv', col0=1024)
            self.load_w(S, wf, win, 8, 8, stage, 'wf', col0=1536)

            for Q in range(NQ):
                s = Q % 2
                qs = slice(Q * 512, (Q + 1) * 512)
                for c in range(8):
                    S.op('pe', 'matmul', dict(out=ps[6][0:8, :], lhsT=wf[:, c, :], rhs=xT[:, c, qs],
                                                             start=(c == 0), stop=(c == 7)),
                         reads=['wf', ('xT', Q)], writes=[('ps', 6)])
                S.op('act', 'activation', dict(out=e8[:, :], in_=ps[6][0:8, :], func=AF.Exp, bias=bcol[:, 1:2], scale=-1.0),
                     reads=['bcol1'], writes=[('ps', 6), 'e8'])
                S.op('act', 'activation', dict(out=sp8[:, :], in_=e8[:, :], func=AF.Ln, bias=onesf[0:8, 0:1], scale=1.0),
                     reads=['e8', 'onesf'], writes=['sp8'])
                if Q == 0:
                    init = 0.0
                else:
                    init = cb[1 - s][:, 511:512]
                S.op('dve', 'tensor_tensor_scan', dict(out=cb[s][:, :], data0=onesf[0:8, :], data1=sp8[:, :],
                                                                           initial=init, op0=ALU.mult, op1=ALU.subtract),
                     reads=['sp8', 'onesf', ('cb', 1 - s)], writes=[('cb', s)])
                S.op('dve', 'tensor_copy', dict(out=c3[s][:, 0, :], in_=cb[s][:, :]), reads=[('cb', s)], writes=[('c3a', s)])
                S.op('dve', 'tensor_tensor', dict(out=r8[:, :], in0=cb[s][:, :], in1=c3[s][:, 0, :], op=ALU.subtract),
                     reads=[('cb', s), ('c3a', s)], writes=['e8'])
                S.op('dve', 'tensor_copy', dict(out=c3[s][:, 1, :], in_=r8[:, :]), reads=['e8'], writes=[('c3b', s)])
                S.op('dve', 'tensor_tensor', dict(out=r9[:, :], in0=r8[:, :], in1=c3[s][:, 1, :], op=ALU.subtract),
                     reads=['e8', ('c3b', s)], writes=['sp8'])
                S.op('dve', 'tensor_copy', dict(out=c3[s][:, 2, :], in_=r9[:, :]), reads=['sp8'], writes=[('c3c', s)])
                S.dma(self.cspl[:, :, qs], c3[s][:, :, :], reads=[('c3a', s), ('c3b', s), ('c3c', s)], writes=[('cspl', Q)])
                for tt in range(4):
                    tile = Q * 4 + tt
                    S.op('pe', 'transpose', dict(out=ps[7][:, tile * 8:(tile + 1) * 8],
                                                                           in_=cb[s][:, tt * 128:(tt + 1) * 128],
                                                                           identity=identf[0:8, 0:8]),
                         reads=[('cb', s), 'identf'], writes=[('ps', 7)])
            S.op('dve', 'tensor_scalar', dict(out=negc[:, :, :].rearrange("p a b -> p (a b)"), in0=ps[7][:, 0:NT * 8],
                                                  scalar1=-1.0, scalar2=None, op0=ALU.mult),
                 writes=[('ps', 7), 'negc'])

            for tile in range(NT):
                b = 6 + tile % 2
                ts_ = slice(tile * 128, (tile + 1) * 128)
                for c in range(8):
                    S.op('pe', 'matmul', dict(out=ps[b][:, :], lhsT=xT[:, c, ts_], rhs=wv[:, c, :],
                                                                   start=(c == 0), stop=(c == 7)),
                         reads=['wv', ('xT', tile // 4)], writes=[('ps', b)])
                src = ps[b][:, :].rearrange("p (a b d) -> p a b d", a=4, b=2)
                S.op('act', 'copy', dict(out=Vb[:, tile, :, 0:64], in_=src[:, :, 0, :]),
                     reads=['Vb1'], writes=[('ps', b), ('V', tile, 0)])
                S.op('dve', 'tensor_copy', dict(out=Vb[:, tile, :, 96:160], in_=src[:, :, 1, :]),
                     reads=['Vb1'], writes=[('ps', b), ('V', tile, 1)])

            for h in range(8):
                hb = h % 2
                pr = h // 2
                odd = (h % 2 == 1)
                hs = slice(h * 64, (h + 1) * 64)
                S.dma(qa[hb][64:67, :], self.cspl[h, :, :], reads=[('cspl', Q) for Q in range(NQ)], writes=[('qac', hb)])
                for Q in range(NQ):
                    qs = slice(Q * 512, (Q + 1) * 512)
                    for c in range(8):
                        S.op('pe', 'matmul', dict(out=ps[6][0:64, :], lhsT=wq[:, c, hs], rhs=xT[:, c, qs],
                                                                 start=(c == 0), stop=(c == 7)),
                             reads=['wq', ('xT', Q)], writes=[('ps', 6)])
                    S.op('dve', 'tensor_scalar', dict(out=qa[hb][0:64, qs], in0=ps[6][0:64, :], scalar1=0.125,
                                                                scalar2=None, op0=ALU.mult),
                         writes=[('ps', 6), ('qa', hb, Q)])
                    for c in range(8):
                        S.op('pe', 'matmul', dict(out=ps[7][0:64, :], lhsT=wk[:, c, hs], rhs=xT[:, c, qs],
                                                                 start=(c == 0), stop=(c == 7)),
                             reads=['wk', ('xT', Q)], writes=[('ps', 7)])
                    S.op('dve', 'tensor_copy', dict(out=ka[hb][0:64, qs], in_=ps[7][0:64, :]),
                         writes=[('ps', 7), ('ka', hb, Q)])
                items = []
                for Q in range(NQ):
                    ob = 3 + Q % 2
                    qs = slice(Q * 512, (Q + 1) * 512)
                    nA, nB = self.make_norm(S, ps, ob, odd, rs, rs2, rinv, bcs, onesf,
                                            lambda p0, qs=qs: oT[p0:p0 + 64, qs], ('oT', Q))
                    for kb in range(4 * Q + 4):
                        j = kb - 4 * Q
                        n0 = 128 * j if j > 0 else 0
                        ks = slice(kb * 128, (kb + 1) * 128)
                        qk = [(ka[hb][0:67, ks], qa[hb][0:67, Q * 512 + n0:(Q + 1) * 512], n0, 512,
                               [('qa', hb, Q), ('qac', hb), ('ka', hb, kb // 4), ('kac', hb)])]
                        if j >= 0:
                            qk.append((identb[:, :], trim[:, :], n0, n0 + 128, ['identb', 'trim']))
                        if odd:
                            lh, M = Vb[:, kb, pr, 32:160], 128
                        else:
                            lh, M = Vb[:, kb, pr, 0:128], 128
                        items.append(dict(qk=qk, n0=n0, bias=negc[:, kb, h:h + 1], bias_keys=['negc'],
                                          pv_lhsT=lh, M=M, obank=ob, first=(kb == 0), last=(kb == 4 * Q + 3),
                                          v_keys=[('V', kb, 0), ('V', kb, 1), 'Vb1'], normA=nA, normB=nB))
                self.emit_attention(S, ps, PT, None, items)
                if odd:
                    S.dma(self.oa[:, pr, :], oT[:, :], reads=[('oT', Q) for Q in range(NQ)], writes=[('oa', pr)])
            S.end_phase(block)

    def phase_idx(self, S, l):
        nc, L, NT, NQ = self.nc, self.L, self.NT, self.NQ
        topk = min(TOPK, L // 4)
        with ExitStack() as es:
            T = lambda name, shape, dt: self.T(es, name, shape, dt)
            wik2 = T("wik2", [128, 8, 128], BF16)
            wiq = T("wiq", [128, 8, 512], BF16)
            wiw = T("wiw", [128, 8, 8], BF16)
            stage = [T("stg%d" % i, [128, 2048], F32) for i in range(2)]
            ikT = T("ikT", [128, L], BF16)
            xs = [T("xs%d" % i, [128, 8, 512], BF16) for i in range(2)]
            sc = T("sc", [128, 4, L], F32)
            mkq = T("mkq", [128, 4, L], BF16)
            junk = T("junk", [128, L], BF16)
            iqT = T("iqT", [128, 4, 512], BF16)
            rl = [T("rl%d" % i, [128, 512], F32) for i in range(4)]
            wtk = T("wtk", [128, 4, 8], F32)
            caus = T("caus", [128, 4, 512], F32)
            targ = T("targ", [128, NT], F32)
            lo = T("lo", [128, 4], F32)
            hi = T("hi", [128, 4], F32)
            step = T("step", [128, 4], F32)
            thr = T("thr", [128, 4], F32)
            cnt = T("cnt", [128, 4], F32)
            ge = T("ge", [128, 4], F32)
            identb = T("identb", [128, 128], BF16)
            mk = T("mk", [128, NT, 512], U8)
            ps = self.PS(es, 6)
            self._uid += 1
            pstb = [es.enter_context(nc.psum_tensor("pst%d_%d" % (i, self._uid), [128, 1024], BF16)) for i in range(2)]
            block = es.enter_context(nc.Block())

            win = self.w_in[l]
            S.dma(caus[:, :, :], self.c_causT[:, :, :], writes=['caus'])
            S.dma(targ[:, :], self.c_targ[:, :], writes=['targ'])
            S.dma(stage[0][:, 0:128], self.c_ident[:, :], writes=[('stage', 0)])
            S.op('dve', 'tensor_copy', dict(out=identb[:, :], in_=stage[0][:, 0:128]), reads=[('stage', 0)], writes=['identb'])
            self.load_w(S, wik2[:, :, 0:64], win, 8, 64, stage, 'wik', col0=3592)
            S.op('dve', 'tensor_copy', dict(out=wik2[:, :, 64:128], in_=wik2[:, :, 0:64]), reads=['wik'], writes=['wik2'])
            self.load_w(S, wiq, win, 8, 512, stage, 'wiq', col0=3080)
            self.load_w(S, wiw, win, 8, 8, stage, 'wiw', col0=3656)

            xcnt = [0]

            def load_xs(Q):
                i = xcnt[0] % 2
                xcnt[0] += 1
                S.dma(xs[i][:, :, :], self.xbf[:, :, Q * 512:(Q + 1) * 512], reads=[('xbf', Q)], writes=[('xs', i)])
                return i

            for Q in range(NQ):
                i = load_xs(Q)
                for c in range(8):
                    S.op('pe', 'matmul', dict(out=ps[4][:, :], lhsT=wik2[:, c, :], rhs=xs[i][:, c, :], start=(c == 0), stop=(c == 7)),
                         reads=['wik', 'wik2', ('xs', i)], writes=[('ps', 4)])
                S.op('act', 'copy', dict(out=ikT[:, Q * 512:(Q + 1) * 512], in_=ps[4][:, :]), writes=[('ps', 4), ('ikT', Q)])

            rcnt = 0
            for Q in range(NQ):
                i = load_xs(Q)
                nk = (Q + 1) * 512
                nkb = 4 * Q + 4
                for pr in range(4):
                    ms = slice(pr * 128, (pr + 1) * 128)
                    for c in range(8):
                        S.op('pe', 'matmul', dict(out=ps[4][:, :], lhsT=wiq[:, c, ms], rhs=xs[i][:, c, :], start=(c == 0), stop=(c == 7)),
                             reads=['wiq', ('xs', i)], writes=[('ps', 4)])
                    S.op('act', 'copy', dict(out=iqT[:, pr, :], in_=ps[4][:, :]), writes=[('ps', 4), ('iqT', pr)])
                for qsub in range(4):
                    for c in range(8):
                        S.op('pe', 'matmul', dict(out=ps[5][:, qsub * 8:(qsub + 1) * 8], lhsT=xs[i][:, c, qsub * 128:(qsub + 1) * 128],
                                                  rhs=wiw[:, c, :], start=(c == 0), stop=(c == 7)),
                             reads=['wiw', ('xs', i)], writes=[('ps', 5)])
                S.op('dve', 'tensor_scalar', dict(out=wtk[:, :, :].rearrange("p a b -> p (a b)"), in0=ps[5][:, 0:32], scalar1=IDX_SCALE,
                                                  scalar2=None, op0=ALU.mult), writes=[('ps', 5), 'wtk'])
                for kc in range(Q + 1):
                    ksl = slice(kc * 512, (kc + 1) * 512)
                    for h in range(8):
                        pr, half = h // 2, h % 2
                        rows = slice(half * 64, half * 64 + 64)
                        for qsub in range(4):
                            rb = rcnt % 4
                            rcnt += 1
                            S.op('pe', 'matmul', dict(out=ps[rb][:, :], lhsT=iqT[rows, pr, qsub * 128:(qsub + 1) * 128], rhs=ikT[rows, ksl],
                                                      start=True, stop=True),
                                 reads=[('ikT', kc), ('iqT', pr)], writes=[('ps', rb)])
                            S.op('act', 'activation', dict(out=rl[rb][:, :], in_=ps[rb][:, :], func=AF.Relu),
                                 writes=[('ps', rb), ('rl', rb)])
                            if h == 0:
                                S.op('dve', 'tensor_scalar', dict(out=sc[:, qsub, ksl], in0=rl[rb][:, :], scalar1=wtk[:, qsub, h:h + 1],
                                                                  scalar2=None, op0=ALU.mult),
                                     reads=[('rl', rb), 'wtk'], writes=[('sc', qsub, kc)])
                            else:
                                S.op('dve', 'scalar_tensor_tensor', dict(out=sc[:, qsub, ksl], in0=rl[rb][:, :], scalar=wtk[:, qsub, h:h + 1],
                                                                         in1=sc[:, qsub, ksl], op0=ALU.mult, op1=ALU.add),
                                     reads=[('rl', rb), 'wtk', ('sc', qsub, kc)], writes=[('sc', qsub, kc)])
                allk = lambda qsub: [('sc', qsub, kc) for kc in range(Q + 1)]
                for qsub in range(4):
                    S.op('dve', 'tensor_reduce', dict(out=lo[:, qsub:qsub + 1], in_=sc[:, qsub, 0:nk], axis=mybir.AxisListType.X, op=ALU.min),
                         reads=allk(qsub), writes=[('lo', qsub)])
                for qsub in range(4):
                    S.op('pool', 'tensor_tensor', dict(out=sc[:, qsub, Q * 512:nk], in0=sc[:, qsub, Q * 512:nk], in1=caus[:, qsub, :], op=ALU.add),
                         reads=[('sc', qsub, Q), 'caus', ('lo', qsub)], writes=[('sc', qsub, Q)])
                for qsub in range(4):
                    S.op('dve', 'tensor_reduce', dict(out=hi[:, qsub:qsub + 1], in_=sc[:, qsub, 0:nk], axis=mybir.AxisListType.X, op=ALU.max),
                         reads=allk(qsub), writes=[('hi', qsub)])
                S.op('dve', 'tensor_tensor', dict(out=step[:, :], in0=hi[:, :], in1=lo[:, :], op=ALU.subtract),
                     reads=[('hi', q) for q in range(4)] + [('lo', q) for q in range(4)], writes=['step'])
                for itn in range(BIS_N):
                    S.op('dve', 'tensor_scalar', dict(out=step[:, :], in0=step[:, :], scalar1=0.5, scalar2=None, op0=ALU.mult),
                         reads=['step'], writes=['step'])
                    S.op('dve', 'tensor_tensor', dict(out=thr[:, :], in0=lo[:, :], in1=step[:, :], op=ALU.add),
                         reads=['step'] + [('lo', q) for q in range(4)], writes=['thr'])
                    for qsub in range(4):
                        S.op('dve', 'tensor_scalar', dict(out=junk[:, 0:nk], in0=sc[:, qsub, 0:nk], scalar1=thr[:, qsub:qsub + 1], scalar2=0.0,
                                                          op0=ALU.is_ge, op1=ALU.add, accum_out=cnt[:, qsub:qsub + 1]),
                             reads=allk(qsub) + ['thr'], writes=['junk', ('cnt', qsub)])
                    S.op('dve', 'tensor_tensor', dict(out=ge[:, :], in0=cnt[:, :], in1=targ[:, 4 * Q:4 * Q + 4], op=ALU.is_ge),
                         reads=[('cnt', q) for q in range(4)] + ['targ'], writes=['ge'])
                    S.op('dve', 'tensor_tensor', dict(out=ge[:, :], in0=ge[:, :], in1=step[:, :], op=ALU.mult),
                         reads=['ge', 'step'], writes=['ge'])
                    S.op('dve', 'tensor_tensor', dict(out=lo[:, :], in0=lo[:, :], in1=ge[:, :], op=ALU.add),
                         reads=['ge'] + [('lo', q) for q in range(4)], writes=[('lo', q) for q in range(4)])
                for qsub in range(4):
                    S.op('dve', 'tensor_scalar', dict(out=mkq[:, qsub, 0:nk], in0=sc[:, qsub, 0:nk], scalar1=lo[:, qsub:qsub + 1], scalar2=None,
                                                      op0=ALU.is_ge),
                         reads=allk(qsub) + [('lo', qsub)], writes=[('mkq', qsub)])
                for kb in range(nkb):
                    pb = kb % 2
                    for qsub in range(4):
                        S.op('pe', 'transpose', dict(out=pstb[pb][:, qsub * 128:(qsub + 1) * 128],
                                                     in_=mkq[:, qsub, kb * 128:(kb + 1) * 128], identity=identb[:, :]),
                             reads=[('mkq', qsub), 'identb'], writes=[('pst', pb)])
                    S.op('act', 'copy', dict(out=mk[:, kb, :], in_=pstb[pb][:, 0:512]), writes=[('pst', pb), ('mk', kb)])
                S.dma(self.mskd[Q, :, 0:nkb, :], mk[:, 0:nkb, :], reads=[('mk', kb) for kb in range(nkb)], writes=[('mskd', Q)])
            S.end_phase(block)

    def phase_dsa(self, S, l):
        nc, L, NT, NQ = self.nc, self.L, self.NT, self.NQ
        with ExitStack() as es:
            T = lambda name, shape, dt: self.T(es, name, shape, dt)
            wdq = T("wdq", [128, 8, 512], BF16)
            wdk = T("wdk", [128, 8, 512], BF16)
            wdv = T("wdv", [128, 8, 512], BF16)
            stage = [T("stg%d" % i, [128, 2048], F32) for i in range(2)]
            xs = [T("xs%d" % i, [128, 8, 512], BF16) for i in range(2)]
            kT = T("kT", [128, 4, L], BF16)
            Vb = T("Vb", [128, NT, 4, 160], BF16)
            qd = T("qd", [128, 4, 512], BF16)
            mk = T("mk", [128, NT, 512], U8)
            kbias = T("kbias", [128, NT, 8], F32)
            kbd = T("kbd", [128, NT, 8], F32)
            kbq = T("kbq", [128, NT, 8], F32)
            nsl = T("nsl", [128, 8, 128], BF16)
            tpq = T("tpq", [128, 512], BF16)
            PT = [T("pt%d" % i, [128, 512], BF16) for i in range(6)]
            PTm = [T("ptm%d" % i, [128, 512], BF16) for i in range(6)]
            rs = T("rs", [128, 512], F32)
            rs2 = None
            rinv = T("rinv", [128, 512], F32)
            bcs = T("bcs", [128, 512], F32)
            onesf = T("onesf", [128, 128], F32)
            oTq = [T("oTq%d" % i, [128, 4, 512], BF16) for i in range(2)]
            identb = T("identb", [128, 128], BF16)
            trim = T("trim", [128, 128], BF16)
            ps = self.PS(es)
            block = es.enter_context(nc.Block())
            S.dma(stage[1][:, 0:128], self.c_trimask[:, :], writes=[('stage', 1)])
            S.op('dve', 'tensor_copy', dict(out=trim[:, :], in_=stage[1][:, 0:128]), reads=[('stage', 1)], writes=['trim'])
            S.dma(stage[1][:, 128:256], self.c_ident[:, :], reads=['trim'], writes=[('stage', 1)])
            S.op('dve', 'tensor_copy', dict(out=identb[:, :], in_=stage[1][:, 128:256]), reads=[('stage', 1)], writes=['identb'])

            win = self.w_in[l]
            S.dma(kbias[:, :, :], self.c_kbias[:, :, :], writes=['kbias'])
            S.dma(kbd[:, :, :], self.c_kbd[:, :, :], writes=['kbd'])
            S.dma(stage[0][:, 0:1024], self.c_negslope[:, :, :].rearrange("p a b -> p (a b)"), writes=[('stage', 0)])
            S.op('dve', 'tensor_copy', dict(out=nsl[:, :, :].rearrange("p a b -> p (a b)"), in_=stage[0][:, 0:1024]),
                 reads=[('stage', 0)], writes=['nsl'])
            S.op('pool', 'memset', dict(ap=onesf[:, :], constant=1.0), writes=['onesf'])
            S.op('pool', 'memset', dict(ap=Vb[:, :, :, :], constant=0.0), writes=['Vb0'])
            S.op('pool', 'memset', dict(ap=Vb[:, :, :, 64:65], constant=1.0), reads=['Vb0'], writes=['Vb1'])
            self.load_w(S, wdq, win, 8, 512, stage, 'wdq', col0=1544)
            self.load_w(S, wdk, win, 8, 512, stage, 'wdk', col0=2056)
            self.load_w(S, wdv, win, 8, 512, stage, 'wdv', col0=2568)
            xcnt = [0]

            def load_xs(Q):
                i = xcnt[0] % 2
                xcnt[0] += 1
                S.dma(xs[i][:, :, :], self.xbf[:, :, Q * 512:(Q + 1) * 512], reads=[('xbf', Q)], writes=[('xs', i)])
                return i

            for Q in range(NQ):
                i = load_xs(Q)
                qs = slice(Q * 512, (Q + 1) * 512)
                for pr in range(4):
                    ms = slice(pr * 128, (pr + 1) * 128)
                    for c in range(8):
                        S.op('pe', 'matmul', dict(out=ps[6][:, :], lhsT=wdk[:, c, ms], rhs=xs[i][:, c, :], start=(c == 0), stop=(c == 7)),
                             reads=['wdk', ('xs', i)], writes=[('ps', 6)])
                    S.op('dve', 'tensor_copy', dict(out=kT[:, pr, qs], in_=ps[6][:, :]), writes=[('ps', 6), ('kT', Q)])
                for tt in range(4):
                    tile = Q * 4 + tt
                    for c in range(8):
                        S.op('pe', 'matmul', dict(out=ps[7][:, :], lhsT=xs[i][:, c, tt * 128:(tt + 1) * 128], rhs=wdv[:, c, :],
                                                  start=(c == 0), stop=(c == 7)),
                             reads=['wdv', ('xs', i)], writes=[('ps', 7)])
                    src = ps[7][:, :].rearrange("p (a b d) -> p a b d", a=4, b=2)
                    S.op('act', 'copy', dict(out=Vb[:, tile, :, 0:64], in_=src[:, :, 0, :]), reads=['Vb1'], writes=[('ps', 7), ('V', tile, 0)])
                    S.op('dve', 'tensor_copy', dict(out=Vb[:, tile, :, 96:160], in_=src[:, :, 1, :]), reads=['Vb1'],
                         writes=[('ps', 7), ('V', tile, 1)])

            for Q in range(NQ):
                i = load_xs(Q)
                nkb = 4 * Q + 4
                qs = slice(Q * 512, (Q + 1) * 512)
                S.dma(mk[:, 0:nkb, :], self.mskd[Q, :, 0:nkb, :], reads=[('mskd', Q)], writes=['mk'])
                S.dma(stage[1][:, 0:512], self.c_tpos[:, qs], writes=[('stage', 1)])
                S.op('dve', 'tensor_copy', dict(out=tpq[:, :], in_=stage[1][:, 0:512]), reads=[('stage', 1)], writes=['tpq'])
                for pr in range(4):
                    ms = slice(pr * 128, (pr + 1) * 128)
                    for c in range(8):
                        S.op('pe', 'matmul', dict(out=ps[6][:, :], lhsT=wdq[:, c, ms], rhs=xs[i][:, c, :], start=(c == 0), stop=(c == 7)),
                             reads=['wdq', ('xs', i)], writes=[('ps', 6)])
                    S.op('dve', 'tensor_scalar', dict(out=qd[:, pr, :], in0=ps[6][:, :], scalar1=0.125, scalar2=None, op0=ALU.mult),
                         writes=[('ps', 6), ('qd', pr)])
                S.op('dve', 'scalar_tensor_tensor', dict(out=kbq[:, :, :].rearrange("p a b -> p (a b)"),
                                                         in0=kbd[:, :, :].rearrange("p a b -> p (a b)"), scalar=float(Q * 512 + 511),
                                                         in1=kbias[:, :, :].rearrange("p a b -> p (a b)"), op0=ALU.mult, op1=ALU.add),
                     reads=['kbias', 'kbd'], writes=['kbq'])
                items = []
                oq = oTq[Q % 2]
                for h in range(8):
                    half, pr, odd = h % 2, h // 2, (h % 2 == 1)
                    rows = slice(half * 64, half * 64 + 64)
                    r2 = slice(half * 64, half * 64 + 2)
                    ob = 3 + h % 2
                    nA, nB = self.make_norm(S, ps, ob, odd, rs, rs2, rinv, bcs, onesf,
                                            lambda p0, oq=oq, pr=pr: oq[p0:p0 + 64, pr, :], ('oTq', Q % 2, h))
                    for kb in range(nkb):
                        j = kb - 4 * Q
                        n0 = 128 * j if j > 0 else 0
                        ks = slice(kb * 128, (kb + 1) * 128)
                        qk = [(kT[rows, pr, ks], qd[rows, pr, n0:512], n0, 512, [('kT', kb // 4), ('qd', pr)])]
                        if h < 2:
                            qk.append((nsl[rows, h, :], tpq[rows, n0:512], n0, 512, ['nsl', 'tpq']))
                        if j >= 0:
                            qk.append((identb[:, :], trim[:, :], n0, n0 + 128, ['identb', 'trim']))
                        lh = Vb[:, kb, pr, 32:160] if odd else Vb[:, kb, pr, 0:128]
                        items.append(dict(qk=qk, n0=n0, bias=kbq[:, kb, h:h + 1], bias_keys=['kbq'],
                                          mask=(mk[:, kb, n0:512], 'mk'),
                                          pv_lhsT=lh, M=128, obank=ob, first=(kb == 0), last=(kb == nkb - 1),
                                          v_keys=[('V', kb, 0), ('V', kb, 1), 'Vb1'], normA=nA, normB=nB))
                self.emit_attention(S, ps, PT, PTm, items)
                S.dma(self.ob[:, :, qs], oq[:, :, :], reads=[('oTq', Q % 2, h) for h in range(8)], writes=[('ob', Q)])
            S.end_phase(block)

    def ln_block(self, S, ps, xr, sq, mean, lnv, rstd, lnp, gi, onesf, epst, outb, xkey):
        for m in range(8):
            S.op('pe', 'matmul', dict(out=ps[5][:, :], lhsT=onesf[:, :], rhs=xr[:, m, :], start=(m == 0), stop=(m == 7)),
                 reads=['onesf', (xkey, m)], writes=[('ps', 5)])
            S.op('act', 'activation', dict(out=sq[m % 2][:, :], in_=xr[:, m, :], func=AF.Square), reads=[(xkey, m)], writes=[('sq', m % 2)])
            S.op('pe', 'matmul', dict(out=ps[6][:, :], lhsT=onesf[:, :], rhs=sq[m % 2][:, :], start=(m == 0), stop=(m == 7)),
                 reads=['onesf', ('sq', m % 2)], writes=[('ps', 6)])
        S.op('dve', 'tensor_scalar', dict(out=mean[:, :], in0=ps[5][:, :], scalar1=1.0 / D, scalar2=None, op0=ALU.mult),
             writes=[('ps', 5), 'mean'])
        S.op('dve', 'tensor_tensor', dict(out=lnv[:, :], in0=mean[:, :], in1=mean[:, :], op=ALU.mult), reads=['mean'], writes=['lnv'])
        S.op('dve', 'scalar_tensor_tensor', dict(out=lnv[:, :], in0=ps[6][:, :], scalar=1.0 / D, in1=lnv[:, :], op0=ALU.mult, op1=ALU.subtract),
             reads=['lnv'], writes=[('ps', 6), 'lnv'])
        S.op('act', 'activation', dict(out=lnv[:, :], in_=lnv[:, :], func=AF.Ln, bias=epst[:, 0:1], scale=1.0), reads=['lnv', 'epst'], writes=['lnv'])
        S.op('act', 'activation', dict(out=rstd[:, :], in_=lnv[:, :], func=AF.Exp, scale=-0.5), reads=['lnv'], writes=['rstd'])
        for m in range(8):
            S.op('dve', 'tensor_tensor', dict(out=xr[:, m, :], in0=xr[:, m, :], in1=mean[:, :], op=ALU.subtract),
                 reads=[(xkey, m), 'mean'], writes=[(xkey, m)])
            S.op('dve', 'tensor_tensor', dict(out=xr[:, m, :], in0=xr[:, m, :], in1=rstd[:, :], op=ALU.mult),
                 reads=[(xkey, m), 'rstd'], writes=[(xkey, m)])
            S.op('dve', 'tensor_scalar', dict(out=xr[:, m, :], in0=xr[:, m, :], scalar1=lnp[:, gi, m:m + 1], scalar2=lnp[:, gi + 1, m:m + 1],
                                              op0=ALU.mult, op1=ALU.add),
                 reads=[(xkey, m), 'lnp'], writes=[(xkey, m)])
            if outb is not None:
                S.op('pool', 'tensor_copy', dict(out=outb[:, m, :], in_=xr[:, m, :]), reads=[(xkey, m)], writes=[('xob', m)])

    def phase_merge(self, S, l):
        nc, L, NT, NQ = self.nc, self.L, self.NT, self.NQ
        with ExitStack() as es:
            T = lambda name, shape, dt: self.T(es, name, shape, dt)
            wA = T("wA", [128, 4, D], BF16)
            wB = T("wB", [128, 4, D], BF16)
            wO = T("wO", [128, 8, D], BF16)
            wga = T("wga", [128, 8, D], BF16)
            wgb = T("wgb", [128, 8, D], BF16)
            stage = [T("stg%d" % i, [128, 2048], F32) for i in range(2)]
            lnp = T("lnp", [128, 4, 8], F32)
            oab = T("oab", [128, 4, 512], BF16)
            obb = T("obb", [128, 4, 512], BF16)
            xs = T("xs", [128, 8, 512], BF16)
            xr = T("xr", [128, 8, 512], F32)
            mg = T("mg", [128, 8, 512], BF16)
            sa = T("sa", [128, 512], F32)
            sb = T("sb", [128, 512], F32)
            sq = [T("sq%d" % i, [128, 512], F32) for i in range(2)]
            mean = T("mean", [128, 512], F32)
            lnv = T("lnv", [128, 512], F32)
            rstd = T("rstd", [128, 512], F32)
            x1b = T("x1b", [128, 8, 512], BF16)
            onesf = T("onesf", [128, 128], F32)
            epst = T("epst", [128, 1], F32)
            ps = self.PS(es)
            block = es.enter_context(nc.Block())
            S.op('pool', 'memset', dict(ap=onesf[:, :], constant=1.0), writes=['onesf'])
            S.op('pool', 'memset', dict(ap=epst[:, :], constant=LN_EPS), writes=['epst'])
            S.dma(lnp[:, :, :], self.lnp[l], writes=['lnp'])
            self.load_w(S, wA, self.w_a[l], 4, D, stage, 'wA')
            self.load_w(S, wB, self.w_b[l], 4, D, stage, 'wB')
            self.load_w(S, wO, self.w_o[l], 8, D, stage, 'wO')
            self.load_w(S, wga, self.w_in[l], 8, D, stage, 'wga', col0=3664)
            self.load_w(S, wgb, self.w_in[l], 8, D, stage, 'wgb', col0=4688)
            for Q in range(NQ):
                qs = slice(Q * 512, (Q + 1) * 512)
                S.dma(oab[:, :, :], self.oa[:, :, qs], reads=[('oa', p) for p in range(4)], writes=['oab'])
                S.dma(obb[:, :, :], self.ob[:, :, qs], reads=[('ob', Q)], writes=['obb'])
                S.dma(xs[:, :, :], self.xbf[:, :, qs], reads=[('xbf', Q)], writes=['xs'])
                S.dma(xr[:, :, :], self.xres[:, :, qs], reads=[('xres', Q)], writes=[('xr', m) for m in range(8)])
                for m in range(8):
                    ms = slice(m * 128, (m + 1) * 128)
                    for c in range(4):
                        S.op('pe', 'matmul', dict(out=ps[0][:, :], lhsT=wA[:, c, ms], rhs=oab[:, c, :], start=(c == 0), stop=(c == 3)),
                             reads=['wA', 'oab'], writes=[('ps', 0)])
                    for c in range(4):
                        S.op('pe', 'matmul', dict(out=ps[1][:, :], lhsT=wB[:, c, ms], rhs=obb[:, c, :], start=(c == 0), stop=(c == 3)),
                             reads=['wB', 'obb'], writes=[('ps', 1)])
                    for c in range(8):
                        S.op('pe', 'matmul', dict(out=ps[2][:, :], lhsT=wga[:, c, ms], rhs=xs[:, c, :], start=(c == 0), stop=(c == 7)),
                             reads=['wga', 'xs'], writes=[('ps', 2)])
                    for c in range(8):
                        S.op('pe', 'matmul', dict(out=ps[3][:, :], lhsT=wgb[:, c, ms], rhs=xs[:, c, :], start=(c == 0), stop=(c == 7)),
                             reads=['wgb', 'xs'], writes=[('ps', 3)])
                    S.op('act', 'activation', dict(out=sa[:, :], in_=ps[2][:, :], func=AF.Sigmoid), writes=[('ps', 2), 'sa'])
                    S.op('act', 'activation', dict(out=sb[:, :], in_=ps[3][:, :], func=AF.Sigmoid), writes=[('ps', 3), 'sb'])
                    S.op('dve', 'tensor_tensor', dict(out=sa[:, :], in0=sa[:, :], in1=ps[0][:, :], op=ALU.mult), reads=['sa'], writes=[('ps', 0), 'sa'])
                    S.op('dve', 'tensor_tensor', dict(out=sb[:, :], in0=sb[:, :], in1=ps[1][:, :], op=ALU.mult), reads=['sb'], writes=[('ps', 1), 'sb'])
                    S.op('pool', 'tensor_tensor', dict(out=mg[:, m, :], in0=sa[:, :], in1=sb[:, :], op=ALU.add), reads=['sa', 'sb'], writes=[('mg', m)])
                for m in range(8):
                    ms = slice(m * 128, (m + 1) * 128)
                    for c in range(8):
                        S.op('pe', 'matmul', dict(out=ps[4][:, :], lhsT=wO[:, c, ms], rhs=mg[:, c, :], start=(c == 0), stop=(c == 7)),
                             reads=['wO', ('mg', c)], writes=[('ps', 4)])
                    S.op('dve', 'scalar_tensor_tensor', dict(out=xr[:, m, :], in0=xr[:, m, :], scalar=ALPHA, in1=ps[4][:, :], op0=ALU.mult, op1=ALU.add),
                         reads=[('xr', m)], writes=[('ps', 4), ('xr', m)])
                self.ln_block(S, ps, xr, sq, mean, lnv, rstd, lnp, 0, onesf, epst, x1b, 'xr')
                S.dma(self.xres[:, :, qs], xr[:, :, :], reads=[('xr', m) for m in range(8)], writes=[('xres', Q)])
                S.dma(self.xbf[:, :, qs], x1b[:, :, :], reads=[('xob', m) for m in range(8)], writes=[('xbf', Q)])
            S.end_phase(block)

    def phase_ffn(self, S, l, last):
        nc, L, NT, NQ = self.nc, self.L, self.NT, self.NQ
        NJ = DFF // 128
        with ExitStack() as es:
            T = lambda name, shape, dt: self.T(es, name, shape, dt)
            w1 = T("w1", [128, 8, 2 * DFF], BF16)
            w2 = T("w2", [128, NJ, D], BF16)
            stage = [T("stg%d" % i, [128, 512], F32) for i in range(2)]
            lnp = T("lnp", [128, 4, 8], F32)
            xs = T("xs", [128, 8, 512], BF16)
            xr = T("xr", [128, 8, 512], F32)
            hT = T("hT", [128, NJ, 512], BF16)
            sq = [T("sq%d" % i, [128, 512], F32) for i in range(2)]
            mean = T("mean", [128, 512], F32)
            lnv = T("lnv", [128, 512], F32)
            rstd = T("rstd", [128, 512], F32)
            if last:
                outst = [T("outst%d" % i, [128, 512], F32) for i in range(4)]
                identf = T("identf", [128, 128], F32)
                x2b = None
            else:
                x2b = T("x2b", [128, 8, 512], BF16)
            onesf = T("onesf", [128, 128], F32)
            epst = T("epst", [128, 1], F32)
            ps = self.PS(es)
            block = es.enter_context(nc.Block())
            S.op('pool', 'memset', dict(ap=onesf[:, :], constant=1.0), writes=['onesf'])
            S.op('pool', 'memset', dict(ap=epst[:, :], constant=LN_EPS), writes=['epst'])
            S.dma(lnp[:, :, :], self.lnp[l], writes=['lnp'])
            if last:
                S.dma(identf[:, :], self.c_ident[:, :], writes=['identf'])
            engs = ['dve', 'act']
            for (dst, src, C, N, tag) in ((w1, self.w_f1[l], 8, 2 * DFF, 'w1'), (w2, self.w_f2[l], NJ, D, 'w2')):
                for c in range(C):
                    for n0 in range(0, N, 512):
                        i = self._ldi
                        self._ldi += 1
                        sl = i % 2
                        S.dma(stage[sl][:, :], src[:, c, n0:n0 + 512], writes=[('stage', sl)])
                        e = engs[i % 2]
                        S.op(e, 'copy' if e == 'act' else 'tensor_copy', dict(out=dst[:, c, n0:n0 + 512], in_=stage[sl][:, :]),
                             reads=[('stage', sl)], writes=[tag])
            ocnt = 0
            for Q in range(NQ):
                qs = slice(Q * 512, (Q + 1) * 512)
                S.dma(xs[:, :, :], self.xbf[:, :, qs], reads=[('xbf', Q)], writes=['xs'])
                S.dma(xr[:, :, :], self.xres[:, :, qs], reads=[('xres', Q)], writes=[('xr', m) for m in range(8)])
                for j in range(NJ):
                    bg, bu = 2 * (j % 2), 2 * (j % 2) + 1
                    for c in range(8):
                        S.op('pe', 'matmul', dict(out=ps[bg][:, :], lhsT=w1[:, c, j * 128:(j + 1) * 128], rhs=xs[:, c, :], start=(c == 0), stop=(c == 7)),
                             reads=['w1', 'xs'], writes=[('ps', bg)])
                    for c in range(8):
                        S.op('pe', 'matmul', dict(out=ps[bu][:, :], lhsT=w1[:, c, DFF + j * 128:DFF + (j + 1) * 128], rhs=xs[:, c, :],
                                                  start=(c == 0), stop=(c == 7)),
                             reads=['w1', 'xs'], writes=[('ps', bu)])
                    S.op('act', 'activation', dict(out=sq[j % 2][:, :], in_=ps[bg][:, :], func=AF.Silu), writes=[('ps', bg), ('sq', j % 2)])
                    S.op('dve', 'tensor_tensor', dict(out=hT[:, j, :], in0=sq[j % 2][:, :], in1=ps[bu][:, :], op=ALU.mult),
                         reads=[('sq', j % 2)], writes=[('ps', bu), ('hT', j)])
                for m in range(8):
                    ms = slice(m * 128, (m + 1) * 128)
                    b = 4 if m % 2 == 0 else 7
                    for j in range(NJ):
                        S.op('pe', 'matmul', dict(out=ps[b][:, :], lhsT=w2[:, j, ms], rhs=hT[:, j, :], start=(j == 0), stop=(j == NJ - 1)),
                             reads=['w2', ('hT', j)], writes=[('ps', b)])
                    S.op('dve', 'scalar_tensor_tensor', dict(out=xr[:, m, :], in0=xr[:, m, :], scalar=ALPHA, in1=ps[b][:, :], op0=ALU.mult, op1=ALU.add),
                         reads=[('xr', m)], writes=[('ps', b), ('xr', m)])
                self.ln_block(S, ps, xr, sq, mean, lnv, rstd, lnp, 2, onesf, epst, x2b, 'xr')
                if not last:
                    S.dma(self.xres[:, :, qs], xr[:, :, :], reads=[('xr', m) for m in range(8)], writes=[('xres', Q)])
                    S.dma(self.xbf[:, :, qs], x2b[:, :, :], reads=[('xob', m) for m in range(8)], writes=[('xbf', Q)])
                else:
                    for tt in range(4):
                        tile = Q * 4 + tt
                        for half in range(2):
                            b = 0 + (ocnt % 4)
                            oi = ocnt % 4
                            ocnt += 1
                            for cc in range(4):
                                c = half * 4 + cc
                                S.op('pe', 'transpose', dict(out=ps[b][:, cc * 128:(cc + 1) * 128], in_=xr[:, c, tt * 128:(tt + 1) * 128],
                                                             identity=identf[:, :]),
                                     reads=[('xr', c), 'identf'], writes=[('ps', b)])
                            S.op('act', 'copy', dict(out=outst[oi][:, :], in_=ps[b][:, :]), writes=[('ps', b), ('outst', oi)])
                            S.dma(self.out[tile * 128:(tile + 1) * 128, half * 512:(half + 1) * 512], outst[oi][:, :],
                                  reads=[('outst', oi)], writes=[('out', tile, half)])
            S.end_phase(block)

    def build(self):
        nc = self.nc
        self._ldi = 0
        with ExitStack() as gs:
            S = Sched(nc, gs)
            self.S = S
            self.phase0(S)
            stop = self.dbg
            for l in range(self.nlayers):
                last = (l == self.nlayers - 1)
                if ('stop_p0' in stop):
                    break
                self.phase_fox(S, l)
                if ('stop_fox' in stop):
                    break
                self.phase_idx(S, l)
                if ('stop_idx' in stop):
                    break
                self.phase_dsa(S, l)
                if ('stop_dsa' in stop):
                    break
                self.phase_merge(S, l)
                if ('stop_merge' in stop):
                    break
                self.phase_ffn(S, l, last)
        return nc


def host_consts(L):
    NT = L // 128
    p = np.arange(128)[:, None]
    f = np.arange(128)[None, :]
    c = {}
    c["c_ident"] = np.eye(128, dtype=np.float32)
    c["c_trimask"] = np.where(p > f, -30000.0, 0.0).astype(np.float32)
    cm = np.zeros((128, 4, 512), np.float32)
    f5 = np.arange(512)[None, :]
    for j in range(4):
        cm[:, j, :] = np.where(f5 > 128 * j + p, -1e30, 0.0)
    c["c_causT"] = cm
    tq = np.arange(NT)[None, :] * 128 + p
    c["c_targ"] = np.minimum(min(TOPK, L // 4), tq + 1).astype(np.float32)
    slopes = np.exp2(-8.0 * (np.arange(8, dtype=np.float32) + 1.0) / 8).astype(np.float32)
    kb = np.zeros((128, NT, 8), np.float32)
    for t in range(NT):
        kb[:, t, :] = (t * 128 + np.arange(128))[:, None] * slopes[None, :] + ALIBI_C
    c["c_kbias"] = kb
    kd = np.zeros((128, NT, 8), np.float32)
    kd[:, :, 2:] = -slopes[None, None, 2:]
    c["c_kbd"] = kd
    ns = np.zeros((128, 8, 128), np.float32)
    for base in (0, 1, 64, 65):
        ns[base, :, :] = -slopes[:, None]
    c["c_negslope"] = ns
    tp = np.zeros((128, L), np.float32)
    t = np.arange(L)
    for base in (0, 64):
        tp[base, :] = (t // 16) * 16
        tp[base + 1, :] = t % 16
    c["c_tpos"] = tp
    return c


def host_weights(w_in, b_forget, w_branch_a, w_branch_b, w_out, ln1_g, ln1_b, w_ffn_in, w_ffn_out, ln2_g, ln2_b):
    def fm(w):
        k = w.shape[1]
        return np.ascontiguousarray(w.reshape(2, k // 128, 128, w.shape[2]).transpose(0, 2, 1, 3))
    m = {}
    m["w_in"] = fm(w_in)
    iw = w_in[:, :, 3656:3664]
    m["w_iwrep"] = fm(np.repeat(iw, 64, axis=2))
    m["bfg"] = np.ascontiguousarray(b_forget.reshape(2, 8, 1))
    m["w_a"] = fm(w_branch_a)
    m["w_b"] = fm(w_branch_b)
    m["w_o"] = fm(w_out)
    lp = np.stack([ln1_g, ln1_b, ln2_g, ln2_b], axis=1)
    m["lnp"] = np.ascontiguousarray(lp.reshape(2, 4, 8, 128).transpose(0, 3, 1, 2))
    m["w_f1"] = fm(w_ffn_in)
    m["w_f2"] = fm(w_ffn_out)
    return m


_CACHE = {}


def kernel(x, w_in, b_forget, w_branch_a, w_branch_b, w_out, ln1_g, ln1_b,
           w_ffn_in, w_ffn_out, ln2_g, ln2_b):
    x = np.asarray(x, np.float32)
    B, L, _ = x.shape
    if L not in _CACHE:
        _CACHE[L] = Builder(L).build()
    nc = _CACHE[L]
    shared = host_consts(L)
    shared.update(host_weights(*[np.asarray(a, np.float32) for a in
                                 (w_in, b_forget, w_branch_a, w_branch_b, w_out, ln1_g, ln1_b,
                                  w_ffn_in, w_ffn_out, ln2_g, ln2_b)]))
    in_maps = []
    for b in range(B):
        m = dict(shared)
        m["x"] = np.ascontiguousarray(x[b])
        in_maps.append(m)
    res = run_bass_kernel_spmd(nc, in_maps, core_ids=list(range(B)))
    return np.stack([np.asarray(r["out"], np.float32) for r in res.results], axis=0)
```

```python
import math
from contextlib import ExitStack
import numpy as np
import concourse.bass as bass
import concourse.mybir as mybir
from concourse.bass_utils import run_bass_kernel_spmd

F32 = mybir.dt.float32
BF16 = mybir.dt.bfloat16
U8 = mybir.dt.uint8
ALU = mybir.AluOpType
AF = mybir.ActivationFunctionType

D = 1024
DFF = 2816
ALPHA = 4.0 ** 0.25
LN_EPS = 1e-5
IDX_SCALE = (8 ** -0.5) * (64 ** -0.5)
TOPK = 256
BIS_B = 64.0
BIS_N = 14
ALIBI_C = 50.0


class Sched:
    def __init__(self, nc, stack):
        self.nc = nc
        self.names = ['sp', 'act', 'dve', 'pool', 'pe']
        self.lists = {k: [] for k in self.names}
        self.sem = {k: stack.enter_context(nc.semaphore("s_" + k)) for k in ['pe', 'act', 'dve', 'pool']}
        self.cnt = {k: 0 for k in self.sem}
        self.ndma = 12
        self.dsem = {q: [stack.enter_context(nc.semaphore("d_%s%d" % (q, i))) for i in range(self.ndma)]
                     for q in ['sp', 'pool', 'act']}
        self.dcnt = {q: 0 for q in self.dsem}
        self.waited = {k: {} for k in self.names}
        self.lastw = {}
        self.readers = {}
        self.ninstr = 0

    def _wait(self, eng, tok):
        semid, sem, val = tok[0], tok[1], tok[2]
        w = self.waited[eng]
        if w.get(semid, 0) >= val:
            return
        w[semid] = val
        self.lists[eng].append(('w', sem, val))

    def _deps(self, eng, reads, writes):
        for r in reads:
            t = self.lastw.get(r)
            if t is not None:
                if not (t[3] == eng and eng == 'pe'):
                    self._wait(eng, t)
        for wk in writes:
            t = self.lastw.get(wk)
            if t is not None and (t[3] != eng or eng != 'pe'):
                self._wait(eng, t)
            rd = self.readers.get(wk)
            if rd:
                for t in rd.values():
                    if t[3] != eng:
                        self._wait(eng, t)

    def _commit(self, tok, reads, writes):
        for wk in writes:
            self.lastw[wk] = tok
            self.readers[wk] = {}
        for r in reads:
            self.readers.setdefault(r, {})[tok[0]] = tok

    def op(self, eng, meth, kw, reads=(), writes=()):
        fn = lambda e: getattr(e, meth)(**kw)
        self._deps(eng, reads, writes)
        self.cnt[eng] += 1
        tok = (eng, self.sem[eng], self.cnt[eng], eng)
        self.lists[eng].append(('o', fn, self.sem[eng], 1))
        self._commit(tok, reads, writes)
        self.ninstr += 1
        return tok

    def dma(self, out_ap, in_ap, reads=(), writes=(), q='sp'):
        self._deps(q, reads, writes)
        i = self.dcnt[q]
        self.dcnt[q] += 1
        slot, rnd = i % self.ndma, i // self.ndma
        sem = self.dsem[q][slot]
        semid = ('d', q, slot)
        if rnd > 0:
            self._wait(q, (semid, sem, 16 * rnd))
        tok = (semid, sem, 16 * (rnd + 1), 'dma')
        self.lists[q].append(('o', lambda e: e.dma_start(out=out_ap, in_=in_ap), sem, 16))
        self._commit(tok, reads, writes)
        self.ninstr += 1
        return tok

    def end_phase(self, block):
        for q in self.dsem:
            n = self.dcnt[q]
            for slot in range(min(n, self.ndma)):
                last = ((n - 1 - slot) // self.ndma) + 1
                self._wait(q, (('d', q, slot), self.dsem[q][slot], 16 * last))
        decos = {'sp': block.sync, 'act': block.scalar, 'dve': block.vector,
                 'pool': block.gpsimd, 'pe': block.tensor}
        for name in self.names:
            lst = self.lists[name]
            self.lists[name] = []

            def body(e, lst=lst):
                for it in lst:
                    if it[0] == 'w':
                        e.wait_ge(it[1], it[2])
                    else:
                        it[1](e).then_inc(it[2], it[3])
            decos[name](body)


class Builder:
    def __init__(self, L, nlayers=2, dbg=()):
        self.L = L
        self.NT = L // 128
        self.NQ = L // 512
        self.nlayers = nlayers
        self.dbg = dbg
        self.nc = bass.Bass("TRN2", target_bir_lowering=False)
        nc = self.nc
        self.inputs = {}

        NT, NQ = self.NT, self.NQ
        self.shapes = {
            "x": ([L, D], F32), "w_in": ([2, 128, 8, 5712], F32),
            "bfg": ([2, 8, 1], F32), "w_a": ([2, 128, 4, D], F32), "w_b": ([2, 128, 4, D], F32),
            "w_o": ([2, 128, 8, D], F32), "lnp": ([2, 128, 4, 8], F32),
            "w_f1": ([2, 128, 8, 2 * DFF], F32), "w_f2": ([2, 128, 22, D], F32),
            "c_ident": ([128, 128], F32), "c_trimask": ([128, 128], F32),
            "c_causT": ([128, 4, 512], F32), "c_targ": ([128, NT], F32),
            "c_kbias": ([128, NT, 8], F32), "c_kbd": ([128, NT, 8], F32), "c_negslope": ([128, 8, 128], F32), "c_tpos": ([128, L], F32),
        }
        self.scr_shapes = {
            "out": ([L, D], F32),
            "xres": ([128, 8, L], F32), "xbf": ([128, 8, L], BF16), "oa": ([128, 4, L], BF16),
            "ob": ([128, 4, L], BF16), "cspl": ([8, 3, L], BF16), "mskd": ([NQ, 128, NT, 512], U8),
        }
        self._aps = {}

    def __getattr__(self, name):
        d = self.__dict__
        if 'shapes' in d and name in d['shapes']:
            if name not in d['_aps']:
                shp, dt = d['shapes'][name]
                d['_aps'][name] = d['nc'].dram_tensor(name, list(shp), dt, kind="ExternalInput").ap()
                d['inputs'][name] = (tuple(shp), dt)
            return d['_aps'][name]
        if 'scr_shapes' in d and name in d['scr_shapes']:
            if name not in d['_aps']:
                shp, dt = d['scr_shapes'][name]
                kind = "ExternalOutput" if (name in d['dbg'] or name == "out") else "Internal"
                d['_aps'][name] = d['nc'].dram_tensor(name, list(shp), dt, kind=kind).ap()
            return d['_aps'][name]
        raise AttributeError(name)

    def T(self, es, name, shape, dt):
        self._uid = getattr(self, '_uid', 0) + 1
        return es.enter_context(self.nc.sbuf_tensor("%s_%d" % (name, self._uid), list(shape), dt))

    def PS(self, es, n=8):
        self._uid = getattr(self, '_uid', 0) + 1
        return [es.enter_context(self.nc.psum_tensor("ps%d_%d" % (i, self._uid), [128, 512], F32)) for i in range(n)]

    def load_w(self, S, dst, src, C, N, stage, tag, col0=0):
        engs = ['dve', 'act']
        step = stage[0].shape[1]
        for c in range(C):
            for n0 in range(0, N, step):
                n1 = min(N, n0 + step)
                i = self._ldi
                self._ldi += 1
                sl = i % 2
                st = stage[sl]
                S.dma(st[:, 0:n1 - n0], src[:, c, col0 + n0:col0 + n1], reads=[tag + 'src'], writes=[('stage', sl)])
                e = engs[i % 2]
                d_ap = dst[:, c, n0:n1]
                s_ap = st[:, 0:n1 - n0]
                S.op(e, 'copy' if e == 'act' else 'tensor_copy', dict(out=d_ap, in_=s_ap),
                     reads=[('stage', sl)], writes=[tag])

    def phase0(self, S):
        nc, L, NT, NQ = self.nc, self.L, self.NT, self.NQ
        with ExitStack() as es:
            ident = self.T(es, "ident", [128, 128], F32)
            xin = [self.T(es, "xin%d" % i, [128, D], F32) for i in range(2)]
            xTf = [self.T(es, "xTf%d" % i, [128, 8, 512], F32) for i in range(2)]
            xTb = [self.T(es, "xTb%d" % i, [128, 8, 512], BF16) for i in range(2)]
            import os
            NB = int(os.environ.get('P0_NB', '8'))
            ps = self.PS(es, NB)
            block = es.enter_context(nc.Block())
            S.dma(ident[:, :], self.c_ident[:, :], writes=['ident'])
            for Q in range(NQ):
                s = Q % 2
                for tt in range(4):
                    tile = Q * 4 + tt
                    sl = tile % 2
                    S.dma(xin[sl][:, :], self.x[tile * 128:(tile + 1) * 128, :], writes=[('xin', sl)])
                    for half in range(2):
                        b = (tile * 2 + half) % NB
                        for cc in range(4):
                            c = half * 4 + cc
                            o_ap = ps[b][:, cc * 128:(cc + 1) * 128]
                            i_ap = xin[sl][:, c * 128:(c + 1) * 128]
                            S.op('pe', 'transpose', dict(out=o_ap, in_=i_ap, identity=ident[:, :]),
                                 reads=[('xin', sl), 'ident'], writes=[('ps', b)])
                        src = ps[b][:, :].rearrange("p (a b) -> p a b", a=4)
                        d1 = xTf[s][:, half * 4:(half + 1) * 4, tt * 128:(tt + 1) * 128]
                        d2 = xTb[s][:, half * 4:(half + 1) * 4, tt * 128:(tt + 1) * 128]
                        MODE = os.environ.get('P0_MODE', 'ad')
                        if 'a' in MODE:
                            S.op('act', 'copy', dict(out=d1, in_=src),
                                 writes=[('ps', b), ('xTf', s, tt, half)])
                        if 'd' in MODE:
                            S.op('dve', 'tensor_copy', dict(out=d2, in_=src),
                                 writes=[('ps', b), ('xTb', s, tt, half)])
                rk = [('xTf', s, tt, h) for tt in range(4) for h in range(2)]
                S.dma(self.xres[:, :, Q * 512:(Q + 1) * 512], xTf[s][:, :, :], reads=rk, writes=[('xres', Q)])
                rk = [('xTb', s, tt, h) for tt in range(4) for h in range(2)]
                S.dma(self.xbf[:, :, Q * 512:(Q + 1) * 512], xTb[s][:, :, :], reads=rk, writes=[('xbf', Q)])
            S.end_phase(block)

    def emit_attention(self, S, ps, PT, PTm, items, Dp=4, sbanks=(0, 1, 2, 6, 7)):
        n = len(items)
        nS = len(sbanks)
        nP = len(PT)
        deferred = []
        for i in range(n + Dp):
            if i < n:
                it = items[i]
                sb = sbanks[i % nS]
                n0 = it['n0']
                nq = len(it['qk'])
                for j, (lh, rh, c0, c1, rk) in enumerate(it['qk']):
                    o_ap = ps[sb][:, c0:c1]
                    S.op('pe', 'matmul', dict(
                        out=o_ap, lhsT=lh, rhs=rh, start=(j == 0), stop=(j == nq - 1)),
                        reads=rk, writes=[('ps', sb)])
                pt = PT[i % nP]
                o_ap = pt[:, n0:512]
                i_ap = ps[sb][:, n0:512]
                b_ap = it['bias']
                S.op('act', 'activation', dict(
                    out=o_ap, in_=i_ap, func=AF.Exp, bias=b_ap, scale=1.0),
                    reads=it['bias_keys'], writes=[('ps', sb), ('pt', i % nP)])
                if it.get('mask') is not None:
                    m_ap, mkey = it['mask']
                    o2 = PTm[i % nP][:, n0:512]
                    S.op('dve', 'tensor_tensor', dict(
                        out=o2, in0=o_ap, in1=m_ap, op=ALU.mult),
                        reads=[('pt', i % nP), mkey], writes=[('ptm', i % nP)])
            k = i - Dp
            if k >= 0:
                it = items[k]
                n0 = it['n0']
                masked = it.get('mask') is not None
                rhs = (PTm if masked else PT)[k % nP][:, n0:512]
                ob = it['obank']
                o_ap = ps[ob][0:it['M'], n0:512]
                lh = it['pv_lhsT']
                S.op('pe', 'matmul', dict(
                    out=o_ap, lhsT=lh, rhs=rhs, start=it['first'], stop=it['last']),
                    reads=[('ptm' if masked else 'pt', k % nP)] + it['v_keys'], writes=[('ps', ob)])
                if it['last']:
                    it['normA']()
                    deferred.append((i + 2, it['normB']))
            while deferred and deferred[0][0] <= i:
                deferred.pop(0)[1]()
        for _, fn in deferred:
            fn()

    def make_norm(self, S, ps, ob, odd, rs, rs2, rinv, bcs, onesf, oT_ap_fn, okey):
        sr = 32 if odd else 64
        p0 = 64 if odd else 0

        def normA():
            S.op('dve', 'tensor_scalar', dict(out=rs[sr:sr + 1, :], in0=ps[ob][sr:sr + 1, :], scalar1=1e-30,
                                                  scalar2=None, op0=ALU.max), writes=[('ps', ob), 'rs'])
            S.op('dve', 'reciprocal', dict(out=rinv[sr:sr + 1, :], in_=rs[sr:sr + 1, :]), reads=['rs'], writes=['rinv'])

        def normB():
            S.op('pe', 'matmul', dict(out=ps[5][:, :], lhsT=onesf[sr:sr + 1, 0:128], rhs=rinv[sr:sr + 1, :],
                                          start=True, stop=True), reads=['rinv', 'onesf'], writes=[('ps', 5)])
            S.op('act', 'copy', dict(out=bcs[:, :], in_=ps[5][:, :]), writes=[('ps', 5), 'bcs'])
            S.op('dve', 'tensor_tensor', dict(out=oT_ap_fn(p0), in0=ps[ob][p0:p0 + 64, :], in1=bcs[p0:p0 + 64, :],
                                                  op=ALU.mult), reads=['bcs'], writes=[('ps', ob), okey])
        return normA, normB

    def phase_fox(self, S, l):
        nc, L, NT, NQ = self.nc, self.L, self.NT, self.NQ
        with ExitStack() as es:
            T = lambda name, shape, dt: self.T(es, name, shape, dt)
            xT = T("xT", [128, 8, L], BF16)
            wq = T("wq", [128, 8, 512], BF16)
            wk = T("wk", [128, 8, 512], BF16)
            wv = T("wv", [128, 8, 512], BF16)
            wf = T("wf", [128, 8, 8], BF16)
            stage = [T("stg%d" % i, [128, 1024], F32) for i in range(2)]
            Vb = T("Vb", [128, NT, 4, 160], BF16)
            qa = [T("qa%d" % i, [67, L], BF16) for i in range(2)]
            ka = [T("ka%d" % i, [67, L], BF16) for i in range(2)]
            negc = T("negc", [128, NT, 8], F32)
            identf = T("identf", [128, 128], F32)
            identb = T("identb", [128, 128], BF16)
            trim = T("trim", [128, 128], BF16)
            onesf = T("onesf", [128, 512], F32)
            bcol = T("bcol", [8, 2], F32)
            e8 = T("e8", [8, 512], F32)
            sp8 = T("sp8", [8, 512], F32)
            cb = [T("cb%d" % i, [8, 512], F32) for i in range(2)]
            r8 = e8
            r9 = sp8
            c3 = [T("c3%d" % i, [8, 3, 512], BF16) for i in range(2)]
            PT = [T("pt%d" % i, [128, 512], BF16) for i in range(6)]
            rs = T("rs", [128, 512], F32)
            rs2 = None
            rinv = T("rinv", [128, 512], F32)
            bcs = T("bcs", [128, 512], F32)
            oT = T("oT", [128, L], BF16)
            ps = self.PS(es)
            block = es.enter_context(nc.Block())

            for Q in range(NQ):
                S.dma(xT[:, :, Q * 512:(Q + 1) * 512], self.xbf[:, :, Q * 512:(Q + 1) * 512],
                      reads=[('xbf', Q)], writes=[('xT', Q)])
            S.dma(identf[:, :], self.c_ident[:, :], writes=['identf'])
            S.dma(stage[0][:, 0:128], self.c_trimask[:, :], writes=[('stage', 0)])
            S.op('dve', 'tensor_copy', dict(out=trim[:, :], in_=stage[0][:, 0:128]), reads=[('stage', 0)], writes=['trim'])
            S.op('dve', 'tensor_copy', dict(out=identb[:, :], in_=identf[:, :]), reads=['identf'], writes=['identb'])
            S.op('pool', 'memset', dict(ap=onesf[:, :], constant=1.0), writes=['onesf'])
            S.op('pool', 'memset', dict(ap=Vb[:, :, :, :], constant=0.0), writes=['Vb0'])
            S.op('pool', 'memset', dict(ap=Vb[:, :, :, 64:65], constant=1.0), reads=['Vb0'], writes=['Vb1'])
            for i in range(2):
                S.op('pool', 'memset', dict(ap=ka[i][64:67, :], constant=1.0), writes=[('kac', i)])
            S.dma(bcol[:, 0:1], self.bfg[l, :, :], writes=['bcol0'])
            S.op('dve', 'tensor_scalar', dict(out=bcol[:, 1:2], in0=bcol[:, 0:1], scalar1=-1.0, scalar2=None,
                                                  op0=ALU.mult), reads=['bcol0'], writes=['bcol1'])
            win = self.w_in[l]
            self.load_w(S, wq, win, 8, 512, stage, 'wq', col0=0)
            self.load_w(S, wk, win, 8, 512, stage, 'wk', col0=512)
            self.load_w(S, wv, win, 8, 512, stage, 'wv', col0=1024)
            self.load_w(S, wf, win, 8, 8, stage, 'wf', col0=1536)

            for Q in range(NQ):
                s = Q % 2
                qs = slice(Q * 512, (Q + 1) * 512)
                for c in range(8):
                    S.op('pe', 'matmul', dict(out=ps[6][0:8, :], lhsT=wf[:, c, :], rhs=xT[:, c, qs],
                                                             start=(c == 0), stop=(c == 7)),
                         reads=['wf', ('xT', Q)], writes=[('ps', 6)])
                S.op('act', 'activation', dict(out=e8[:, :], in_=ps[6][0:8, :], func=AF.Exp, bias=bcol[:, 1:2], scale=-1.0),
                     reads=['bcol1'], writes=[('ps', 6), 'e8'])
                S.op('act', 'activation', dict(out=sp8[:, :], in_=e8[:, :], func=AF.Ln, bias=onesf[0:8, 0:1], scale=1.0),
                     reads=['e8', 'onesf'], writes=['sp8'])
                if Q == 0:
                    init = 0.0
                else:
                    init = cb[1 - s][:, 511:512]
                S.op('dve', 'tensor_tensor_scan', dict(out=cb[s][:, :], data0=onesf[0:8, :], data1=sp8[:, :],
                                                                           initial=init, op0=ALU.mult, op1=ALU.subtract),
                     reads=['sp8', 'onesf', ('cb', 1 - s)], writes=[('cb', s)])
                S.op('dve', 'tensor_copy', dict(out=c3[s][:, 0, :], in_=cb[s][:, :]), reads=[('cb', s)], writes=[('c3a', s)])
                S.op('dve', 'tensor_tensor', dict(out=r8[:, :], in0=cb[s][:, :], in1=c3[s][:, 0, :], op=ALU.subtract),
                     reads=[('cb', s), ('c3a', s)], writes=['e8'])
                S.op('dve', 'tensor_copy', dict(out=c3[s][:, 1, :], in_=r8[:, :]), reads=['e8'], writes=[('c3b', s)])
                S.op('dve', 'tensor_tensor', dict(out=r9[:, :], in0=r8[:, :], in1=c3[s][:, 1, :], op=ALU.subtract),
                     reads=['e8', ('c3b', s)], writes=['sp8'])
                S.op('dve', 'tensor_copy', dict(out=c3[s][:, 2, :], in_=r9[:, :]), reads=['sp8'], writes=[('c3c', s)])
                S.dma(self.cspl[:, :, qs], c3[s][:, :, :], reads=[('c3a', s), ('c3b', s), ('c3c', s)], writes=[('cspl', Q)])
                for tt in range(4):
                    tile = Q * 4 + tt
                    S.op('pe', 'transpose', dict(out=ps[7][:, tile * 8:(tile + 1) * 8],
                                                                           in_=cb[s][:, tt * 128:(tt + 1) * 128],
                                                                           identity=identf[0:8, 0:8]),
                         reads=[('cb', s), 'identf'], writes=[('ps', 7)])
            S.op('dve', 'tensor_scalar', dict(out=negc[:, :, :].rearrange("p a b -> p (a b)"), in0=ps[7][:, 0:NT * 8],
                                                  scalar1=-1.0, scalar2=None, op0=ALU.mult),
                 writes=[('ps', 7), 'negc'])

            for tile in range(NT):
                b = 6 + tile % 2
                ts_ = slice(tile * 128, (tile + 1) * 128)
                for c in range(8):
                    S.op('pe', 'matmul', dict(out=ps[b][:, :], lhsT=xT[:, c, ts_], rhs=wv[:, c, :],
                                                                   start=(c == 0), stop=(c == 7)),
                         reads=['wv', ('xT', tile // 4)], writes=[('ps', b)])
                src = ps[b][:, :].rearrange("p (a b d) -> p a b d", a=4, b=2)
                S.op('act', 'copy', dict(out=Vb[:, tile, :, 0:64], in_=src[:, :, 0, :]),
                     reads=['Vb1'], writes=[('ps', b), ('V', tile, 0)])
                S.op('dve', 'tensor_copy', dict(out=Vb[:, tile, :, 96:160], in_=src[:, :, 1, :]),
                     reads=['Vb1'], writes=[('ps', b), ('V', tile, 1)])

            for h in range(8):
                hb = h % 2
                pr = h // 2
                odd = (h % 2 == 1)
                hs = slice(h * 64, (h + 1) * 64)
                S.dma(qa[hb][64:67, :], self.cspl[h, :, :], reads=[('cspl', Q) for Q in range(NQ)], writes=[('qac', hb)])
                for Q in range(NQ):
                    qs = slice(Q * 512, (Q + 1) * 512)
                    for c in range(8):
                        S.op('pe', 'matmul', dict(out=ps[6][0:64, :], lhsT=wq[:, c, hs], rhs=xT[:, c, qs],
                                                                 start=(c == 0), stop=(c == 7)),
                             reads=['wq', ('xT', Q)], writes=[('ps', 6)])
                    S.op('dve', 'tensor_scalar', dict(out=qa[hb][0:64, qs], in0=ps[6][0:64, :], scalar1=0.125,
                                                                scalar2=None, op0=ALU.mult),
                         writes=[('ps', 6), ('qa', hb, Q)])
                    for c in range(8):
                        S.op('pe', 'matmul', dict(out=ps[7][0:64, :], lhsT=wk[:, c, hs], rhs=xT[:, c, qs],
                                                                 start=(c == 0), stop=(c == 7)),
                             reads=['wk', ('xT', Q)], writes=[('ps', 7)])
                    S.op('dve', 'tensor_copy', dict(out=ka[hb][0:64, qs], in_=ps[7][0:64, :]),
                         writes=[('ps', 7), ('ka', hb, Q)])
                items = []
                for Q in range(NQ):
                    ob = 3 + Q % 2
                    qs = slice(Q * 512, (Q + 1) * 512)
                    nA, nB = self.make_norm(S, ps, ob, odd, rs, rs2, rinv, bcs, onesf,
                                            lambda p0, qs=qs: oT[p0:p0 + 64, qs], ('oT', Q))
                    for kb in range(4 * Q + 4):
                        j = kb - 4 * Q
                        n0 = 128 * j if j > 0 else 0
                        ks = slice(kb * 128, (kb + 1) * 128)
                        qk = [(ka[hb][0:67, ks], qa[hb][0:67, Q * 512 + n0:(Q + 1) * 512], n0, 512,
                               [('qa', hb, Q), ('qac', hb), ('ka', hb, kb // 4), ('kac', hb)])]
                        if j >= 0:
                            qk.append((identb[:, :], trim[:, :], n0, n0 + 128, ['identb', 'trim']))
                        if odd:
                            lh, M = Vb[:, kb, pr, 32:160], 128
                        else:
                            lh, M = Vb[:, kb, pr, 0:128], 128
                        items.append(dict(qk=qk, n0=n0, bias=negc[:, kb, h:h + 1], bias_keys=['negc'],
                                          pv_lhsT=lh, M=M, obank=ob, first=(kb == 0), last=(kb == 4 * Q + 3),
                                          v_keys=[('V', kb, 0), ('V', kb, 1), 'Vb1'], normA=nA, normB=nB))
                self.emit_attention(S, ps, PT, None, items)
                if odd:
                    S.dma(self.oa[:, pr, :], oT[:, :], reads=[('oT', Q) for Q in range(NQ)], writes=[('oa', pr)])
            S.end_phase(block)

    def phase_idx(self, S, l):
        nc, L, NT, NQ = self.nc, self.L, self.NT, self.NQ
        topk = min(TOPK, L // 4)
        with ExitStack() as es:
            T = lambda name, shape, dt: self.T(es, name, shape, dt)
            wik2 = T("wik2", [128, 8, 128], BF16)
            wiq = T("wiq", [128, 8, 512], BF16)
            wiw = T("wiw", [128, 8, 8], BF16)
            stage = [T("stg%d" % i, [128, 2048], F32) for i in range(2)]
            ikT = T("ikT", [128, L], BF16)
            xs = [T("xs%d" % i, [128, 8, 512], BF16) for i in range(2)]
            sc = T("sc", [128, 4, L], F32)
            mkq = T("mkq", [128, 4, L], BF16)
            junk = T("junk", [128, L], BF16)
            iqT = T("iqT", [128, 4, 512], BF16)
            rl = [T("rl%d" % i, [128, 512], F32) for i in range(4)]
            wtk = T("wtk", [128, 4, 8], F32)
            caus = T("caus", [128, 4, 512], F32)
            targ = T("targ", [128, NT], F32)
            lo = T("lo", [128, 4], F32)
            hi = T("hi", [128, 4], F32)
            step = T("step", [128, 4], F32)
            thr = T("thr", [128, 4], F32)
            cnt = T("cnt", [128, 4], F32)
            ge = T("ge", [128, 4], F32)
            identb = T("identb", [128, 128], BF16)
            mk = T("mk", [128, NT, 512], U8)
            ps = self.PS(es, 6)
            self._uid += 1
            pstb = [es.enter_context(nc.psum_tensor("pst%d_%d" % (i, self._uid), [128, 1024], BF16)) for i in range(2)]
            block = es.enter_context(nc.Block())

            win = self.w_in[l]
            S.dma(caus[:, :, :], self.c_causT[:, :, :], writes=['caus'])
            S.dma(targ[:, :], self.c_targ[:, :], writes=['targ'])
            S.dma(stage[0][:, 0:128], self.c_ident[:, :], writes=[('stage', 0)])
            S.op('dve', 'tensor_copy', dict(out=identb[:, :], in_=stage[0][:, 0:128]), reads=[('stage', 0)], writes=['identb'])
            self.load_w(S, wik2[:, :, 0:64], win, 8, 64, stage, 'wik', col0=3592)
            S.op('dve', 'tensor_copy', dict(out=wik2[:, :, 64:128], in_=wik2[:, :, 0:64]), reads=['wik'], writes=['wik2'])
            self.load_w(S, wiq, win, 8, 512, stage, 'wiq', col0=3080)
            self.load_w(S, wiw, win, 8, 8, stage, 'wiw', col0=3656)

            xcnt = [0]

            def load_xs(Q):
                i = xcnt[0] % 2
                xcnt[0] += 1
                S.dma(xs[i][:, :, :], self.xbf[:, :, Q * 512:(Q + 1) * 512], reads=[('xbf', Q)], writes=[('xs', i)])
                return i

            for Q in range(NQ):
                i = load_xs(Q)
                for c in range(8):
                    S.op('pe', 'matmul', dict(out=ps[4][:, :], lhsT=wik2[:, c, :], rhs=xs[i][:, c, :], start=(c == 0), stop=(c == 7)),
                         reads=['wik', 'wik2', ('xs', i)], writes=[('ps', 4)])
                S.op('act', 'copy', dict(out=ikT[:, Q * 512:(Q + 1) * 512], in_=ps[4][:, :]), writes=[('ps', 4), ('ikT', Q)])

            rcnt = 0
            for Q in range(NQ):
                i = load_xs(Q)
                nk = (Q + 1) * 512
                nkb = 4 * Q + 4
                for pr in range(4):
                    ms = slice(pr * 128, (pr + 1) * 128)
                    for c in range(8):
                        S.op('pe', 'matmul', dict(out=ps[4][:, :], lhsT=wiq[:, c, ms], rhs=xs[i][:, c, :], start=(c == 0), stop=(c == 7)),
                             reads=['wiq', ('xs', i)], writes=[('ps', 4)])
                    S.op('act', 'copy', dict(out=iqT[:, pr, :], in_=ps[4][:, :]), writes=[('ps', 4), ('iqT', pr)])
                for qsub in range(4):
                    for c in range(8):
                        S.op('pe', 'matmul', dict(out=ps[5][:, qsub * 8:(qsub + 1) * 8], lhsT=xs[i][:, c, qsub * 128:(qsub + 1) * 128],
                                                  rhs=wiw[:, c, :], start=(c == 0), stop=(c == 7)),
                             reads=['wiw', ('xs', i)], writes=[('ps', 5)])
                S.op('dve', 'tensor_scalar', dict(out=wtk[:, :, :].rearrange("p a b -> p (a b)"), in0=ps[5][:, 0:32], scalar1=IDX_SCALE,
                                                  scalar2=None, op0=ALU.mult), writes=[('ps', 5), 'wtk'])
                for kc in range(Q + 1):
                    ksl = slice(kc * 512, (kc + 1) * 512)
                    for h in range(8):
                        pr, half = h // 2, h % 2
                        rows = slice(half * 64, half * 64 + 64)
                        for qsub in range(4):
                            rb = rcnt % 4
                            rcnt += 1
                            S.op('pe', 'matmul', dict(out=ps[rb][:, :], lhsT=iqT[rows, pr, qsub * 128:(qsub + 1) * 128], rhs=ikT[rows, ksl],
                                                      start=True, stop=True),
                                 reads=[('ikT', kc), ('iqT', pr)], writes=[('ps', rb)])
                            S.op('act', 'activation', dict(out=rl[rb][:, :], in_=ps[rb][:, :], func=AF.Relu),
                                 writes=[('ps', rb), ('rl', rb)])
                            if h == 0:
                                S.op('dve', 'tensor_scalar', dict(out=sc[:, qsub, ksl], in0=rl[rb][:, :], scalar1=wtk[:, qsub, h:h + 1],
                                                                  scalar2=None, op0=ALU.mult),
                                     reads=[('rl', rb), 'wtk'], writes=[('sc', qsub, kc)])
                            else:
                                S.op('dve', 'scalar_tensor_tensor', dict(out=sc[:, qsub, ksl], in0=rl[rb][:, :], scalar=wtk[:, qsub, h:h + 1],
                                                                         in1=sc[:, qsub, ksl], op0=ALU.mult, op1=ALU.add),
                                     reads=[('rl', rb), 'wtk', ('sc', qsub, kc)], writes=[('sc', qsub, kc)])
                allk = lambda qsub: [('sc', qsub, kc) for kc in range(Q + 1)]
                for qsub in range(4):
                    S.op('dve', 'tensor_reduce', dict(out=lo[:, qsub:qsub + 1], in_=sc[:, qsub, 0:nk], axis=mybir.AxisListType.X, op=ALU.min),
                         reads=allk(qsub), writes=[('lo', qsub)])
                for qsub in range(4):
                    S.op('pool', 'tensor_tensor', dict(out=sc[:, qsub, Q * 512:nk], in0=sc[:, qsub, Q * 512:nk], in1=caus[:, qsub, :], op=ALU.add),
                         reads=[('sc', qsub, Q), 'caus', ('lo', qsub)], writes=[('sc', qsub, Q)])
                for qsub in range(4):
                    S.op('dve', 'tensor_reduce', dict(out=hi[:, qsub:qsub + 1], in_=sc[:, qsub, 0:nk], axis=mybir.AxisListType.X, op=ALU.max),
                         reads=allk(qsub), writes=[('hi', qsub)])
                S.op('dve', 'tensor_tensor', dict(out=step[:, :], in0=hi[:, :], in1=lo[:, :], op=ALU.subtract),
                     reads=[('hi', q) for q in range(4)] + [('lo', q) for q in range(4)], writes=['step'])
                for itn in range(BIS_N):
                    S.op('dve', 'tensor_scalar', dict(out=step[:, :], in0=step[:, :], scalar1=0.5, scalar2=None, op0=ALU.mult),
                         reads=['step'], writes=['step'])
                    S.op('dve', 'tensor_tensor', dict(out=thr[:, :], in0=lo[:, :], in1=step[:, :], op=ALU.add),
                         reads=['step'] + [('lo', q) for q in range(4)], writes=['thr'])
                    for qsub in range(4):
                        S.op('dve', 'tensor_scalar', dict(out=junk[:, 0:nk], in0=sc[:, qsub, 0:nk], scalar1=thr[:, qsub:qsub + 1], scalar2=0.0,
                                                          op0=ALU.is_ge, op1=ALU.add, accum_out=cnt[:, qsub:qsub + 1]),
                             reads=allk(qsub) + ['thr'], writes=['junk', ('cnt', qsub)])
                    S.op('dve', 'tensor_tensor', dict(out=ge[:, :], in0=cnt[:, :], in1=targ[:, 4 * Q:4 * Q + 4], op=ALU.is_ge),
                         reads=[('cnt', q) for q in range(4)] + ['targ'], writes=['ge'])
                    S.op('dve', 'tensor_tensor', dict(out=ge[:, :], in0=ge[:, :], in1=step[:, :], op=ALU.mult),
                         reads=['ge', 'step'], writes=['ge'])
                    S.op('dve', 'tensor_tensor', dict(out=lo[:, :], in0=lo[:, :], in1=ge[:, :], op=ALU.add),
                         reads=['ge'] + [('lo', q) for q in range(4)], writes=[('lo', q) for q in range(4)])
                for qsub in range(4):
                    S.op('dve', 'tensor_scalar', dict(out=mkq[:, qsub, 0:nk], in0=sc[:, qsub, 0:nk], scalar1=lo[:, qsub:qsub + 1], scalar2=None,
                                                      op0=ALU.is_ge),
                         reads=allk(qsub) + [('lo', qsub)], writes=[('mkq', qsub)])
                for kb in range(nkb):
                    pb = kb % 2
                    for qsub in range(4):
                        S.op('pe', 'transpose', dict(out=pstb[pb][:, qsub * 128:(qsub + 1) * 128],
                                                     in_=mkq[:, qsub, kb * 128:(kb + 1) * 128], identity=identb[:, :]),
                             reads=[('mkq', qsub), 'identb'], writes=[('pst', pb)])
                    S.op('act', 'copy', dict(out=mk[:, kb, :], in_=pstb[pb][:, 0:512]), writes=[('pst', pb), ('mk', kb)])
                S.dma(self.mskd[Q, :, 0:nkb, :], mk[:, 0:nkb, :], reads=[('mk', kb) for kb in range(nkb)], writes=[('mskd', Q)])
            S.end_phase(block)

    def phase_dsa(self, S, l):
        nc, L, NT, NQ = self.nc, self.L, self.NT, self.NQ
        with ExitStack() as es:
            T = lambda name, shape, dt: self.T(es, name, shape, dt)
            wdq = T("wdq", [128, 8, 512], BF16)
            wdk = T("wdk", [128, 8, 512], BF16)
            wdv = T("wdv", [128, 8, 512], BF16)
            stage = [T("stg%d" % i, [128, 2048], F32) for i in range(2)]
            xs = [T("xs%d" % i, [128, 8, 512], BF16) for i in range(2)]
            kT = T("kT", [128, 4, L], BF16)
            Vb = T("Vb", [128, NT, 4, 160], BF16)
            qd = T("qd", [128, 4, 512], BF16)
            mk = T("mk", [128, NT, 512], U8)
            kbias = T("kbias", [128, NT, 8], F32)
            kbd = T("kbd", [128, NT, 8], F32)
            kbq = T("kbq", [128, NT, 8], F32)
            nsl = T("nsl", [128, 8, 128], BF16)
            tpq = T("tpq", [128, 512], BF16)
            PT = [T("pt%d" % i, [128, 512], BF16) for i in range(6)]
            PTm = [T("ptm%d" % i, [128, 512], BF16) for i in range(6)]
            rs = T("rs", [128, 512], F32)
            rs2 = None
            rinv = T("rinv", [128, 512], F32)
            bcs = T("bcs", [128, 512], F32)
            onesf = T("onesf", [128, 128], F32)
            oTq = [T("oTq%d" % i, [128, 4, 512], BF16) for i in range(2)]
            identb = T("identb", [128, 128], BF16)
            trim = T("trim", [128, 128], BF16)
            ps = self.PS(es)
            block = es.enter_context(nc.Block())
            S.dma(stage[1][:, 0:128], self.c_trimask[:, :], writes=[('stage', 1)])
            S.op('dve', 'tensor_copy', dict(out=trim[:, :], in_=stage[1][:, 0:128]), reads=[('stage', 1)], writes=['trim'])
            S.dma(stage[1][:, 128:256], self.c_ident[:, :], reads=['trim'], writes=[('stage', 1)])
            S.op('dve', 'tensor_copy', dict(out=identb[:, :], in_=stage[1][:, 128:256]), reads=[('stage', 1)], writes=['identb'])

            win = self.w_in[l]
            S.dma(kbias[:, :, :], self.c_kbias[:, :, :], writes=['kbias'])
            S.dma(kbd[:, :, :], self.c_kbd[:, :, :], writes=['kbd'])
            S.dma(stage[0][:, 0:1024], self.c_negslope[:, :, :].rearrange("p a b -> p (a b)"), writes=[('stage', 0)])
            S.op('dve', 'tensor_copy', dict(out=nsl[:, :, :].rearrange("p a b -> p (a b)"), in_=stage[0][:, 0:1024]),
                 reads=[('stage', 0)], writes=['nsl'])
            S.op('pool', 'memset', dict(ap=onesf[:, :], constant=1.0), writes=['onesf'])
            S.op('pool', 'memset', dict(ap=Vb[:, :, :, :], constant=0.0), writes=['Vb0'])
            S.op('pool', 'memset', dict(ap=Vb[:, :, :, 64:65], constant=1.0), reads=['Vb0'], writes=['Vb1'])
            self.load_w(S, wdq, win, 8, 512, stage, 'wdq', col0=1544)
            self.load_w(S, wdk, win, 8, 512, stage, 'wdk', col0=2056)
            self.load_w(S, wdv, win, 8, 512, stage, 'wdv', col0=2568)
            xcnt = [0]

            def load_xs(Q):
                i = xcnt[0] % 2
                xcnt[0] += 1
                S.dma(xs[i][:, :, :], self.xbf[:, :, Q * 512:(Q + 1) * 512], reads=[('xbf', Q)], writes=[('xs', i)])
                return i

            for Q in range(NQ):
                i = load_xs(Q)
                qs = slice(Q * 512, (Q + 1) * 512)
                for pr in range(4):
                    ms = slice(pr * 128, (pr + 1) * 128)
                    for c in range(8):
                        S.op('pe', 'matmul', dict(out=ps[6][:, :], lhsT=wdk[:, c, ms], rhs=xs[i][:, c, :], start=(c == 0), stop=(c == 7)),
                             reads=['wdk', ('xs', i)], writes=[('ps', 6)])
                    S.op('dve', 'tensor_copy', dict(out=kT[:, pr, qs], in_=ps[6][:, :]), writes=[('ps', 6), ('kT', Q)])
                for tt in range(4):
                    tile = Q * 4 + tt
                    for c in range(8):
                        S.op('pe', 'matmul', dict(out=ps[7][:, :], lhsT=xs[i][:, c, tt * 128:(tt + 1) * 128], rhs=wdv[:, c, :],
                                                  start=(c == 0), stop=(c == 7)),
                             reads=['wdv', ('xs', i)], writes=[('ps', 7)])
                    src = ps[7][:, :].rearrange("p (a b d) -> p a b d", a=4, b=2)
                    S.op('act', 'copy', dict(out=Vb[:, tile, :, 0:64], in_=src[:, :, 0, :]), reads=['Vb1'], writes=[('ps', 7), ('V', tile, 0)])
                    S.op('dve', 'tensor_copy', dict(out=Vb[:, tile, :, 96:160], in_=src[:, :, 1, :]), reads=['Vb1'],
                         writes=[('ps', 7), ('V', tile, 1)])

            for Q in range(NQ):
                i = load_xs(Q)
                nkb = 4 * Q + 4
                qs = slice(Q * 512, (Q + 1) * 512)
                S.dma(mk[:, 0:nkb, :], self.mskd[Q, :, 0:nkb, :], reads=[('mskd', Q)], writes=['mk'])
                S.dma(stage[1][:, 0:512], self.c_tpos[:, qs], writes=[('stage', 1)])
                S.op('dve', 'tensor_copy', dict(out=tpq[:, :], in_=stage[1][:, 0:512]), reads=[('stage', 1)], writes=['tpq'])
                for pr in range(4):
                    ms = slice(pr * 128, (pr + 1) * 128)
                    for c in range(8):
                        S.op('pe', 'matmul', dict(out=ps[6][:, :], lhsT=wdq[:, c, ms], rhs=xs[i][:, c, :], start=(c == 0), stop=(c == 7)),
                             reads=['wdq', ('xs', i)], writes=[('ps', 6)])
                    S.op('dve', 'tensor_scalar', dict(out=qd[:, pr, :], in0=ps[6][:, :], scalar1=0.125, scalar2=None, op0=ALU.mult),
                         writes=[('ps', 6), ('qd', pr)])
                S.op('dve', 'scalar_tensor_tensor', dict(out=kbq[:, :, :].rearrange("p a b -> p (a b)"),
                                                         in0=kbd[:, :, :].rearrange("p a b -> p (a b)"), scalar=float(Q * 512 + 511),
                                                         in1=kbias[:, :, :].rearrange("p a b -> p (a b)"), op0=ALU.mult, op1=ALU.add),
                     reads=['kbias', 'kbd'], writes=['kbq'])
                items = []
                oq = oTq[Q % 2]
                for h in range(8):
                    half, pr, odd = h % 2, h // 2, (h % 2 == 1)
                    rows = slice(half * 64, half * 64 + 64)
                    r2 = slice(half * 64, half * 64 + 2)
                    ob = 3 + h % 2
                    nA, nB = self.make_norm(S, ps, ob, odd, rs, rs2, rinv, bcs, onesf,
                                            lambda p0, oq=oq, pr=pr: oq[p0:p0 + 64, pr, :], ('oTq', Q % 2, h))
                    for kb in range(nkb):
                        j = kb - 4 * Q
                        n0 = 128 * j if j > 0 else 0
                        ks = slice(kb * 128, (kb + 1) * 128)
                        qk = [(kT[rows, pr, ks], qd[rows, pr, n0:512], n0, 512, [('kT', kb // 4), ('qd', pr)])]
                        if h < 2:
                            qk.append((nsl[rows, h, :], tpq[rows, n0:512], n0, 512, ['nsl', 'tpq']))
                        if j >= 0:
                            qk.append((identb[:, :], trim[:, :], n0, n0 + 128, ['identb', 'trim']))
                        lh = Vb[:, kb, pr, 32:160] if odd else Vb[:, kb, pr, 0:128]
                        items.append(dict(qk=qk, n0=n0, bias=kbq[:, kb, h:h + 1], bias_keys=['kbq'],
                                          mask=(mk[:, kb, n0:512], 'mk'),
                                          pv_lhsT=lh, M=128, obank=ob, first=(kb == 0), last=(kb == nkb - 1),
                                          v_keys=[('V', kb, 0), ('V', kb, 1), 'Vb1'], normA=nA, normB=nB))
                self.emit_attention(S, ps, PT, PTm, items)
                S.dma(self.ob[:, :, qs], oq[:, :, :], reads=[('oTq', Q % 2, h) for h in range(8)], writes=[('ob', Q)])
            S.end_phase(block)

    def ln_block(self, S, ps, xr, sq, mean, lnv, rstd, lnp, gi, onesf, epst, outb, xkey):
        for m in range(8):
            S.op('pe', 'matmul', dict(out=ps[5][:, :], lhsT=onesf[:, :], rhs=xr[:, m, :], start=(m == 0), stop=(m == 7)),
                 reads=['onesf', (xkey, m)], writes=[('ps', 5)])
            S.op('act', 'activation', dict(out=sq[m % 2][:, :], in_=xr[:, m, :], func=AF.Square), reads=[(xkey, m)], writes=[('sq', m % 2)])
            S.op('pe', 'matmul', dict(out=ps[6][:, :], lhsT=onesf[:, :], rhs=sq[m % 2][:, :], start=(m == 0), stop=(m == 7)),
                 reads=['onesf', ('sq', m % 2)], writes=[('ps', 6)])
        S.op('dve', 'tensor_scalar', dict(out=mean[:, :], in0=ps[5][:, :], scalar1=1.0 / D, scalar2=None, op0=ALU.mult),
             writes=[('ps', 5), 'mean'])
        S.op('dve', 'tensor_tensor', dict(out=lnv[:, :], in0=mean[:, :], in1=mean[:, :], op=ALU.mult), reads=['mean'], writes=['lnv'])
        S.op('dve', 'scalar_tensor_tensor', dict(out=lnv[:, :], in0=ps[6][:, :], scalar=1.0 / D, in1=lnv[:, :], op0=ALU.mult, op1=ALU.subtract),
             reads=['lnv'], writes=[('ps', 6), 'lnv'])
        S.op('act', 'activation', dict(out=lnv[:, :], in_=lnv[:, :], func=AF.Ln, bias=epst[:, 0:1], scale=1.0), reads=['lnv', 'epst'], writes=['lnv'])
        S.op('act', 'activation', dict(out=rstd[:, :], in_=lnv[:, :], func=AF.Exp, scale=-0.5), reads=['lnv'], writes=['rstd'])
        for m in range(8):
            S.op('dve', 'tensor_tensor', dict(out=xr[:, m, :], in0=xr[:, m, :], in1=mean[:, :], op=ALU.subtract),
                 reads=[(xkey, m), 'mean'], writes=[(xkey, m)])
            S.op('dve', 'tensor_tensor', dict(out=xr[:, m, :], in0=xr[:, m, :], in1=rstd[:, :], op=ALU.mult),
                 reads=[(xkey, m), 'rstd'], writes=[(xkey, m)])
            S.op('dve', 'tensor_scalar', dict(out=xr[:, m, :], in0=xr[:, m, :], scalar1=lnp[:, gi, m:m + 1], scalar2=lnp[:, gi + 1, m:m + 1],
                                              op0=ALU.mult, op1=ALU.add),
                 reads=[(xkey, m), 'lnp'], writes=[(xkey, m)])
            if outb is not None:
                S.op('pool', 'tensor_copy', dict(out=outb[:, m, :], in_=xr[:, m, :]), reads=[(xkey, m)], writes=[('xob', m)])

    def phase_merge(self, S, l):
        nc, L, NT, NQ = self.nc, self.L, self.NT, self.NQ
        with ExitStack() as es:
            T = lambda name, shape, dt: self.T(es, name, shape, dt)
            wA = T("wA", [128, 4, D], BF16)
            wB = T("wB", [128, 4, D], BF16)
            wO = T("wO", [128, 8, D], BF16)
            wga = T("wga", [128, 8, D], BF16)
            wgb = T("wgb", [128, 8, D], BF16)
            stage = [T("stg%d" % i, [128, 2048], F32) for i in range(2)]
            lnp = T("lnp", [128, 4, 8], F32)
            oab = T("oab", [128, 4, 512], BF16)
            obb = T("obb", [128, 4, 512], BF16)
            xs = T("xs", [128, 8, 512], BF16)
            xr = T("xr", [128, 8, 512], F32)
            mg = T("mg", [128, 8, 512], BF16)
            sa = T("sa", [128, 512], F32)
            sb = T("sb", [128, 512], F32)
            sq = [T("sq%d" % i, [128, 512], F32) for i in range(2)]
            mean = T("mean", [128, 512], F32)
            lnv = T("lnv", [128, 512], F32)
            rstd = T("rstd", [128, 512], F32)
            x1b = T("x1b", [128, 8, 512], BF16)
            onesf = T("onesf", [128, 128], F32)
            epst = T("epst", [128, 1], F32)
            ps = self.PS(es)
            block = es.enter_context(nc.Block())
            S.op('pool', 'memset', dict(ap=onesf[:, :], constant=1.0), writes=['onesf'])
            S.op('pool', 'memset', dict(ap=epst[:, :], constant=LN_EPS), writes=['epst'])
            S.dma(lnp[:, :, :], self.lnp[l], writes=['lnp'])
            self.load_w(S, wA, self.w_a[l], 4, D, stage, 'wA')
            self.load_w(S, wB, self.w_b[l], 4, D, stage, 'wB')
            self.load_w(S, wO, self.w_o[l], 8, D, stage, 'wO')
            self.load_w(S, wga, self.w_in[l], 8, D, stage, 'wga', col0=3664)
            self.load_w(S, wgb, self.w_in[l], 8, D, stage, 'wgb', col0=4688)
            for Q in range(NQ):
                qs = slice(Q * 512, (Q + 1) * 512)
                S.dma(oab[:, :, :], self.oa[:, :, qs], reads=[('oa', p) for p in range(4)], writes=['oab'])
                S.dma(obb[:, :, :], self.ob[:, :, qs], reads=[('ob', Q)], writes=['obb'])
                S.dma(xs[:, :, :], self.xbf[:, :, qs], reads=[('xbf', Q)], writes=['xs'])
                S.dma(xr[:, :, :], self.xres[:, :, qs], reads=[('xres', Q)], writes=[('xr', m) for m in range(8)])
                for m in range(8):
                    ms = slice(m * 128, (m + 1) * 128)
                    for c in range(4):
                        S.op('pe', 'matmul', dict(out=ps[0][:, :], lhsT=wA[:, c, ms], rhs=oab[:, c, :], start=(c == 0), stop=(c == 3)),
                             reads=['wA', 'oab'], writes=[('ps', 0)])
                    for c in range(4):
                        S.op('pe', 'matmul', dict(out=ps[1][:, :], lhsT=wB[:, c, ms], rhs=obb[:, c, :], start=(c == 0), stop=(c == 3)),
                             reads=['wB', 'obb'], writes=[('ps', 1)])
                    for c in range(8):
                        S.op('pe', 'matmul', dict(out=ps[2][:, :], lhsT=wga[:, c, ms], rhs=xs[:, c, :], start=(c == 0), stop=(c == 7)),
                             reads=['wga', 'xs'], writes=[('ps', 2)])
                    for c in range(8):
                        S.op('pe', 'matmul', dict(out=ps[3][:, :], lhsT=wgb[:, c, ms], rhs=xs[:, c, :], start=(c == 0), stop=(c == 7)),
                             reads=['wgb', 'xs'], writes=[('ps', 3)])
                    S.op('act', 'activation', dict(out=sa[:, :], in_=ps[2][:, :], func=AF.Sigmoid), writes=[('ps', 2), 'sa'])
                    S.op('act', 'activation', dict(out=sb[:, :], in_=ps[3][:, :], func=AF.Sigmoid), writes=[('ps', 3), 'sb'])
                    S.op('dve', 'tensor_tensor', dict(out=sa[:, :], in0=sa[:, :], in1=ps[0][:, :], op=ALU.mult), reads=['sa'], writes=[('ps', 0), 'sa'])
                    S.op('dve', 'tensor_tensor', dict(out=sb[:, :], in0=sb[:, :], in1=ps[1][:, :], op=ALU.mult), reads=['sb'], writes=[('ps', 1), 'sb'])
                    S.op('pool', 'tensor_tensor', dict(out=mg[:, m, :], in0=sa[:, :], in1=sb[:, :], op=ALU.add), reads=['sa', 'sb'], writes=[('mg', m)])
                for m in range(8):
                    ms = slice(m * 128, (m + 1) * 128)
                    for c in range(8):
                        S.op('pe', 'matmul', dict(out=ps[4][:, :], lhsT=wO[:, c, ms], rhs=mg[:, c, :], start=(c == 0), stop=(c == 7)),
                             reads=['wO', ('mg', c)], writes=[('ps', 4)])
                    S.op('dve', 'scalar_tensor_tensor', dict(out=xr[:, m, :], in0=xr[:, m, :], scalar=ALPHA, in1=ps[4][:, :], op0=ALU.mult, op1=ALU.add),
                         reads=[('xr', m)], writes=[('ps', 4), ('xr', m)])
                self.ln_block(S, ps, xr, sq, mean, lnv, rstd, lnp, 0, onesf, epst, x1b, 'xr')
                S.dma(self.xres[:, :, qs], xr[:, :, :], reads=[('xr', m) for m in range(8)], writes=[('xres', Q)])
                S.dma(self.xbf[:, :, qs], x1b[:, :, :], reads=[('xob', m) for m in range(8)], writes=[('xbf', Q)])
            S.end_phase(block)

    def phase_ffn(self, S, l, last):
        nc, L, NT, NQ = self.nc, self.L, self.NT, self.NQ
        NJ = DFF // 128
        with ExitStack() as es:
            T = lambda name, shape, dt: self.T(es, name, shape, dt)
            w1 = T("w1", [128, 8, 2 * DFF], BF16)
            w2 = T("w2", [128, NJ, D], BF16)
            stage = [T("stg%d" % i, [128, 512], F32) for i in range(2)]
            lnp = T("lnp", [128, 4, 8], F32)
            xs = T("xs", [128, 8, 512], BF16)
            xr = T("xr", [128, 8, 512], F32)
            hT = T("hT", [128, NJ, 512], BF16)
            sq = [T("sq%d" % i, [128, 512], F32) for i in range(2)]
            mean = T("mean", [128, 512], F32)
            lnv = T("lnv", [128, 512], F32)
            rstd = T("rstd", [128, 512], F32)
            if last:
                outst = [T("outst%d" % i, [128, 512], F32) for i in range(4)]
                identf = T("identf", [128, 128], F32)
                x2b = None
            else:
                x2b = T("x2b", [128, 8, 512], BF16)
            onesf = T("onesf", [128, 128], F32)
            epst = T("epst", [128, 1], F32)
            ps = self.PS(es)
            block = es.enter_context(nc.Block())
            S.op('pool', 'memset', dict(ap=onesf[:, :], constant=1.0), writes=['onesf'])
            S.op('pool', 'memset', dict(ap=epst[:, :], constant=LN_EPS), writes=['epst'])
            S.dma(lnp[:, :, :], self.lnp[l], writes=['lnp'])
            if last:
                S.dma(identf[:, :], self.c_ident[:, :], writes=['identf'])
            engs = ['dve', 'act']
            for (dst, src, C, N, tag) in ((w1, self.w_f1[l], 8, 2 * DFF, 'w1'), (w2, self.w_f2[l], NJ, D, 'w2')):
                for c in range(C):
                    for n0 in range(0, N, 512):
                        i = self._ldi
                        self._ldi += 1
                        sl = i % 2
                        S.dma(stage[sl][:, :], src[:, c, n0:n0 + 512], writes=[('stage', sl)])
                        e = engs[i % 2]
                        S.op(e, 'copy' if e == 'act' else 'tensor_copy', dict(out=dst[:, c, n0:n0 + 512], in_=stage[sl][:, :]),
                             reads=[('stage', sl)], writes=[tag])
            ocnt = 0
            for Q in range(NQ):
                qs = slice(Q * 512, (Q + 1) * 512)
                S.dma(xs[:, :, :], self.xbf[:, :, qs], reads=[('xbf', Q)], writes=['xs'])
                S.dma(xr[:, :, :], self.xres[:, :, qs], reads=[('xres', Q)], writes=[('xr', m) for m in range(8)])
                for j in range(NJ):
                    bg, bu = 2 * (j % 2), 2 * (j % 2) + 1
                    for c in range(8):
                        S.op('pe', 'matmul', dict(out=ps[bg][:, :], lhsT=w1[:, c, j * 128:(j + 1) * 128], rhs=xs[:, c, :], start=(c == 0), stop=(c == 7)),
                             reads=['w1', 'xs'], writes=[('ps', bg)])
                    for c in range(8):
                        S.op('pe', 'matmul', dict(out=ps[bu][:, :], lhsT=w1[:, c, DFF + j * 128:DFF + (j + 1) * 128], rhs=xs[:, c, :],
                                                  start=(c == 0), stop=(c == 7)),
                             reads=['w1', 'xs'], writes=[('ps', bu)])
                    S.op('act', 'activation', dict(out=sq[j % 2][:, :], in_=ps[bg][:, :], func=AF.Silu), writes=[('ps', bg), ('sq', j % 2)])
                    S.op('dve', 'tensor_tensor', dict(out=hT[:, j, :], in0=sq[j % 2][:, :], in1=ps[bu][:, :], op=ALU.mult),
                         reads=[('sq', j % 2)], writes=[('ps', bu), ('hT', j)])
                for m in range(8):
                    ms = slice(m * 128, (m + 1) * 128)
                    b = 4 if m % 2 == 0 else 7
                    for j in range(NJ):
                        S.op('pe', 'matmul', dict(out=ps[b][:, :], lhsT=w2[:, j, ms], rhs=hT[:, j, :], start=(j == 0), stop=(j == NJ - 1)),
                             reads=['w2', ('hT', j)], writes=[('ps', b)])
                    S.op('dve', 'scalar_tensor_tensor', dict(out=xr[:, m, :], in0=xr[:, m, :], scalar=ALPHA, in1=ps[b][:, :], op0=ALU.mult, op1=ALU.add),
                         reads=[('xr', m)], writes=[('ps', b), ('xr', m)])
                self.ln_block(S, ps, xr, sq, mean, lnv, rstd, lnp, 2, onesf, epst, x2b, 'xr')
                if not last:
                    S.dma(self.xres[:, :, qs], xr[:, :, :], reads=[('xr', m) for m in range(8)], writes=[('xres', Q)])
                    S.dma(self.xbf[:, :, qs], x2b[:, :, :], reads=[('xob', m) for m in range(8)], writes=[('xbf', Q)])
                else:
                    for tt in range(4):
                        tile = Q * 4 + tt
                        for half in range(2):
                            b = 0 + (ocnt % 4)
                            oi = ocnt % 4
                            ocnt += 1
                            for cc in range(4):
                                c = half * 4 + cc
                                S.op('pe', 'transpose', dict(out=ps[b][:, cc * 128:(cc + 1) * 128], in_=xr[:, c, tt * 128:(tt + 1) * 128],
                                                             identity=identf[:, :]),
                                     reads=[('xr', c), 'identf'], writes=[('ps', b)])
                            S.op('act', 'copy', dict(out=outst[oi][:, :], in_=ps[b][:, :]), writes=[('ps', b), ('outst', oi)])
                            S.dma(self.out[tile * 128:(tile + 1) * 128, half * 512:(half + 1) * 512], outst[oi][:, :],
                                  reads=[('outst', oi)], writes=[('out', tile, half)])
            S.end_phase(block)

    def build(self):
        nc = self.nc
        self._ldi = 0
        with ExitStack() as gs:
            S = Sched(nc, gs)
            self.S = S
            self.phase0(S)
            stop = self.dbg
            for l in range(self.nlayers):
                last = (l == self.nlayers - 1)
                if ('stop_p0' in stop):
                    break
                self.phase_fox(S, l)
                if ('stop_fox' in stop):
                    break
                self.phase_idx(S, l)
                if ('stop_idx' in stop):
                    break
                self.phase_dsa(S, l)
                if ('stop_dsa' in stop):
                    break
                self.phase_merge(S, l)
                if ('stop_merge' in stop):
                    break
                self.phase_ffn(S, l, last)
        return nc


def host_consts(L):
    NT = L // 128
    p = np.arange(128)[:, None]
    f = np.arange(128)[None, :]
    c = {}
    c["c_ident"] = np.eye(128, dtype=np.float32)
    c["c_trimask"] = np.where(p > f, -30000.0, 0.0).astype(np.float32)
    cm = np.zeros((128, 4, 512), np.float32)
    f5 = np.arange(512)[None, :]
    for j in range(4):
        cm[:, j, :] = np.where(f5 > 128 * j + p, -1e30, 0.0)
    c["c_causT"] = cm
    tq = np.arange(NT)[None, :] * 128 + p
    c["c_targ"] = np.minimum(min(TOPK, L // 4), tq + 1).astype(np.float32)
    slopes = np.exp2(-8.0 * (np.arange(8, dtype=np.float32) + 1.0) / 8).astype(np.float32)
    kb = np.zeros((128, NT, 8), np.float32)
    for t in range(NT):
        kb[:, t, :] = (t * 128 + np.arange(128))[:, None] * slopes[None, :] + ALIBI_C
    c["c_kbias"] = kb
    kd = np.zeros((128, NT, 8), np.float32)
    kd[:, :, 2:] = -slopes[None, None, 2:]
    c["c_kbd"] = kd
    ns = np.zeros((128, 8, 128), np.float32)
    for base in (0, 1, 64, 65):
        ns[base, :, :] = -slopes[:, None]
    c["c_negslope"] = ns
    tp = np.zeros((128, L), np.float32)
    t = np.arange(L)
    for base in (0, 64):
        tp[base, :] = (t // 16) * 16
        tp[base + 1, :] = t % 16
    c["c_tpos"] = tp
    return c


def host_weights(w_in, b_forget, w_branch_a, w_branch_b, w_out, ln1_g, ln1_b, w_ffn_in, w_ffn_out, ln2_g, ln2_b):
    def fm(w):
        k = w.shape[1]
        return np.ascontiguousarray(w.reshape(2, k // 128, 128, w.shape[2]).transpose(0, 2, 1, 3))
    m = {}
    m["w_in"] = fm(w_in)
    m["bfg"] = np.ascontiguousarray(b_forget.reshape(2, 8, 1))
    m["w_a"] = fm(w_branch_a)
    m["w_b"] = fm(w_branch_b)
    m["w_o"] = fm(w_out)
    lp = np.stack([ln1_g, ln1_b, ln2_g, ln2_b], axis=1)
    m["lnp"] = np.ascontiguousarray(lp.reshape(2, 4, 8, 128).transpose(0, 3, 1, 2))
    m["w_f1"] = fm(w_ffn_in)
    m["w_f2"] = fm(w_ffn_out)
    return m


_CACHE = {}


def kernel(x, w_in, b_forget, w_branch_a, w_branch_b, w_out, ln1_g, ln1_b,
           w_ffn_in, w_ffn_out, ln2_g, ln2_b):
    x = np.asarray(x, np.float32)
    B, L, _ = x.shape
    if L not in _CACHE:
        bld = Builder(L)
        _CACHE[L] = (bld.build(), set(bld.inputs))
    nc, used = _CACHE[L]
    shared = host_consts(L)
    shared.update(host_weights(*[np.asarray(a, np.float32) for a in
                                 (w_in, b_forget, w_branch_a, w_branch_b, w_out, ln1_g, ln1_b,
                                  w_ffn_in, w_ffn_out, ln2_g, ln2_b)]))
    in_maps = []
    for b in range(B):
        m = {k: v for k, v in shared.items() if k in used}
        m["x"] = np.ascontiguousarray(x[b])
        in_maps.append(m)
    res = run_bass_kernel_spmd(nc, in_maps, core_ids=list(range(B)))
    return np.stack([np.asarray(r["out"], np.float32) for r in res.results], axis=0)
```
